# Optimizing a Trainium2 kernel written in Bass

```python
import math
import jax, jax.numpy as jnp
from jax import lax
import numpy as np

D_MODEL = 2048
BATCH = 2
SEQ = 8192
DEPTH = 1

D_SG = D_MODEL // 2
SG_GROUPS = 8
SG_GROUP_DIM = D_SG // SG_GROUPS
SG_CHUNK = 128
ATT_HEADS = 8
HEAD_DIM = 128
D_ATT = ATT_HEADS * HEAD_DIM
MOBA_BLOCK = 256
MOBA_TOPK = 3
Q_CHUNK = 32
REL_BUCKETS = 32
REL_MAX_DIST = 128
N_GROUPS = 4
EXPERTS_PER_GROUP = 4
N_EXPERTS = N_GROUPS * EXPERTS_PER_GROUP
TOP_K_IN_GROUP = 2
D_EXPERT = 512
EPS = 1e-6
IN_SIZES = (D_SG, D_SG, D_ATT, D_ATT, D_ATT, D_MODEL, D_MODEL)
IN_COLS = sum(IN_SIZES)
IN_SPLITS = tuple(int(s) for s in np.cumsum(IN_SIZES)[:-1])

kernel_name = "gated_sgu_moba_hmoe_adaln_block"


def rmsnorm(x, w):
    xf = x.astype(jnp.float32)
    y = xf * lax.rsqrt(jnp.mean(xf * xf, axis=-1, keepdims=True) + EPS)
    return (y * w.astype(jnp.float32)).astype(x.dtype)


def layernorm(x, w, b):
    xf = x.astype(jnp.float32)
    mu = jnp.mean(xf, axis=-1, keepdims=True)
    var = jnp.mean(jnp.square(xf - mu), axis=-1, keepdims=True)
    y = (xf - mu) * lax.rsqrt(var + EPS)
    return (y * w.astype(jnp.float32) + b.astype(jnp.float32)).astype(x.dtype)


def t5_bucket(n):
    max_exact = REL_BUCKETS // 2
    nf = jnp.maximum(n, max_exact).astype(jnp.float32)
    large = max_exact + (jnp.log(nf / max_exact) / math.log(REL_MAX_DIST / max_exact)
                         * (REL_BUCKETS - max_exact)).astype(jnp.int32)
    large = jnp.minimum(large, REL_BUCKETS - 1)
    return jnp.where(n < max_exact, n, large)


def spatial_gating(u, v, ln_w, ln_b, w_s, b_s):
    B, S, _ = u.shape
    vn = layernorm(v, ln_w, ln_b).reshape(B, S // SG_CHUNK, SG_CHUNK, SG_GROUPS, SG_GROUP_DIM)
    causal = jnp.tril(jnp.ones((SG_CHUNK, SG_CHUNK), dtype=bool))
    w_m = jnp.where(causal[None], w_s, jnp.zeros_like(w_s))
    z = jnp.einsum('gts,bcsgd->bctgd', w_m, vn) + b_s.T[None, None, :, :, None]
    return u * z.reshape(B, S, D_SG)


def moba_attention(q, k, v, rel_bias):
    B, S, H, dh = q.shape
    nb = -(-S // MOBA_BLOCK)
    pad = nb * MOBA_BLOCK - S
    q = q.transpose(0, 2, 1, 3)
    kb = jnp.pad(k, ((0, 0), (0, pad), (0, 0), (0, 0))).transpose(0, 2, 1, 3).reshape(B, H, nb, MOBA_BLOCK, dh)
    vb = jnp.pad(v, ((0, 0), (0, pad), (0, 0), (0, 0))).transpose(0, 2, 1, 3).reshape(B, H, nb, MOBA_BLOCK, dh)
    qpos = jnp.arange(S, dtype=jnp.int32)
    own = qpos // MOBA_BLOCK
    kmean = jnp.mean(kb.astype(jnp.float32), axis=3)
    score = jnp.einsum('bhsd,bhnd->bhsn', q.astype(jnp.float32), kmean)
    past = jnp.arange(nb, dtype=jnp.int32)[None, :] < own[:, None]
    score = jnp.where(past[None, None], score, -jnp.inf)
    k_sel = min(MOBA_TOPK, nb)
    _, sel = lax.top_k(score, k_sel)
    sel = sel.astype(jnp.int32)
    sel_ok = sel < own[None, None, :, None]
    own_b = jnp.broadcast_to(own[None, None, :, None], (B, H, S, 1))
    blocks = jnp.concatenate([sel, own_b], axis=-1)
    ok = jnp.concatenate([sel_ok, jnp.ones_like(own_b, dtype=bool)], axis=-1)
    nqc = S // Q_CHUNK
    q_c = jnp.moveaxis(q.reshape(B, H, nqc, Q_CHUNK, dh), 2, 0)
    blk_c = jnp.moveaxis(blocks.reshape(B, H, nqc, Q_CHUNK, k_sel + 1), 2, 0)
    ok_c = jnp.moveaxis(ok.reshape(B, H, nqc, Q_CHUNK, k_sel + 1), 2, 0)
    qpos_c = qpos.reshape(nqc, Q_CHUNK)
    bi = jnp.arange(B)[:, None, None, None]
    hi = jnp.arange(H)[None, :, None, None]
    bias_t = rel_bias.T
    scale = HEAD_DIM ** -0.5
    key_off = jnp.arange(MOBA_BLOCK, dtype=jnp.int32)

    def one_chunk(args):
        qc, blk, okc, qp = args
        kg = kb[bi, hi, blk]
        vg = vb[bi, hi, blk]
        kpos = blk[..., None] * MOBA_BLOCK + key_off
        rel = qp[None, None, :, None, None] - kpos
        mask = okc[..., None] & (rel >= 0)
        bias = bias_t[hi[..., None], t5_bucket(jnp.maximum(rel, 0))]
        logits = jnp.einsum('bhqd,bhqnkd->bhqnk', qc, kg).astype(jnp.float32) * scale + bias.astype(jnp.float32)
        logits = jnp.where(mask, logits, -jnp.inf)
        p = jax.nn.softmax(logits, axis=(-2, -1))
        return jnp.einsum('bhqnk,bhqnkd->bhqd', p.astype(vg.dtype), vg)

    out = lax.map(one_chunk, (q_c, blk_c, ok_c, qpos_c))
    return out.transpose(1, 0, 3, 2, 4).reshape(B, S, H * dh)


def hierarchical_moe(h, w_rg, w_re, w1, w3, w2):
    B, S, D = h.shape
    t = h.reshape(B * S, D)
    g_prob = jax.nn.softmax((t @ w_rg).astype(jnp.float32), axis=-1)
    g_p, g_idx = lax.top_k(g_prob, 1)
    e_logits = (t @ w_re).astype(jnp.float32).reshape(-1, N_GROUPS, EXPERTS_PER_GROUP)
    e_logits = jnp.take_along_axis(e_logits, g_idx[:, :, None], axis=1)[:, 0]
    e_p, e_idx = lax.top_k(jax.nn.softmax(e_logits, axis=-1), TOP_K_IN_GROUP)
    e_p = e_p / jnp.sum(e_p, axis=-1, keepdims=True)
    weights = g_p * e_p
    expert_id = g_idx * EXPERTS_PER_GROUP + e_idx
    gates = jnp.sum(jax.nn.one_hot(expert_id, N_EXPERTS, dtype=jnp.float32) * weights[..., None], axis=1)

    def expert_step(acc, xs):
        w1e, w3e, w2e, ge = xs
        y = (jax.nn.silu(t @ w1e) * (t @ w3e)) @ w2e
        return acc + ge[:, None].astype(t.dtype) * y, None

    y, _ = lax.scan(expert_step, jnp.zeros_like(t), (w1, w3, w2, gates.T))
    return y.reshape(B, S, D)


def setup_inputs(seed: int = 0) -> dict:
    key = jax.random.key(seed)
    ks = jax.random.split(key, 24)
    f32 = jnp.float32
    nrm = lambda k, shape, s: jax.random.normal(k, shape, f32) * s
    L, D = DEPTH, D_MODEL
    return {
        "x": nrm(ks[0], (BATCH, SEQ, D), 1.0),
        "c": nrm(ks[1], (BATCH, D), 1.0),
        "w_ada": nrm(ks[2], (L, D, 6 * D), 0.5 * D ** -0.5),
        "b_ada": nrm(ks[3], (L, 6 * D), 0.02),
        "norm1_w": 1.0 + nrm(ks[4], (L, D), 0.05),
        "norm2_w": 1.0 + nrm(ks[5], (L, D), 0.05),
        "final_norm_w": 1.0 + nrm(ks[6], (D,), 0.05),
        "w_in": nrm(ks[7], (L, D, IN_COLS), D ** -0.5),
        "sg_ln_w": 1.0 + nrm(ks[8], (L, D_SG), 0.05),
        "sg_ln_b": nrm(ks[9], (L, D_SG), 0.02),
        "w_spatial": nrm(ks[10], (L, SG_GROUPS, SG_CHUNK, SG_CHUNK), SG_CHUNK ** -0.5),
        "b_spatial": 1.0 + nrm(ks[11], (L, SG_GROUPS, SG_CHUNK), 0.05),
        "rel_bias": nrm(ks[12], (REL_BUCKETS, ATT_HEADS), 0.5),
        "w_out_sg": nrm(ks[13], (L, D_SG, D), D_SG ** -0.5),
        "w_out_att": nrm(ks[14], (L, D_ATT, D), D_ATT ** -0.5),
        "w_o": nrm(ks[15], (L, D, D), D ** -0.5),
        "w_router_group": nrm(ks[16], (L, D, N_GROUPS), D ** -0.5),
        "w_router_expert": nrm(ks[17], (L, D, N_EXPERTS), D ** -0.5),
        "w_exp_gate": nrm(ks[18], (L, N_EXPERTS, D, D_EXPERT), D ** -0.5),
        "w_exp_up": nrm(ks[19], (L, N_EXPERTS, D, D_EXPERT), D ** -0.5),
        "w_exp_down": nrm(ks[20], (L, N_EXPERTS, D_EXPERT, D), D_EXPERT ** -0.5),
    }


def reference(x, c, w_ada, b_ada, norm1_w, norm2_w, final_norm_w, w_in, sg_ln_w, sg_ln_b,
              w_spatial, b_spatial, rel_bias, w_out_sg, w_out_att, w_o, w_router_group,
              w_router_expert, w_exp_gate, w_exp_up, w_exp_down):
    B, S, D = x.shape
    c_act = jax.nn.silu(c)
    for l in range(DEPTH):
        mod = c_act @ w_ada[l] + b_ada[l]
        sh1, sc1, g1, sh2, sc2, g2 = [m[:, None, :] for m in jnp.split(mod, 6, axis=-1)]
        h = rmsnorm(x, norm1_w[l]) * (1.0 + sc1) + sh1
        proj = h @ w_in[l]
        u_a, v_a, q, k, v, gate_sg, gate_att = jnp.split(proj, IN_SPLITS, axis=-1)
        y_sg = spatial_gating(jax.nn.gelu(u_a, approximate=False), jax.nn.gelu(v_a, approximate=False),
                              sg_ln_w[l], sg_ln_b[l], w_spatial[l], b_spatial[l])
        y_att = moba_attention(q.reshape(B, S, ATT_HEADS, HEAD_DIM), k.reshape(B, S, ATT_HEADS, HEAD_DIM),
                               v.reshape(B, S, ATT_HEADS, HEAD_DIM), rel_bias)
        merged = jax.nn.sigmoid(gate_sg) * (y_sg @ w_out_sg[l]) + jax.nn.sigmoid(gate_att) * (y_att @ w_out_att[l])
        x = x + g1 * (merged @ w_o[l])
        h2 = rmsnorm(x, norm2_w[l]) * (1.0 + sc2) + sh2
        x = x + g2 * hierarchical_moe(h2, w_router_group[l], w_router_expert[l],
                                      w_exp_gate[l], w_exp_up[l], w_exp_down[l])
    return rmsnorm(x, final_norm_w)
```

```python
from contextlib import ExitStack
import numpy as np
import concourse.bass as bass
import concourse.mybir as mybir
from concourse.bass_utils import run_bass_kernel_spmd

F32 = mybir.dt.float32
BF16 = mybir.dt.bfloat16
I32 = mybir.dt.int32
ALU = mybir.AluOpType
AF = mybir.ActivationFunctionType
AX = mybir.AxisListType

ENGS = ("pe", "act", "dve", "pool", "sp")


class Tile:
    def __init__(self, name, ap, space):
        self.name = name
        self.ap = ap
        self.space = space
        self.writers = {}
        self.readers = {}
        self.dsem = None
        self.dcount = 0
        self.last_dma = None

    def __getitem__(self, k):
        return self.ap[k]


class Instr:
    __slots__ = ("eng", "fn", "deps", "sig", "sval", "is_dma", "dtile", "dval", "chan")

    def __init__(self, eng, fn):
        self.eng = eng
        self.fn = fn
        self.deps = []
        self.sig = False
        self.sval = 0
        self.is_dma = False
        self.dtile = None
        self.dval = 0
        self.chan = eng


class Arena:
    def __init__(self, sched, ap, words):
        self.S = sched
        self.ap = ap
        self.words = words
        self.top = 0
        self.grave = []
        self.live = {}

    def alloc(self, name, shape, dtype):
        assert shape[0] <= 128
        n = 1
        for s in shape[1:]:
            n *= s
        bpe = 2 if dtype == BF16 else 4
        w = (n * bpe + 3) // 4
        w = (w + 7) // 8 * 8
        lo = self.top
        hi = lo + w
        assert hi <= self.words, f"SBUF arena overflow allocating {name}: {hi} > {self.words}"
        self.top = hi
        v = self.ap[0:shape[0], lo:lo + (n * bpe + 3) // 4]
        if dtype != F32:
            v = v.bitcast(dtype)
        if len(shape) == 3:
            v = v.rearrange("p (a b) -> p a b", a=shape[1])
        elif len(shape) == 4:
            v = v.rearrange("p (a b c) -> p a b c", a=shape[1], b=shape[2])
        t = Tile(name, v, "sb")
        t.lo, t.hi = lo, hi
        for (glo, ghi, gw, gr) in self.grave:
            if glo < hi and lo < ghi:
                for k, i in gw.items():
                    _merge(t.readers, k, i)
                for k, i in gr.items():
                    _merge(t.readers, k, i)
        self.live[name] = t
        return t

    def mark(self):
        return self.top

    def release(self, mark):
        for name in list(self.live):
            t = self.live[name]
            if t.lo >= mark:
                self.grave.append((t.lo, t.hi, dict(t.writers), dict(t.readers)))
                del self.live[name]
        self.top = mark


def _merge(d, k, ins):
    old = d.get(k)
    if old is None or _order(ins) >= _order(old):
        d[k] = ins


_ctr = [0]


def _order(ins):
    return ins.sval


class Sched:
    def __init__(self, nc, arena_words=50688):
        self.nc = nc
        self.es = ExitStack()
        self.instrs = {e: [] for e in ENGS}
        self.n = 0
        self.final = []
        self.dsems = []
        arena_t = self.es.enter_context(nc.sbuf_tensor("arena", [128, arena_words], F32))
        self.arena = Arena(self, arena_t[:, :], arena_words)
        self.psum = []
        for i in range(8):
            p = self.es.enter_context(nc.psum_tensor(f"psb{i}", [128, 512], F32))
            self.psum.append(Tile(f"ps{i}", p[:, :], "ps"))
        self.ps_i = 0
        self.dram_tiles = {}

    def ps(self):
        pool = getattr(self, "ps_pool", None) or list(range(8))
        t = self.psum[pool[self.ps_i % len(pool)]]
        self.ps_i += 1
        return t

    def dram(self, name, shape, dtype, kind="Internal"):
        h = self.nc.dram_tensor(name, list(shape), dtype, kind=kind)
        t = Tile(name, h.ap(), "dram")
        self.dram_tiles[name] = t
        return t

    def _record(self, ins, reads, writes):
        self.n += 1
        ins.sval = self.n
        deps = {}
        for t in reads:
            for k, i in t.writers.items():
                deps[id(i)] = i
        for t in writes:
            for k, i in t.writers.items():
                deps[id(i)] = i
            for k, i in t.readers.items():
                deps[id(i)] = i
        for i in deps.values():
            if i is ins:
                continue
            if ins.eng == "pe" and i.eng == "pe" and not i.is_dma:
                continue
            ins.deps.append(i)
        for t in reads:
            t.readers[ins.chan] = ins
        for t in writes:
            t.writers[ins.chan] = ins
        self.instrs[ins.eng].append(ins)
        return ins

    def op(self, eng, fn, reads=(), writes=()):
        ins = Instr(eng, fn)
        return self._record(ins, list(reads), list(writes))

    def dma(self, q, out, in_, reads=(), writes=(), final=False, semtile=None, **kw):
        reads = list(reads)
        writes = list(writes)
        if semtile is None:
            for t in writes + reads:
                if t.space == "sb":
                    semtile = t
                    break
        assert semtile is not None
        if semtile.dsem is None:
            semtile.dsem = self.es.enter_context(self.nc.semaphore(f"d_{semtile.name}_{len(self.dsems)}"))
            self.dsems.append(semtile.dsem)
            self.dtiles = getattr(self, "dtiles", [])
            self.dtiles.append(semtile)
        ins = Instr(q, lambda e: e.dma_start(out=out, in_=in_, **kw))
        ins.is_dma = True
        ins.dtile = semtile
        semtile.dcount += 16
        ins.dval = semtile.dcount
        ins.chan = ("d", semtile.name)
        prev = semtile.last_dma
        self._record(ins, reads, writes)
        if prev is not None and all(d is not prev for d in ins.deps):
            ins.deps.append(prev)
        semtile.last_dma = ins
        if final:
            self.final.append(ins)
        return ins

    def emit(self):
        nc = self.nc
        fin = Instr("sp", lambda e: e.nop())
        fin.deps = list(self.final) + [t.last_dma for t in getattr(self, "dtiles", []) if t.last_dma is not None]
        self.n += 1
        fin.sval = self.n
        self.instrs["sp"].append(fin)
        for e in ENGS:
            for ins in self.instrs[e]:
                for d in ins.deps:
                    if not d.is_dma:
                        d.sig = True
        EPOCH = 30000
        esems = {}
        for e in ENGS:
            cnt = 0
            for ins in self.instrs[e]:
                if ins.is_dma:
                    continue
                if ins.sig:
                    cnt += 1
                    ins.sval = cnt
                else:
                    ins.sval = -1
            nsem = max(1, (cnt + EPOCH - 1) // EPOCH)
            esems[e] = [self.es.enter_context(nc.semaphore(f"e_{e}_{k}")) for k in range(nsem)]

        def sigof(d):
            if d.is_dma:
                return d.dtile.dsem, d.dval
            k = (d.sval - 1) // EPOCH
            return esems[d.eng][k], d.sval - k * EPOCH

        def body(ename):
            def run(eng):
                seen = {}
                for ins in self.instrs[ename]:
                    need = {}
                    for d in ins.deps:
                        sem, val = sigof(d)
                        key = id(sem)
                        if key not in need or need[key][1] < val:
                            need[key] = (sem, val)
                    for key, (sem, val) in need.items():
                        if seen.get(key, 0) >= val:
                            continue
                        eng.wait_ge(sem, val)
                        seen[key] = val
                    bi = ins.fn(eng)
                    if ins.is_dma:
                        bi.then_inc(ins.dtile.dsem, 16)
                    elif ins.sig:
                        sem, val = sigof(ins)
                        bi.then_inc(sem, 1)
            return run

        with nc.Block() as block:
            block.sync(body("sp"))
            block.scalar(body("act"))
            block.vector(body("dve"))
            block.gpsimd(body("pool"))
            block.tensor(body("pe"))
        self.es.close()


D = 2048
SEQ = 8192
BATCH = 2
NCORE = 8
NBLK = 32
TOK = 2048
KC = 16
D_SG = 1024
D_ATT = 1024
IN_COLS = 9216
C_U, C_V, C_Q, C_K, C_VV, C_GSG, C_GATT = 0, 1024, 2048, 3072, 4096, 5120, 7168
NEXP = 16
DEXP = 512
EPS = 1e-6
BIG = 30000.0
RLEN = 1792


def t5_bucket_np(n):
    n = np.asarray(n)
    nf = np.maximum(n, 16).astype(np.float32)
    large = 16 + (np.log(nf / np.float32(16)) / np.float32(np.log(8.0)) * np.float32(16)).astype(np.int32)
    large = np.minimum(large, 31)
    return np.where(n < 16, n, large)


def host_consts(j):
    c = {}
    c["ident"] = np.eye(128, dtype=np.float32)
    c["antiident"] = np.ascontiguousarray(np.eye(128, dtype=np.float32)[::-1])
    c["tril"] = np.tril(np.ones((128, 128), dtype=np.float32))
    i = np.arange(RLEN)
    d = 256 * j + 767 - i
    E = np.zeros((33, RLEN), dtype=np.float32)
    pos = d >= 0
    bk = t5_bucket_np(np.maximum(d, 0))
    E[bk[pos], i[pos]] = 1.0
    E[32, ~pos] = 1.0
    c["ebkt"] = E
    c["negbig8"] = np.full((1, 8), -BIG, dtype=np.float32)
    blk = np.zeros((3, 8, NBLK), dtype=np.float32)
    for s in range(8):
        own = 4 * s + j
        kb = np.arange(NBLK)
        blk[0, s] = np.where(kb < own, 0.0, -BIG)
        blk[1, s] = np.where(kb < own, -BIG, 0.0)
        blk[2, s] = np.where(kb < 4 * s - 2, 1.0, 0.0)
    c["blkc"] = blk.reshape(1, 3 * 8 * NBLK)
    return c


def build_program(stop_after=None, debug=False):
    nc = bass.Bass("TRN2", target_bir_lowering=False)
    S = Sched(nc)
    A = S.arena
    dbgset = set(debug) if debug else set()

    INSPEC = {
        "xs": [SEQ, D], "xo": [TOK, D], "cT": [128, KC], "w_ada": [D, 6 * D], "b_adaT": [128, 96],
        "n1T": [128, KC], "n2T": [128, KC], "fnw": [1, D], "w_in": [D, IN_COLS], "lnw": [1, D_SG],
        "lnb": [1, D_SG], "w_sp": [8, 128, 128], "b_spT": [128, 8], "relb": [32, 8], "w_osg": [D_SG, D],
        "w_oatt": [D_ATT, D], "w_o": [D, D], "w_rt": [D, 20], "w_eg": [NEXP, D, DEXP], "w_eu": [NEXP, D, DEXP],
        "w_ed": [NEXP, DEXP, D], "ident": [128, 128], "tril": [128, 128], "ebkt": [33, RLEN],
        "negbig8": [1, 8], "blkc": [1, 3 * 8 * NBLK], "antiident": [128, 128],
    }
    declared = {}

    class _In:
        def __getattr__(self, name):
            if name not in declared:
                declared[name] = nc.dram_tensor(name, list(INSPEC[name]), F32, kind="ExternalInput").ap()
            return declared[name]
    IN = _In()
    nc._declared_inputs = declared
    out_d = nc.dram_tensor("out", [TOK, D], F32, kind="ExternalOutput").ap()

    kT_d = S.dram("kT_d", [8, 128, SEQ], BF16, "ExternalOutput" if "kT_d" in dbgset else "Internal")
    v_d = S.dram("v_d", [SEQ, D_ATT], BF16, "ExternalOutput" if "v_d" in dbgset else "Internal")
    hT_d = S.dram("hT_d", [128, KC, TOK], BF16, "ExternalOutput" if "hT_d" in dbgset else "Internal")
    ysgT_d = S.dram("ysgT_d", [128, 8, TOK], BF16, "ExternalOutput" if "ysgT_d" in dbgset else "Internal")
    yattT_d = S.dram("yattT_d", [128, 8, TOK], BF16, "ExternalOutput" if "yattT_d" in dbgset else "Internal")
    gsg_d = S.dram("gsg_d", [128, KC, TOK], BF16, "ExternalOutput" if "gsg_d" in dbgset else "Internal")
    gatt_d = S.dram("gatt_d", [128, KC, TOK], BF16, "ExternalOutput" if "gatt_d" in dbgset else "Internal")
    mrgT_d = S.dram("mrgT_d", [128, KC, TOK], BF16, "ExternalOutput" if "mrgT_d" in dbgset else "Internal")
    x1_d = S.dram("x1_d", [TOK, D], F32, "ExternalOutput" if "x1_d" in dbgset else "Internal")
    h2T_d = S.dram("h2T_d", [128, KC, TOK], BF16, "ExternalOutput" if "h2T_d" in dbgset else "Internal")
    r_d = S.dram("r_d", [8, RLEN], F32, "ExternalOutput" if "r_d" in dbgset else "Internal")
    dbg_d = S.dram("dbg_d", [128, 4096], F32, "ExternalOutput") if debug else None

    def stop(name):
        return stop_after == name

    ident_f = A.alloc("ident_f", [128, 128], F32)
    ident_b = A.alloc("ident_b", [128, 128], BF16)
    ones_f = A.alloc("ones_f", [128, 128], F32)
    modT = A.alloc("modT", [128, 96], F32)
    g1s = A.alloc("g1s", [128, KC], F32)
    g2s = A.alloc("g2s", [128, KC], F32)
    ksum = A.alloc("ksum", [128, 8, NBLK], F32)
    S.dma("sp", ident_f.ap, IN.ident, writes=[ident_f])
    S.dma("pool", ident_b.ap, IN.ident, writes=[ident_b])
    S.op("dve", lambda e: e.memset(ones_f.ap, 1.0), writes=[ones_f])
    S.op("dve", lambda e: e.memset(ksum.ap, 0.0), writes=[ksum])

    def wview(w2d, r0, nrows, c0, ncols):
        return w2d[r0:r0 + nrows, c0:c0 + ncols].rearrange("(k p) n -> p k n", p=128)

    def load_w(dst, w2d, r0, nrows, c0, ncols):
        S.dma("pool", dst.ap, wview(w2d, r0, nrows, c0, ncols), writes=[dst])

    mk0 = A.mark()
    c_sb = A.alloc("c_sb", [128, KC], F32)
    c_act = A.alloc("c_act", [128, KC], BF16)
    badaT = A.alloc("badaT", [128, 96], F32)
    n1T_sb = A.alloc("n1T_sb", [128, KC], F32)
    n2T_sb = A.alloc("n2T_sb", [128, KC], F32)
    S.dma("sp", c_sb.ap, IN.cT, writes=[c_sb])
    S.dma("sp", badaT.ap, IN.b_adaT, writes=[badaT])
    S.dma("sp", n1T_sb.ap, IN.n1T, writes=[n1T_sb])
    S.dma("sp", n2T_sb.ap, IN.n2T, writes=[n2T_sb])
    S.op("act", lambda e: e.activation(out=c_act.ap, in_=c_sb.ap, func=AF.Silu), reads=[c_sb], writes=[c_act])
    wada = [A.alloc(f"wada{i}", [128, KC, 512], BF16) for i in range(3)]
    NAD = 24
    for i in range(min(2, NAD)):
        load_w(wada[i % 3], IN.w_ada, 0, D, i * 512, 512)
    for i in range(NAD):
        if i + 2 < NAD:
            load_w(wada[(i + 2) % 3], IN.w_ada, 0, D, (i + 2) * 512, 512)
        wt = wada[i % 3]
        ps = S.ps()
        for mm in range(4):
            m = i * 4 + mm
            for kc in range(KC):
                S.op("pe", lambda e, ps=ps, wt=wt, mm=mm, kc=kc, m=m: e.matmul(
                    ps.ap[:, mm:mm + 1], lhsT=wt.ap[:, kc, mm * 128:(mm + 1) * 128], rhs=c_act.ap[:, kc:kc + 1],
                    start=(kc == 0), stop=(kc == KC - 1)), reads=[wt, c_act], writes=[ps])
        S.op("dve", lambda e, ps=ps, i=i: e.tensor_tensor(out=modT.ap[:, i * 4:i * 4 + 4], in0=ps.ap[:, 0:4],
                                                            in1=badaT.ap[:, i * 4:i * 4 + 4], op=ALU.add),
             reads=[ps, badaT], writes=[modT])
    S.op("dve", lambda e: e.scalar_tensor_tensor(out=g1s.ap, in0=modT.ap[:, 16:32], scalar=1.0, in1=n1T_sb.ap,
                                                 op0=ALU.add, op1=ALU.mult), reads=[modT, n1T_sb], writes=[g1s])
    S.op("dve", lambda e: e.scalar_tensor_tensor(out=g2s.ap, in0=modT.ap[:, 64:80], scalar=1.0, in1=n2T_sb.ap,
                                                 op0=ALU.add, op1=ALU.mult), reads=[modT, n2T_sb], writes=[g2s])
    SH1, G1, SH2, G2 = 0, 32, 48, 80
    A.release(mk0)

    def finish():
        S.emit()
        return nc

    if stop("p0"):
        S.dma("sp", dbg_d.ap[:, 0:96], modT.ap, reads=[modT], final=True)
        S.dma("sp", dbg_d.ap[:, 96:112], g1s.ap, reads=[g1s], final=True)
        return finish()

    def norm_group(x_src, row0, gs, sh_col, xst, xnb, junk, stat, hT):
        for tt in range(4):
            xt = xst[tt % len(xst)]
            S.dma("sp", xt.ap, x_src[row0 + tt * 128: row0 + (tt + 1) * 128, :], writes=[xt])
            ss = stat[tt]
            S.op("act", lambda e, xt=xt, ss=ss: e.activation(out=junk.ap, in_=xt.ap, func=AF.Square,
                                                               accum_out=ss.ap[:, 0:1]),
                 reads=[xt], writes=[junk, ss])
            S.op("act", lambda e, ss=ss: e.activation(out=ss.ap[:, 1:2], in_=ss.ap[:, 0:1], func=AF.Sqrt,
                                                      scale=1.0 / D, bias=EPS), reads=[ss], writes=[ss])
            S.op("dve", lambda e, ss=ss: e.reciprocal(out=ss.ap[:, 2:3], in_=ss.ap[:, 1:2]), reads=[ss], writes=[ss])
            S.op("act", lambda e, xt=xt, ss=ss, tt=tt: e.activation(out=xnb.ap[:, tt, :], in_=xt.ap, func=AF.Identity,
                                                                      scale=ss.ap[:, 2:3]),
                 reads=[xt, ss], writes=[xnb])
        for kc in range(KC):
            ps = S.ps()
            pb = ps.ap.bitcast(BF16)
            for tt in range(4):
                S.op("pe", lambda e, pb=pb, tt=tt, kc=kc: e.transpose(
                    out=pb[:, tt * 128:(tt + 1) * 128], in_=xnb.ap[:, tt, kc * 128:(kc + 1) * 128],
                    identity=ident_b.ap), reads=[xnb, ident_b], writes=[ps])
            S.op("dve", lambda e, pb=pb, kc=kc: e.tensor_scalar(
                out=hT.ap[:, kc, :], in0=pb[:, 0:512], scalar1=gs.ap[:, kc:kc + 1],
                scalar2=modT.ap[:, sh_col + kc:sh_col + kc + 1], op0=ALU.mult, op1=ALU.add),
                reads=[ps, gs, modT], writes=[hT])

    mkA = A.mark()
    wk = A.alloc("wk", [128, KC, 1024], BF16)
    wv = A.alloc("wv", [128, KC, 1024], BF16)
    for q4 in range(2):
        S.dma("pool", wk.ap[:, :, q4 * 512:(q4 + 1) * 512], wview(IN.w_in, 0, D, C_K + q4 * 512, 512), writes=[wk])
    for q4 in range(2):
        S.dma("pool", wv.ap[:, :, q4 * 512:(q4 + 1) * 512], wview(IN.w_in, 0, D, C_VV + q4 * 512, 512), writes=[wv])
    xst = [A.alloc(f"xst{i}", [128, D], F32) for i in range(3)]
    xnb = A.alloc("xnb", [128, 4, D], BF16)
    junk = A.alloc("junk", [128, D], BF16)
    stat = [A.alloc(f"stat{i}", [128, 4], F32) for i in range(4)]
    hTg = [A.alloc(f"hTg{i}", [128, KC, 512], BF16) for i in range(2)]
    kto = [A.alloc(f"kto{i}", [128, 8, 512], BF16) for i in range(2)]
    vo = [A.alloc(f"vo{i}", [128, 4, 1024], BF16) for i in range(2)]
    NGA = SEQ // 512
    if stop("pA_small"):
        NGA = 2
    for g in range(NGA):
        hT = hTg[g % 2]
        norm_group(IN.xs, g * 512, g1s, SH1, xst, xnb, junk, stat, hT)
        ko = kto[g % 2]
        for h in range(8):
            ps = S.ps()
            for kc in range(KC):
                S.op("pe", lambda e, ps=ps, h=h, kc=kc, hT=hT: e.matmul(
                    ps.ap, lhsT=wk.ap[:, kc, h * 128:(h + 1) * 128], rhs=hT.ap[:, kc, :],
                    start=(kc == 0), stop=(kc == KC - 1)), reads=[wk, hT], writes=[ps])
            S.op("act", lambda e, ps=ps, h=h, ko=ko: e.activation(out=ko.ap[:, h, :], in_=ps.ap, func=AF.Identity), reads=[ps], writes=[ko])
            S.op("dve", lambda e, ko=ko, h=h, g=g: e.tensor_reduce(
                out=ksum.ap[:, h, 2 * g:2 * g + 2], in_=ko.ap[:, h, :].rearrange("p (a b) -> p a b", a=2),
                axis=AX.X, op=ALU.add), reads=[ko], writes=[ksum])
        S.dma("sp", kT_d.ap[:, :, g * 512:(g + 1) * 512].rearrange("h d t -> d h t"), ko.ap,
              reads=[ko], writes=[kT_d])
        vt = vo[g % 2]
        for tt in range(4):
            for cc in range(2):
                ps = S.ps()
                for kc in range(KC):
                    S.op("pe", lambda e, ps=ps, tt=tt, cc=cc, kc=kc, hT=hT: e.matmul(
                        ps.ap, lhsT=hT.ap[:, kc, tt * 128:(tt + 1) * 128], rhs=wv.ap[:, kc, cc * 512:(cc + 1) * 512],
                        start=(kc == 0), stop=(kc == KC - 1)), reads=[wv, hT], writes=[ps])
                if cc == 0:
                    S.op("dve", lambda e, ps=ps, tt=tt, cc=cc, vt=vt: e.tensor_copy(
                        out=vt.ap[:, tt, cc * 512:(cc + 1) * 512], in_=ps.ap), reads=[ps], writes=[vt])
                else:
                    S.op("act", lambda e, ps=ps, tt=tt, cc=cc, vt=vt: e.activation(
                        out=vt.ap[:, tt, cc * 512:(cc + 1) * 512], in_=ps.ap, func=AF.Identity), reads=[ps], writes=[vt])
        S.dma("sp", v_d.ap[g * 512:(g + 1) * 512, :].rearrange("(t p) c -> p t c", p=128), vt.ap,
              reads=[vt], writes=[v_d])
    if stop("pA_small") or stop("pA"):
        S.dma("sp", dbg_d.ap[:, 0:256], ksum.ap.rearrange("p h n -> p (h n)"), reads=[ksum], final=True)
        S.dma("sp", dbg_d.ap[:, 512:608], modT.ap, reads=[modT], final=True)
        S.dma("sp", dbg_d.ap[:, 608:624], g1s.ap, reads=[g1s], final=True)
        fin = S.dma("sp", dbg_d.ap[0:1, 300:301], ones_f.ap[0:1, 0:1], reads=[ones_f, kT_d, v_d], final=True)
        return finish()
    A.release(mkA)
    QT = A.alloc("QT", [128, 8, TOK], BF16)
    mkC = A.mark()
    hTo = A.alloc("hTo", [128, KC, TOK], BF16)
    mkB = A.mark()
    xst = [A.alloc(f"xstB{i}", [128, D], F32) for i in range(2)]
    xnb = A.alloc("xnbB", [128, 4, D], BF16)
    junk = A.alloc("junkB", [128, D], BF16)
    stat = [A.alloc(f"statB{i}", [128, 4], F32) for i in range(4)]
    hTg1 = A.alloc("hTgB", [128, KC, 512], BF16)
    for g in range(4):
        norm_group(IN.xo, g * 512, g1s, SH1, xst, xnb, junk, stat, hTg1)
        S.op("pool", lambda e, g=g: e.tensor_copy(out=hTo.ap[:, :, g * 512:(g + 1) * 512], in_=hTg1.ap),
             reads=[hTg1], writes=[hTo])
    A.release(mkB)
    if stop("pB"):
        S.dma("sp", hT_d.ap, hTo.ap, reads=[hTo], writes=[hT_d], final=True)
        return finish()

    mk1 = A.mark()
    wsl = [A.alloc(f"wsl{i}", [128, KC, 512], BF16) for i in range(2)]
    gvz = A.alloc("gvz", [128, 16, D_SG], BF16)
    lnw_b = A.alloc("lnw_b", [128, D_SG], F32)
    lnb_b = A.alloc("lnb_b", [128, D_SG], F32)
    wmT = A.alloc("wmT", [128, 8, 128], BF16)
    bsT = A.alloc("bsT", [128, 8], F32)
    S.dma("sp", lnw_b.ap, IN.lnw.partition_broadcast(128), writes=[lnw_b])
    S.dma("sp", lnb_b.ap, IN.lnb.partition_broadcast(128), writes=[lnb_b])
    S.dma("sp", bsT.ap, IN.b_spT, writes=[bsT])
    mkw = A.mark()
    tril_sb = A.alloc("tril_sb", [128, 128], F32)
    wsp_sb = A.alloc("wsp_sb", [128, 8, 128], F32)
    S.dma("sp", tril_sb.ap, IN.tril, writes=[tril_sb])
    S.dma("sp", wsp_sb.ap, IN.w_sp.rearrange("g t s -> t g s"), writes=[wsp_sb])
    for g in range(8):
        S.op("dve", lambda e, g=g: e.tensor_tensor(out=wsp_sb.ap[:, g, :], in0=wsp_sb.ap[:, g, :], in1=tril_sb.ap,
                                                   op=ALU.mult), reads=[wsp_sb, tril_sb], writes=[wsp_sb])
    for g2 in range(2):
        ps = S.ps()
        for gg in range(4):
            g = g2 * 4 + gg
            S.op("pe", lambda e, ps=ps, g=g, gg=gg: e.transpose(out=ps.ap[:, gg * 128:(gg + 1) * 128],
                                                                in_=wsp_sb.ap[:, g, :], identity=ident_f.ap),
                 reads=[wsp_sb, ident_f], writes=[ps])
        S.op("dve", lambda e, ps=ps, g2=g2: e.tensor_copy(
            out=wmT.ap[:, g2 * 4:(g2 + 1) * 4, :], in_=ps.ap.rearrange("p (a b) -> p a b", a=4)),
            reads=[ps], writes=[wmT])
    A.release(mkw)
    lstat = [A.alloc(f"lstat{i}", [128, 8], F32) for i in range(2)]
    vtmp = [A.alloc(f"vtmp{i}", [128, D_SG], F32) for i in range(1)]
    vnb = [A.alloc(f"vnb{i}", [128, D_SG], BF16) for i in range(2)]
    junk2 = A.alloc("junk2", [128, D_SG], BF16)
    gub = [A.alloc(f"gub{i}", [128, 512], F32) for i in range(2)]
    ysb = [A.alloc(f"ysb{i}", [128, 512], BF16) for i in range(2)]
    ysT = [A.alloc(f"ysT{i}", [128, 8, 128], BF16) for i in range(2)]

    def tm_linear(wt, tt, ps):
        for kc in range(KC):
            S.op("pe", lambda e, kc=kc: e.matmul(ps.ap, lhsT=hTo.ap[:, kc, tt * 128:(tt + 1) * 128],
                                                  rhs=wt.ap[:, kc, :], start=(kc == 0), stop=(kc == KC - 1)),
                 reads=[hTo, wt], writes=[ps])

    load_w(wsl[0], IN.w_in, 0, D, C_V, 512)
    load_w(wsl[1], IN.w_in, 0, D, C_V + 512, 512)
    for cc in range(2):
        for tt in range(16):
            ps = S.ps()
            tm_linear(wsl[cc], tt, ps)
            S.op("act", lambda e, ps=ps, tt=tt, cc=cc: e.activation(
                out=gvz.ap[:, tt, cc * 512:(cc + 1) * 512], in_=ps.ap, func=AF.Gelu), reads=[ps], writes=[gvz])
    load_w(wsl[0], IN.w_in, 0, D, C_U, 512)
    load_w(wsl[1], IN.w_in, 0, D, C_U + 512, 512)
    for tt in range(16):
        ls = lstat[tt % 2]
        vt = vtmp[0]
        vn = vnb[tt % 2]
        gv = gvz.ap[:, tt, :]
        S.op("act", lambda e, gv=gv, ls=ls: e.activation(out=junk2.ap, in_=gv, func=AF.Identity,
                                                          accum_out=ls.ap[:, 0:1]), reads=[gvz], writes=[junk2, ls])
        S.op("act", lambda e, gv=gv, ls=ls: e.activation(out=junk2.ap, in_=gv, func=AF.Square,
                                                          accum_out=ls.ap[:, 1:2]), reads=[gvz], writes=[junk2, ls])
        S.op("dve", lambda e, ls=ls: e.tensor_scalar(out=ls.ap[:, 2:3], in0=ls.ap[:, 0:1], scalar1=1.0 / D_SG,
                                                     scalar2=None, op0=ALU.mult), reads=[ls], writes=[ls])
        S.op("dve", lambda e, ls=ls: e.tensor_tensor(out=ls.ap[:, 3:4], in0=ls.ap[:, 2:3], in1=ls.ap[:, 2:3],
                                                     op=ALU.mult), reads=[ls], writes=[ls])
        S.op("dve", lambda e, ls=ls: e.scalar_tensor_tensor(out=ls.ap[:, 4:5], in0=ls.ap[:, 1:2], scalar=1.0 / D_SG,
                                                            in1=ls.ap[:, 3:4], op0=ALU.mult, op1=ALU.subtract),
             reads=[ls], writes=[ls])
        S.op("act", lambda e, ls=ls: e.activation(out=ls.ap[:, 5:6], in_=ls.ap[:, 4:5], func=AF.Sqrt, bias=EPS),
             reads=[ls], writes=[ls])
        S.op("dve", lambda e, ls=ls: e.reciprocal(out=ls.ap[:, 6:7], in_=ls.ap[:, 5:6]), reads=[ls], writes=[ls])
        S.op("dve", lambda e, gv=gv, ls=ls, vt=vt: e.tensor_scalar(
            out=vt.ap, in0=gv, scalar1=ls.ap[:, 2:3], scalar2=ls.ap[:, 6:7], op0=ALU.subtract, op1=ALU.mult),
            reads=[gvz, ls], writes=[vt])
        S.op("pool", lambda e, vt=vt: e.tensor_tensor(out=vt.ap, in0=vt.ap, in1=lnw_b.ap, op=ALU.mult),
             reads=[vt, lnw_b], writes=[vt])
        S.op("dve", lambda e, vt=vt, vn=vn: e.tensor_tensor(out=vn.ap, in0=vt.ap, in1=lnb_b.ap, op=ALU.add),
             reads=[vt, lnb_b], writes=[vn])
        for g2 in range(2):
            ps = S.ps()
            for gg in range(4):
                g = g2 * 4 + gg
                S.op("pe", lambda e, ps=ps, g=g, gg=gg, vn=vn: e.matmul(
                    ps.ap[:, gg * 128:(gg + 1) * 128], lhsT=wmT.ap[:, g, :], rhs=vn.ap[:, g * 128:(g + 1) * 128],
                    start=True, stop=True), reads=[wmT, vn], writes=[ps])
            for gg in range(4):
                g = g2 * 4 + gg
                S.op("act", lambda e, ps=ps, g=g, gg=gg, tt=tt: e.activation(
                    out=gvz.ap[:, tt, g * 128:(g + 1) * 128], in_=ps.ap[:, gg * 128:(gg + 1) * 128],
                    func=AF.Identity, bias=bsT.ap[:, g:g + 1]), reads=[ps, bsT], writes=[gvz])
    for tt in range(16):
        yT = ysT[tt % 2]
        for cc in range(2):
            ps = S.ps()
            tm_linear(wsl[cc], tt, ps)
            gu = gub[cc]
            ys = ysb[cc]
            S.op("act", lambda e, ps=ps, gu=gu: e.activation(out=gu.ap, in_=ps.ap, func=AF.Gelu),
                 reads=[ps], writes=[gu])
            S.op("dve", lambda e, gu=gu, ys=ys, tt=tt, cc=cc: e.tensor_tensor(
                out=ys.ap, in0=gu.ap, in1=gvz.ap[:, tt, cc * 512:(cc + 1) * 512], op=ALU.mult),
                reads=[gu, gvz], writes=[ys])
            ps2 = S.ps()
            pb = ps2.ap.bitcast(BF16)
            for q in range(4):
                S.op("pe", lambda e, pb=pb, q=q, ys=ys: e.transpose(
                    out=pb[:, q * 128:(q + 1) * 128], in_=ys.ap[:, q * 128:(q + 1) * 128], identity=ident_b.ap),
                    reads=[ys, ident_b], writes=[ps2])
            S.op("dve", lambda e, pb=pb, cc=cc, yT=yT: e.tensor_copy(
                out=yT.ap[:, cc * 4:(cc + 1) * 4, :], in_=pb[:, 0:512].rearrange("p (a b) -> p a b", a=4)),
                reads=[ps2], writes=[yT])
        S.dma("sp", ysgT_d.ap[:, :, tt * 128:(tt + 1) * 128], yT.ap, reads=[yT], writes=[ysgT_d])
    A.release(mk1)
    if stop("pC1"):
        S.dma("sp", dbg_d.ap[0:1, 300:301], ones_f.ap[0:1, 0:1], reads=[ones_f, ysgT_d], final=True)
        return finish()

    mk2 = A.mark()
    wsl = [A.alloc(f"wslq{i}", [128, KC, 512], BF16) for i in range(3)]
    gst = [A.alloc(f"gst{i}", [128, 4, TOK], BF16) for i in range(2)]
    cols = [C_Q, C_Q + 512] + [C_GSG + i * 512 for i in range(4)] + [C_GATT + i * 512 for i in range(4)]
    for i in range(2):
        load_w(wsl[i], IN.w_in, 0, D, cols[i], 512)
    for i, c0 in enumerate(cols):
        if i + 2 < len(cols):
            load_w(wsl[(i + 2) % 3], IN.w_in, 0, D, cols[i + 2], 512)
        wt = wsl[i % 3]
        stg = gst[i % 2]
        for cl in range(4):
            for tg in range(4):
                ps = S.ps()
                for kc in range(KC):
                    S.op("pe", lambda e, ps=ps, wt=wt, cl=cl, tg=tg, kc=kc: e.matmul(
                        ps.ap, lhsT=wt.ap[:, kc, cl * 128:(cl + 1) * 128], rhs=hTo.ap[:, kc, tg * 512:(tg + 1) * 512],
                        start=(kc == 0), stop=(kc == KC - 1)), reads=[wt, hTo], writes=[ps])
                if i < 2:
                    h = i * 4 + cl
                    S.op("act", lambda e, ps=ps, h=h, tg=tg: e.activation(
                        out=QT.ap[:, h, tg * 512:(tg + 1) * 512], in_=ps.ap, func=AF.Identity, scale=float(128 ** -0.5)),
                        reads=[ps], writes=[QT])
                else:
                    S.op("act", lambda e, ps=ps, cl=cl, tg=tg, stg=stg: e.activation(
                        out=stg.ap[:, cl, tg * 512:(tg + 1) * 512], in_=ps.ap, func=AF.Sigmoid),
                        reads=[ps], writes=[stg])
        if i >= 2:
            dst = gsg_d if i < 6 else gatt_d
            c4 = (i - 2) % 4
            S.dma("sp", dst.ap[:, c4 * 4:(c4 + 1) * 4, :], stg.ap, reads=[stg], writes=[dst])
    A.release(mkC)
    if stop("pC"):
        S.dma("sp", dbg_d.ap[0:1, 300:301], ones_f.ap[0:1, 0:1], reads=[ones_f, gsg_d, gatt_d], final=True)
        S.dma("sp", hT_d.ap[:, 0:8, :], QT.ap, reads=[QT], writes=[hT_d], final=True)
        return finish()
    mkD = A.mark()
    S.ps_pool = [0, 1, 2, 3, 4, 5]
    blk_b = A.alloc("blk_b", [128, 3, 8, NBLK], F32)
    b31_b = A.alloc("b31_b", [128, 8], F32)
    khi = A.alloc("khi", [128, 8, NBLK], BF16)
    S.dma("sp", blk_b.ap.rearrange("p a s n -> p (a s n)"), IN.blkc.partition_broadcast(128), writes=[blk_b])
    S.dma("sp", b31_b.ap, IN.relb[31:32, :].partition_broadcast(128), writes=[b31_b])
    S.op("dve", lambda e: e.tensor_copy(out=khi.ap, in_=ksum.ap), reads=[ksum], writes=[khi])
    mkr = A.mark()
    raug = A.alloc("raug", [33, 8], F32)
    e_sb = A.alloc("e_sb", [33, RLEN], F32)
    r_sb = A.alloc("r_sb", [8, RLEN], F32)
    S.dma("sp", raug.ap[0:32, :], IN.relb, writes=[raug])
    S.dma("sp", raug.ap[32:33, :], IN.negbig8, writes=[raug])
    S.dma("sp", e_sb.ap, IN.ebkt, writes=[e_sb])
    for n0 in range(0, RLEN, 512):
        n = min(512, RLEN - n0)
        ps = S.ps()
        S.op("pe", lambda e, ps=ps, n0=n0, n=n: e.matmul(ps.ap[0:8, 0:n], lhsT=raug.ap, rhs=e_sb.ap[:, n0:n0 + n],
                                                         start=True, stop=True), reads=[raug, e_sb], writes=[ps])
        S.op("dve", lambda e, ps=ps, n0=n0, n=n: e.tensor_copy(out=r_sb.ap[:, n0:n0 + n], in_=ps.ap[0:8, 0:n]),
             reads=[ps], writes=[r_sb])
    S.dma("sp", r_d.ap, r_sb.ap, reads=[r_sb], writes=[r_d])
    A.release(mkr)
    KTh = [A.alloc(f"KTh{i}", [128, SEQ], BF16) for i in range(2)]
    Vh = [A.alloc(f"Vh{i}", [128, 64, 128], BF16) for i in range(2)]
    Bh = [A.alloc(f"Bh{i}", [128, 2, 1536], F32) for i in range(2)]
    Sb = A.alloc("Sb", [128, SEQ], F32)
    Pb = [A.alloc(f"Pb{i}", [128, 1024], BF16) for i in range(2)]
    PTb = [A.alloc(f"PTb{i}", [128, 8, 128], BF16) for i in range(2)]
    yTs = [A.alloc(f"yTs{i}", [128, TOK], BF16) for i in range(2)]
    ob = [A.alloc(f"ob{i}", [128, 128], BF16) for i in range(2)]
    sm = [A.alloc(f"sm{i}", [128, 160], F32) for i in range(2)]
    OPS = [S.psum[6], S.psum[7]]
    Brev = [A.alloc(f"Brev{i}", [128, 1536], F32) for i in range(2)]
    antiI = A.alloc("antiI", [128, 128], F32)
    S.dma("sp", antiI.ap, IN.antiident, writes=[antiI])

    def load_head(h):
        kt, vv, bb = KTh[h % 2], Vh[h % 2], Bh[h % 2]
        S.dma("sp", kt.ap, kT_d.ap[h], reads=[kT_d], writes=[kt])
        for q4 in range(4):
            S.dma("sp", vv.ap[:, q4 * 16:(q4 + 1) * 16, :],
                  v_d.ap[q4 * 2048:(q4 + 1) * 2048, h * 128:(h + 1) * 128].rearrange("(n p) d -> p n d", p=128),
                  reads=[v_d], writes=[vv])
        for hf in range(2):
            brv = Brev[hf]
            src = bass.AP(r_d.ap.tensor, h * RLEN + 128 - 128 * hf, [[1, 128], [1, 1536]])
            S.dma("sp", brv.ap, src, reads=[r_d], writes=[brv])
            for wc in range(3):
                ps = S.ps()
                S.op("pe", lambda e, ps=ps, brv=brv, wc=wc: e.matmul(
                    ps.ap, lhsT=antiI.ap, rhs=brv.ap[:, wc * 512:(wc + 1) * 512], start=True, stop=True),
                    reads=[antiI, brv], writes=[ps])
                S.op("dve", lambda e, ps=ps, bb=bb, hf=hf, wc=wc: e.tensor_copy(
                    out=bb.ap[:, hf, wc * 512:(wc + 1) * 512], in_=ps.ap), reads=[ps], writes=[bb])

    load_head(0)
    it = 0
    for h in range(8):
        if h + 1 < 8:
            load_head(h + 1)
        kt, vv, bb = KTh[h % 2], Vh[h % 2], Bh[h % 2]
        yT = yTs[h % 2]
        for s in range(8):
            nblk = 4 * s + 4
            nch = 2 * s + 2
            for hf in range(2):
                t0 = s * 256 + hf * 128
                w = sm[it % 2]
                ops = OPS[it % 2]
                it += 1
                q_l = QT.ap[:, h, t0:t0 + 128]
                ps = S.ps()
                S.op("pe", lambda e, ps=ps, q_l=q_l, h=h: e.matmul(ps.ap[:, 0:NBLK], lhsT=q_l, rhs=khi.ap[:, h, :],
                                                                  start=True, stop=True), reads=[QT, khi], writes=[ps])
                S.op("dve", lambda e, ps=ps, w=w, s=s: e.tensor_tensor(out=w.ap[:, 0:32], in0=ps.ap[:, 0:NBLK],
                                                                        in1=blk_b.ap[:, 0, s, :], op=ALU.add),
                     reads=[ps, blk_b], writes=[w])
                S.op("dve", lambda e, w=w: e.max(out=w.ap[:, 32:40], in_=w.ap[:, 0:32]), reads=[w], writes=[w])
                S.op("dve", lambda e, w=w: e.tensor_scalar(out=w.ap[:, 40:72], in0=w.ap[:, 0:32], scalar1=w.ap[:, 34:35],
                                                           scalar2=None, op0=ALU.is_lt), reads=[w], writes=[w])
                S.op("dve", lambda e, w=w, s=s: e.tensor_tensor(out=w.ap[:, 40:72], in0=w.ap[:, 40:72],
                                                                 in1=blk_b.ap[:, 1, s, :], op=ALU.mult),
                     reads=[w, blk_b], writes=[w])
                S.op("dve", lambda e, w=w, s=s, h=h: e.scalar_tensor_tensor(
                    out=w.ap[:, 40:72], in0=blk_b.ap[:, 2, s, :], scalar=b31_b.ap[:, h:h + 1], in1=w.ap[:, 40:72],
                    op0=ALU.mult, op1=ALU.add), reads=[w, blk_b, b31_b], writes=[w])
                for c in range(nch):
                    ps = S.ps()
                    S.op("pe", lambda e, ps=ps, q_l=q_l, c=c, kt=kt: e.matmul(
                        ps.ap, lhsT=q_l, rhs=kt.ap[:, c * 512:(c + 1) * 512], start=True, stop=True),
                        reads=[QT, kt], writes=[ps])
                    wc = c - (2 * s - 1)
                    if wc >= 0:
                        S.op("dve", lambda e, ps=ps, c=c, wc=wc, hf=hf, bb=bb: e.tensor_tensor(
                            out=Sb.ap[:, c * 512:(c + 1) * 512], in0=ps.ap, in1=bb.ap[:, hf, wc * 512:(wc + 1) * 512],
                            op=ALU.add), reads=[ps, bb], writes=[Sb])
                    elif c % 2 == 0:
                        S.op("act", lambda e, ps=ps, c=c: e.activation(out=Sb.ap[:, c * 512:(c + 1) * 512], in_=ps.ap,
                                                                        func=AF.Identity), reads=[ps], writes=[Sb])
                    else:
                        S.op("dve", lambda e, ps=ps, c=c: e.tensor_copy(out=Sb.ap[:, c * 512:(c + 1) * 512], in_=ps.ap),
                             reads=[ps], writes=[Sb])
                S.op("dve", lambda e, w=w, nblk=nblk: e.tensor_reduce(
                    out=w.ap[:, 72:72 + nblk], in_=Sb.ap[:, 0:nblk * 256].rearrange("p (n k) -> p n k", k=256),
                    axis=AX.X, op=ALU.max), reads=[Sb], writes=[w])
                S.op("dve", lambda e, w=w, nblk=nblk: e.tensor_tensor(out=w.ap[:, 72:72 + nblk], in0=w.ap[:, 72:72 + nblk],
                                                                       in1=w.ap[:, 40:40 + nblk], op=ALU.add),
                     reads=[w], writes=[w])
                S.op("dve", lambda e, w=w, nblk=nblk: e.tensor_reduce(out=w.ap[:, 136:137], in_=w.ap[:, 72:72 + nblk],
                                                                      axis=AX.X, op=ALU.max, negate=True),
                     reads=[w], writes=[w])
                S.op("dve", lambda e, w=w, nblk=nblk: e.tensor_scalar(out=w.ap[:, 72:72 + nblk], in0=w.ap[:, 40:40 + nblk],
                                                                      scalar1=w.ap[:, 136:137], scalar2=None, op0=ALU.add),
                     reads=[w], writes=[w])
                nseg = s + 1
                for sg in range(nseg):
                    pbuf = Pb[sg % 2]
                    ptb = PTb[sg % 2]
                    for b4 in range(4):
                        n = sg * 4 + b4
                        S.op("act", lambda e, pbuf=pbuf, b4=b4, n=n, w=w: e.activation(
                            out=pbuf.ap[:, b4 * 256:(b4 + 1) * 256], in_=Sb.ap[:, n * 256:(n + 1) * 256], func=AF.Exp,
                            bias=w.ap[:, 72 + n:73 + n], accum_out=w.ap[:, 104 + n:105 + n]),
                            reads=[Sb, w], writes=[pbuf, w])
                    ps = S.ps()
                    pb = ps.ap.bitcast(BF16)
                    for k8 in range(8):
                        S.op("pe", lambda e, pb=pb, k8=k8, pbuf=pbuf: e.transpose(
                            out=pb[:, k8 * 128:(k8 + 1) * 128], in_=pbuf.ap[:, k8 * 128:(k8 + 1) * 128],
                            identity=ident_b.ap), reads=[pbuf, ident_b], writes=[ps])
                    if sg % 2 == 0:
                        S.op("dve", lambda e, pb=pb, ptb=ptb: e.tensor_copy(
                            out=ptb.ap, in_=pb.rearrange("p (a b) -> p a b", a=8)), reads=[ps], writes=[ptb])
                    else:
                        S.op("act", lambda e, pb=pb, ptb=ptb: e.activation(
                            out=ptb.ap, in_=pb.rearrange("p (a b) -> p a b", a=8), func=AF.Identity),
                            reads=[ps], writes=[ptb])
                    for k8 in range(8):
                        ktile = sg * 8 + k8
                        S.op("pe", lambda e, ops=ops, ptb=ptb, k8=k8, ktile=ktile, vv=vv, sg=sg, nseg=nseg: e.matmul(
                            ops.ap[:, 0:128], lhsT=ptb.ap[:, k8, :], rhs=vv.ap[:, ktile, :],
                            start=(sg == 0 and k8 == 0), stop=(sg == nseg - 1 and k8 == 7)),
                            reads=[ptb, vv], writes=[ops])
                S.op("dve", lambda e, w=w, nblk=nblk: e.tensor_reduce(out=w.ap[:, 137:138], in_=w.ap[:, 104:104 + nblk],
                                                                      axis=AX.X, op=ALU.add), reads=[w], writes=[w])
                S.op("dve", lambda e, w=w: e.reciprocal(out=w.ap[:, 138:139], in_=w.ap[:, 137:138]), reads=[w], writes=[w])
                o_sb = ob[it % 2]
                S.op("dve", lambda e, ops=ops, o_sb=o_sb, w=w: e.tensor_scalar(
                    out=o_sb.ap, in0=ops.ap[:, 0:128], scalar1=w.ap[:, 138:139], scalar2=None, op0=ALU.mult),
                    reads=[ops, w], writes=[o_sb])
                ps = S.ps()
                pb = ps.ap.bitcast(BF16)
                S.op("pe", lambda e, pb=pb, o_sb=o_sb: e.transpose(out=pb[:, 0:128], in_=o_sb.ap, identity=ident_b.ap),
                     reads=[o_sb, ident_b], writes=[ps])
                S.op("act", lambda e, pb=pb, yT=yT, t0=t0: e.activation(out=yT.ap[:, t0:t0 + 128], in_=pb[:, 0:128],
                                                                         func=AF.Identity), reads=[ps], writes=[yT])
        S.dma("sp", yattT_d.ap[:, h, :], yT.ap, reads=[yT], writes=[yattT_d])
    S.ps_pool = list(range(8))
    A.release(mkD)
    A.release(mkC - 0)
    if stop("pD"):
        S.dma("sp", dbg_d.ap[0:1, 300:301], ones_f.ap[0:1, 0:1], reads=[ones_f, yattT_d], final=True)
        return finish()
    A.release(0 + ksum.hi)
    mkE = A.mark()
    wos = [A.alloc(f"wos{i}", [128, 8, 512], BF16) for i in range(2)]
    woa = [A.alloc(f"woa{i}", [128, 8, 512], BF16) for i in range(2)]
    ysg_t = [A.alloc(f"ysg_t{i}", [128, 8, 512], BF16) for i in range(2)]
    yat_t = [A.alloc(f"yat_t{i}", [128, 8, 512], BF16) for i in range(2)]
    gs_t = [A.alloc(f"gs_t{i}", [128, 4, 512], BF16) for i in range(2)]
    ga_t = [A.alloc(f"ga_t{i}", [128, 4, 512], BF16) for i in range(2)]
    t1 = [A.alloc(f"t1_{i}", [128, 512], F32) for i in range(2)]
    t2 = [A.alloc(f"t2_{i}", [128, 512], F32) for i in range(2)]
    mst = [A.alloc(f"mst{i}", [128, 4, 512], BF16) for i in range(2)]
    load_w(wos[0], IN.w_osg, 0, D_SG, 0, 512)
    load_w(woa[0], IN.w_oatt, 0, D_ATT, 0, 512)
    k = 0
    for cg in range(4):
        if cg + 1 < 4:
            load_w(wos[(cg + 1) % 2], IN.w_osg, 0, D_SG, (cg + 1) * 512, 512)
            load_w(woa[(cg + 1) % 2], IN.w_oatt, 0, D_ATT, (cg + 1) * 512, 512)
        ws, wa = wos[cg % 2], woa[cg % 2]
        for tg in range(4):
            ys, ya, gs, ga, ms = ysg_t[k % 2], yat_t[k % 2], gs_t[k % 2], ga_t[k % 2], mst[k % 2]
            k += 1
            tsl = slice(tg * 512, (tg + 1) * 512)
            S.dma("sp", ys.ap, ysgT_d.ap[:, :, tsl], reads=[ysgT_d], writes=[ys])
            S.dma("sp", ya.ap, yattT_d.ap[:, :, tsl], reads=[yattT_d], writes=[ya])
            S.dma("sp", gs.ap, gsg_d.ap[:, cg * 4:(cg + 1) * 4, tsl], reads=[gsg_d], writes=[gs])
            S.dma("sp", ga.ap, gatt_d.ap[:, cg * 4:(cg + 1) * 4, tsl], reads=[gatt_d], writes=[ga])
            for cl in range(4):
                ps1 = S.ps()
                for kk in range(8):
                    S.op("pe", lambda e, ps1=ps1, kk=kk, cl=cl, ws=ws, ys=ys: e.matmul(
                        ps1.ap, lhsT=ws.ap[:, kk, cl * 128:(cl + 1) * 128], rhs=ys.ap[:, kk, :],
                        start=(kk == 0), stop=(kk == 7)), reads=[ws, ys], writes=[ps1])
                ps2 = S.ps()
                for kk in range(8):
                    S.op("pe", lambda e, ps2=ps2, kk=kk, cl=cl, wa=wa, ya=ya: e.matmul(
                        ps2.ap, lhsT=wa.ap[:, kk, cl * 128:(cl + 1) * 128], rhs=ya.ap[:, kk, :],
                        start=(kk == 0), stop=(kk == 7)), reads=[wa, ya], writes=[ps2])
                a1, a2 = t1[cl % 2], t2[cl % 2]
                S.op("dve", lambda e, ps1=ps1, a1=a1, gs=gs, cl=cl: e.tensor_tensor(
                    out=a1.ap, in0=ps1.ap, in1=gs.ap[:, cl, :], op=ALU.mult), reads=[ps1, gs], writes=[a1])
                S.op("dve", lambda e, ps2=ps2, a2=a2, ga=ga, cl=cl: e.tensor_tensor(
                    out=a2.ap, in0=ps2.ap, in1=ga.ap[:, cl, :], op=ALU.mult), reads=[ps2, ga], writes=[a2])
                S.op("pool", lambda e, a1=a1, a2=a2, ms=ms, cl=cl: e.tensor_tensor(
                    out=ms.ap[:, cl, :], in0=a1.ap, in1=a2.ap, op=ALU.add), reads=[a1, a2], writes=[ms])
            S.dma("sp", mrgT_d.ap[:, cg * 4:(cg + 1) * 4, tsl], ms.ap, reads=[ms], writes=[mrgT_d])
    A.release(mkE)
    if stop("pE1"):
        S.dma("sp", dbg_d.ap[0:1, 300:301], ones_f.ap[0:1, 0:1], reads=[ones_f, mrgT_d], final=True)
        return finish()

    def bcast_row(dst, col0, dt_tmp):
        for c in range(KC):
            dg = dt_tmp[c % 2]
            S.op("dve", lambda e, dg=dg, c=c: e.tensor_scalar(out=dg.ap, in0=ident_f.ap,
                                                               scalar1=modT.ap[:, col0 + c:col0 + c + 1], scalar2=None,
                                                               op0=ALU.mult), reads=[ident_f, modT], writes=[dg])
            ps = S.ps()
            S.op("pe", lambda e, ps=ps, dg=dg: e.matmul(ps.ap[:, 0:128], lhsT=ones_f.ap, rhs=dg.ap, start=True, stop=True),
                 reads=[ones_f, dg], writes=[ps])
            S.op("act", lambda e, ps=ps, c=c: e.activation(out=dst.ap[:, c * 128:(c + 1) * 128], in_=ps.ap[:, 0:128],
                                                            func=AF.Identity), reads=[ps], writes=[dst])

    mkE2 = A.mark()
    wo_sb = A.alloc("wo_sb", [128, KC, D], BF16)
    g1_b = A.alloc("g1_b", [128, D], F32)
    dtmp = [A.alloc(f"dtmp{i}", [128, 128], F32) for i in range(2)]
    for q4 in range(4):
        S.dma("pool", wo_sb.ap[:, :, q4 * 512:(q4 + 1) * 512], wview(IN.w_o, 0, D, q4 * 512, 512), writes=[wo_sb])
    bcast_row(g1_b, G1, dtmp)
    mrg_t = [A.alloc(f"mrg_t{i}", [128, KC, 512], BF16) for i in range(2)]
    xt2 = [A.alloc(f"xt2_{i}", [128, D], F32) for i in range(2)]
    x1t = [A.alloc(f"x1t{i}", [128, D], F32) for i in range(2)]
    xnb = A.alloc("xnbE", [128, 4, D], BF16)
    junk = A.alloc("junkE", [128, D], BF16)
    stat = [A.alloc(f"statE{i}", [128, 4], F32) for i in range(4)]
    h2g = A.alloc("h2g", [128, KC, 512], BF16)
    pp = A.alloc("ppE", [128, 512], F32)

    def norm_tile(xt, ss, xnb, tt, junk):
        S.op("act", lambda e: e.activation(out=junk.ap, in_=xt.ap, func=AF.Square, accum_out=ss.ap[:, 0:1]),
             reads=[xt], writes=[junk, ss])
        S.op("act", lambda e: e.activation(out=ss.ap[:, 1:2], in_=ss.ap[:, 0:1], func=AF.Sqrt, scale=1.0 / D, bias=EPS),
             reads=[ss], writes=[ss])
        S.op("dve", lambda e: e.reciprocal(out=ss.ap[:, 2:3], in_=ss.ap[:, 1:2]), reads=[ss], writes=[ss])
        S.op("act", lambda e: e.activation(out=xnb.ap[:, tt, :], in_=xt.ap, func=AF.Identity, scale=ss.ap[:, 2:3]),
             reads=[xt, ss], writes=[xnb])

    def transpose_group(xnb, gs, sh_col, hT):
        for kc in range(KC):
            ps = S.ps()
            pb = ps.ap.bitcast(BF16)
            for tt in range(4):
                S.op("pe", lambda e, pb=pb, tt=tt, kc=kc: e.transpose(
                    out=pb[:, tt * 128:(tt + 1) * 128], in_=xnb.ap[:, tt, kc * 128:(kc + 1) * 128],
                    identity=ident_b.ap), reads=[xnb, ident_b], writes=[ps])
            S.op("dve", lambda e, pb=pb, kc=kc: e.tensor_scalar(
                out=hT.ap[:, kc, :], in0=pb[:, 0:512], scalar1=gs.ap[:, kc:kc + 1],
                scalar2=modT.ap[:, sh_col + kc:sh_col + kc + 1], op0=ALU.mult, op1=ALU.add),
                reads=[ps, gs, modT], writes=[hT])

    for tg in range(4):
        mt = mrg_t[tg % 2]
        S.dma("sp", mt.ap, mrgT_d.ap[:, :, tg * 512:(tg + 1) * 512], reads=[mrgT_d], writes=[mt])
        for tt in range(4):
            r0 = tg * 512 + tt * 128
            xt = xt2[tt % 2]
            x1 = x1t[tt % 2]
            S.dma("sp", xt.ap, IN.xo[r0:r0 + 128, :], writes=[xt])
            for cc in range(4):
                ps = S.ps()
                for kc in range(KC):
                    S.op("pe", lambda e, ps=ps, kc=kc, tt=tt, cc=cc, mt=mt: e.matmul(
                        ps.ap, lhsT=mt.ap[:, kc, tt * 128:(tt + 1) * 128], rhs=wo_sb.ap[:, kc, cc * 512:(cc + 1) * 512],
                        start=(kc == 0), stop=(kc == KC - 1)), reads=[mt, wo_sb], writes=[ps])
                S.op("dve", lambda e, ps=ps, cc=cc: e.tensor_tensor(out=pp.ap, in0=ps.ap, in1=g1_b.ap[:, cc * 512:(cc + 1) * 512],
                                                                    op=ALU.mult), reads=[ps, g1_b], writes=[pp])
                S.op("pool", lambda e, cc=cc, xt=xt, x1=x1: e.tensor_tensor(
                    out=x1.ap[:, cc * 512:(cc + 1) * 512], in0=pp.ap, in1=xt.ap[:, cc * 512:(cc + 1) * 512], op=ALU.add),
                    reads=[pp, xt], writes=[x1])
            S.dma("sp", x1_d.ap[r0:r0 + 128, :], x1.ap, reads=[x1], writes=[x1_d])
            norm_tile(x1, stat[tt], xnb, tt, junk)
        transpose_group(xnb, g2s, SH2, h2g)
        S.dma("sp", h2T_d.ap[:, :, tg * 512:(tg + 1) * 512], h2g.ap, reads=[h2g], writes=[h2T_d])
    A.release(mkE2)
    if stop("pE2"):
        S.dma("sp", dbg_d.ap[0:1, 300:301], ones_f.ap[0:1, 0:1], reads=[ones_f, x1_d, h2T_d], final=True)
        return finish()

    g2_b = A.alloc("g2_b", [128, D], BF16)
    mkg = A.mark()
    g2_f = A.alloc("g2_f", [128, D], F32)
    dtmp = [A.alloc(f"dtmpF{i}", [128, 128], F32) for i in range(2)]
    bcast_row(g2_f, G2, dtmp)
    S.op("dve", lambda e: e.tensor_copy(out=g2_b.ap, in_=g2_f.ap), reads=[g2_f], writes=[g2_b])
    A.release(mkg)
    wr_f = A.alloc("wr_f", [128, KC, 20], F32)
    wr_b = A.alloc("wr_b", [128, KC, 20], BF16)
    S.dma("sp", wr_f.ap, IN.w_rt.rearrange("(k p) n -> p k n", p=128), writes=[wr_f])
    S.op("dve", lambda e: e.tensor_copy(out=wr_b.ap, in_=wr_f.ap), reads=[wr_f], writes=[wr_b])
    h2h = A.alloc("h2h", [128, KC, 1024], BF16)
    accb = [A.alloc(f"accb{i}", [128, D], F32) for i in range(8)]
    acc = [[Tile(f"acc{t}_{c}", accb[t].ap[:, c * 512:(c + 1) * 512], "sb") for c in range(4)] for t in range(8)]
    ring = [A.alloc(f"ering{i}", [128, KC, 512], BF16) for i in range(4)]
    scr = A.alloc("scr", [128, D], F32)
    s1v = scr.ap.bitcast(BF16).rearrange("p (a b) -> p a b", a=8)[:, 0:8, 0:512] if False else None
    s1_full = scr.ap.bitcast(BF16)
    ATb = A.alloc("ATb", [128, 4, 1024], BF16)
    gts = A.alloc("gts", [128, 8, 16], F32)
    rw = A.alloc("rw", [128, 64], F32)
    pad8 = A.alloc("pad8", [128, 8], F32)
    fst = [A.alloc(f"fst{i}", [128, 4], F32) for i in range(2)]
    junkF = A.alloc("junkF", [128, 512], BF16)
    S.op("dve", lambda e: e.memset(pad8.ap, -BIG), writes=[pad8])

    def w2view(eidx, c0, ncols):
        return IN.w_ed[eidx][:, c0:c0 + ncols].rearrange("(k p) n -> p k n", p=128)

    for half in range(2):
        S.dma("sp", h2h.ap, h2T_d.ap[:, :, half * 1024:(half + 1) * 1024], reads=[h2T_d], writes=[h2h])
        for tt in range(8):
            r0 = half * 1024 + tt * 128
            S.dma("sp", accb[tt].ap, x1_d.ap[r0:r0 + 128, :], reads=[x1_d], writes=acc[tt])
        for tt in range(8):
            ps = S.ps()
            for kc in range(KC):
                S.op("pe", lambda e, ps=ps, kc=kc, tt=tt: e.matmul(
                    ps.ap[:, 0:20], lhsT=h2h.ap[:, kc, tt * 128:(tt + 1) * 128], rhs=wr_b.ap[:, kc, :],
                    start=(kc == 0), stop=(kc == KC - 1)), reads=[h2h, wr_b], writes=[ps])
            R_ = rw.ap
            dv = lambda fn, rd=(rw,), wr=(rw,): S.op("dve", fn, reads=list(rd), writes=list(wr))
            S.op("dve", lambda e, ps=ps: e.tensor_copy(out=R_[:, 0:20], in_=ps.ap[:, 0:20]), reads=[ps], writes=[rw])
            dv(lambda e: e.tensor_reduce(out=R_[:, 20:21], in_=R_[:, 0:4], axis=AX.X, op=ALU.max, negate=True))
            dv(lambda e: e.tensor_scalar(out=R_[:, 21:25], in0=R_[:, 0:4], scalar1=R_[:, 20:21], scalar2=0.0,
                                         op0=ALU.add, op1=ALU.is_ge))
            S.op("act", lambda e: e.activation(out=R_[:, 25:29], in_=R_[:, 0:4], func=AF.Exp, bias=R_[:, 20:21],
                                               accum_out=R_[:, 29:30]), reads=[rw], writes=[rw])
            dv(lambda e: e.reciprocal(out=R_[:, 30:31], in_=R_[:, 29:30]))
            dv(lambda e: e.tensor_scalar(out=R_[:, 31:35], in0=R_[:, 4:8], scalar1=R_[:, 21:22], scalar2=None, op0=ALU.mult))
            for g in range(1, 4):
                dv(lambda e, g=g: e.scalar_tensor_tensor(out=R_[:, 31:35], in0=R_[:, 4 + 4 * g:8 + 4 * g],
                                                         scalar=R_[:, 21 + g:22 + g], in1=R_[:, 31:35],
                                                         op0=ALU.mult, op1=ALU.add))
            S.op("dve", lambda e: e.tensor_copy(out=pad8.ap[:, 0:4], in_=R_[:, 31:35]), reads=[rw], writes=[pad8])
            S.op("dve", lambda e: e.max(out=R_[:, 35:43], in_=pad8.ap), reads=[pad8], writes=[rw])
            dv(lambda e: e.tensor_tensor(out=R_[:, 43:44], in0=R_[:, 36:37], in1=R_[:, 35:36], op=ALU.subtract))
            S.op("act", lambda e: e.activation(out=R_[:, 44:45], in_=R_[:, 43:44], func=AF.Exp), reads=[rw], writes=[rw])
            dv(lambda e: e.tensor_scalar(out=R_[:, 45:46], in0=R_[:, 44:45], scalar1=1.0, scalar2=None, op0=ALU.add))
            dv(lambda e: e.reciprocal(out=R_[:, 46:47], in_=R_[:, 45:46]))
            dv(lambda e: e.tensor_tensor(out=R_[:, 47:48], in0=R_[:, 44:45], in1=R_[:, 46:47], op=ALU.mult))
            dv(lambda e: e.tensor_scalar(out=R_[:, 48:52], in0=R_[:, 31:35], scalar1=R_[:, 35:36], scalar2=R_[:, 46:47],
                                         op0=ALU.is_ge, op1=ALU.mult))
            dv(lambda e: e.tensor_scalar(out=R_[:, 52:56], in0=R_[:, 31:35], scalar1=R_[:, 36:37], scalar2=R_[:, 47:48],
                                         op0=ALU.is_equal, op1=ALU.mult))
            dv(lambda e: e.tensor_tensor(out=R_[:, 56:60], in0=R_[:, 48:52], in1=R_[:, 52:56], op=ALU.add))
            dv(lambda e: e.tensor_scalar(out=R_[:, 56:60], in0=R_[:, 56:60], scalar1=R_[:, 30:31], scalar2=None, op0=ALU.mult))
            for g in range(4):
                S.op("dve", lambda e, g=g, tt=tt: e.tensor_scalar(out=gts.ap[:, tt, 4 * g:4 * g + 4], in0=R_[:, 56:60],
                                                                   scalar1=R_[:, 21 + g:22 + g], scalar2=None, op0=ALU.mult),
                     reads=[rw], writes=[gts])
        stage = 0

        def issue(st):
            eidx, kind = st // 3, st % 3
            sl = ring[st % 4]
            if kind == 0:
                S.dma("pool", sl.ap, IN.w_eg[eidx].rearrange("(k p) n -> p k n", p=128), writes=[sl])
            elif kind == 1:
                S.dma("pool", sl.ap, IN.w_eu[eidx].rearrange("(k p) n -> p k n", p=128), writes=[sl])
            else:
                v = sl.ap.rearrange("p k n -> p (k n)").rearrange("p (k n) -> p k n", k=4)
                for c2 in range(2):
                    S.dma("pool", v[:, :, c2 * 1024:(c2 + 1) * 1024], w2view(eidx, c2 * 1024, 1024), writes=[sl])
                for k4 in range(4):
                    S.op("pool", lambda e, v=v, k4=k4: e.tensor_tensor(
                        out=v[:, k4, :], in0=v[:, k4, :], in1=g2_b.ap, op=ALU.mult),
                        reads=[sl, g2_b], writes=[sl])

        NST = NEXP * 3
        for st in range(min(3, NST)):
            issue(st)
        for eidx in range(NEXP):
            for kind in range(3):
                st = eidx * 3 + kind
                if st + 3 < NST:
                    issue(st + 3)
                sl = ring[st % 4]
                if kind == 0:
                    for tg in range(2):
                        for fc in range(4):
                            ps = S.ps()
                            for kc in range(KC):
                                S.op("pe", lambda e, ps=ps, kc=kc, fc=fc, tg=tg, sl=sl: e.matmul(
                                    ps.ap, lhsT=sl.ap[:, kc, fc * 128:(fc + 1) * 128], rhs=h2h.ap[:, kc, tg * 512:(tg + 1) * 512],
                                    start=(kc == 0), stop=(kc == KC - 1)), reads=[sl, h2h], writes=[ps])
                            o0 = (tg * 4 + fc) * 512
                            S.op("act", lambda e, ps=ps, o0=o0: e.activation(out=s1_full[:, o0:o0 + 512], in_=ps.ap,
                                                                              func=AF.Silu), reads=[ps], writes=[scr])
                elif kind == 1:
                    for tg in range(2):
                        for fc in range(4):
                            ps = S.ps()
                            for kc in range(KC):
                                S.op("pe", lambda e, ps=ps, kc=kc, fc=fc, tg=tg, sl=sl: e.matmul(
                                    ps.ap, lhsT=sl.ap[:, kc, fc * 128:(fc + 1) * 128], rhs=h2h.ap[:, kc, tg * 512:(tg + 1) * 512],
                                    start=(kc == 0), stop=(kc == KC - 1)), reads=[sl, h2h], writes=[ps])
                            o0 = (tg * 4 + fc) * 512
                            S.op("dve", lambda e, ps=ps, o0=o0, fc=fc, tg=tg: e.tensor_tensor(
                                out=ATb.ap[:, fc, tg * 512:(tg + 1) * 512], in0=ps.ap, in1=s1_full[:, o0:o0 + 512], op=ALU.mult),
                                reads=[ps, scr], writes=[ATb])
                else:
                    v = sl.ap.rearrange("p k n -> p (k n)").rearrange("p (k n) -> p k n", k=4)
                    for tt in range(8):
                        for cc in range(4):
                            ps = S.ps()
                            for fc in range(4):
                                S.op("pe", lambda e, ps=ps, fc=fc, tt=tt, cc=cc, v=v: e.matmul(
                                    ps.ap, lhsT=ATb.ap[:, fc, tt * 128:(tt + 1) * 128], rhs=v[:, fc, cc * 512:(cc + 1) * 512],
                                    start=(fc == 0), stop=(fc == 3)), reads=[ATb, sl], writes=[ps])
                            a = acc[tt][cc]
                            S.op("dve", lambda e, ps=ps, a=a, tt=tt, eidx=eidx: e.scalar_tensor_tensor(
                                out=a.ap, in0=ps.ap, scalar=gts.ap[:, tt, eidx:eidx + 1], in1=a.ap, op0=ALU.mult, op1=ALU.add),
                                reads=[ps, gts, a], writes=[a])
        S.dma("sp", scr.ap, IN.fnw.partition_broadcast(128), writes=[scr])
        for tt in range(8):
            ss = fst[tt % 2]
            r0 = half * 1024 + tt * 128
            for cc in range(4):
                S.op("act", lambda e, tt=tt, cc=cc, ss=ss: e.activation(
                    out=junkF.ap, in_=acc[tt][cc].ap, func=AF.Square, accum_out=ss.ap[:, cc:cc + 1]),
                    reads=[acc[tt][cc]], writes=[junkF, ss])
            S.op("dve", lambda e, ss=ss: e.tensor_reduce(out=ss.ap[:, 0:1], in_=ss.ap[:, 0:4], axis=AX.X, op=ALU.add),
                 reads=[ss], writes=[ss])
            S.op("act", lambda e, ss=ss: e.activation(out=ss.ap[:, 1:2], in_=ss.ap[:, 0:1], func=AF.Sqrt, scale=1.0 / D,
                                                      bias=EPS), reads=[ss], writes=[ss])
            S.op("dve", lambda e, ss=ss: e.reciprocal(out=ss.ap[:, 2:3], in_=ss.ap[:, 1:2]), reads=[ss], writes=[ss])
            for cc in range(4):
                a = acc[tt][cc]
                S.op("dve", lambda e, a=a, cc=cc, ss=ss: e.scalar_tensor_tensor(
                    out=a.ap, in0=a.ap, scalar=ss.ap[:, 2:3], in1=scr.ap[:, cc * 512:(cc + 1) * 512],
                    op0=ALU.mult, op1=ALU.mult), reads=[a, ss, scr], writes=[a])
            S.dma("sp", out_d[r0:r0 + 128, :], accb[tt].ap, reads=acc[tt], semtile=accb[tt], final=True)
    return finish()


def host_prepare(inp):
    f = lambda a: np.ascontiguousarray(np.asarray(a, dtype=np.float32))
    x = f(inp["x"])
    shared = {
        "w_ada": f(inp["w_ada"][0]),
        "b_adaT": f(np.asarray(inp["b_ada"][0]).reshape(96, 128).T),
        "n1T": f(np.asarray(inp["norm1_w"][0]).reshape(KC, 128).T),
        "n2T": f(np.asarray(inp["norm2_w"][0]).reshape(KC, 128).T),
        "fnw": f(np.asarray(inp["final_norm_w"]).reshape(1, D)),
        "w_in": f(inp["w_in"][0]),
        "lnw": f(np.asarray(inp["sg_ln_w"][0]).reshape(1, D_SG)),
        "lnb": f(np.asarray(inp["sg_ln_b"][0]).reshape(1, D_SG)),
        "w_sp": f(inp["w_spatial"][0]),
        "b_spT": f(np.asarray(inp["b_spatial"][0]).T),
        "relb": f(inp["rel_bias"]),
        "w_osg": f(inp["w_out_sg"][0]),
        "w_oatt": f(inp["w_out_att"][0]),
        "w_o": f(inp["w_o"][0]),
        "w_rt": f(np.concatenate([np.asarray(inp["w_router_group"][0]), np.asarray(inp["w_router_expert"][0])], axis=1)),
        "w_eg": f(inp["w_exp_gate"][0]),
        "w_eu": f(inp["w_exp_up"][0]),
        "w_ed": f(inp["w_exp_down"][0]),
    }
    maps = []
    for core in range(NCORE):
        b, j = core // 4, core % 4
        m = dict(shared)
        m["xs"] = x[b]
        xb = x[b].reshape(NBLK, 256, D)
        m["xo"] = np.ascontiguousarray(xb[j::4].reshape(TOK, D))
        m["cT"] = f(np.asarray(inp["c"][b]).reshape(KC, 128).T)
        m.update(host_consts(j))
        maps.append(m)
    return maps


def assemble(outs):
    res = np.zeros((BATCH, SEQ, D), dtype=np.float32)
    for core in range(NCORE):
        b, j = core // 4, core % 4
        res[b].reshape(NBLK, 256, D)[j::4] = outs[core].reshape(8, 256, D)
    return res


_NC_CACHE = {}


def kernel(**inputs):
    if "nc" not in _NC_CACHE:
        _NC_CACHE["nc"] = build_program()
    nc = _NC_CACHE["nc"]
    maps = host_prepare(inputs)
    maps = [{k: m[k] for k in nc._declared_inputs} for m in maps]
    res = run_bass_kernel_spmd(nc, maps, core_ids=list(range(NCORE)))
    return assemble([np.asarray(r["out"]) for r in res.results])
```

```python
from contextlib import ExitStack
import numpy as np
import concourse.bass as bass
import concourse.mybir as mybir
from concourse.bass_utils import run_bass_kernel_spmd

F32 = mybir.dt.float32
BF16 = mybir.dt.bfloat16
I32 = mybir.dt.int32
ALU = mybir.AluOpType
AF = mybir.ActivationFunctionType
AX = mybir.AxisListType

ENGS = ("pe", "act", "dve", "pool", "sp")


class Tile:
    def __init__(self, name, ap, space):
        self.name = name
        self.ap = ap
        self.space = space
        self.writers = {}
        self.readers = {}
        self.dsem = None
        self.dcount = 0
        self.last_dma = None

    def __getitem__(self, k):
        return self.ap[k]


class Instr:
    __slots__ = ("eng", "fn", "deps", "sig", "sval", "is_dma", "dtile", "dval", "chan")

    def __init__(self, eng, fn):
        self.eng = eng
        self.fn = fn
        self.deps = []
        self.sig = False
        self.sval = 0
        self.is_dma = False
        self.dtile = None
        self.dval = 0
        self.chan = eng


class Arena:
    def __init__(self, sched, ap, words):
        self.S = sched
        self.ap = ap
        self.words = words
        self.top = 0
        self.grave = []
        self.live = {}

    def alloc(self, name, shape, dtype):
        assert shape[0] <= 128
        n = 1
        for s in shape[1:]:
            n *= s
        bpe = 2 if dtype == BF16 else 4
        w = (n * bpe + 3) // 4
        w = (w + 7) // 8 * 8
        lo = self.top
        hi = lo + w
        assert hi <= self.words, f"SBUF arena overflow allocating {name}: {hi} > {self.words}"
        self.top = hi
        v = self.ap[0:shape[0], lo:lo + (n * bpe + 3) // 4]
        if dtype != F32:
            v = v.bitcast(dtype)
        if len(shape) == 3:
            v = v.rearrange("p (a b) -> p a b", a=shape[1])
        elif len(shape) == 4:
            v = v.rearrange("p (a b c) -> p a b c", a=shape[1], b=shape[2])
        t = Tile(name, v, "sb")
        t.lo, t.hi = lo, hi
        for (glo, ghi, gw, gr) in self.grave:
            if glo < hi and lo < ghi:
                for k, i in gw.items():
                    _merge(t.readers, k, i)
                for k, i in gr.items():
                    _merge(t.readers, k, i)
        self.live[name] = t
        return t

    def mark(self):
        return self.top

    def release(self, mark):
        for name in list(self.live):
            t = self.live[name]
            if t.lo >= mark:
                self.grave.append((t.lo, t.hi, dict(t.writers), dict(t.readers)))
                del self.live[name]
        self.top = mark


def _merge(d, k, ins):
    old = d.get(k)
    if old is None or _order(ins) >= _order(old):
        d[k] = ins


_ctr = [0]


def _order(ins):
    return ins.sval


class Sched:
    def __init__(self, nc, arena_words=50688):
        self.nc = nc
        self.es = ExitStack()
        self.instrs = {e: [] for e in ENGS}
        self.n = 0
        self.final = []
        self.dsems = []
        arena_t = self.es.enter_context(nc.sbuf_tensor("arena", [128, arena_words], F32))
        self.arena = Arena(self, arena_t[:, :], arena_words)
        self.psum = []
        for i in range(8):
            p = self.es.enter_context(nc.psum_tensor(f"psb{i}", [128, 512], F32))
            self.psum.append(Tile(f"ps{i}", p[:, :], "ps"))
        self.ps_i = 0
        self.dram_tiles = {}

    def ps(self):
        pool = getattr(self, "ps_pool", None) or list(range(8))
        t = self.psum[pool[self.ps_i % len(pool)]]
        self.ps_i += 1
        return t

    def dram(self, name, shape, dtype, kind="Internal"):
        h = self.nc.dram_tensor(name, list(shape), dtype, kind=kind)
        t = Tile(name, h.ap(), "dram")
        self.dram_tiles[name] = t
        return t

    def _record(self, ins, reads, writes):
        self.n += 1
        ins.sval = self.n
        deps = {}
        for t in reads:
            for k, i in t.writers.items():
                deps[id(i)] = i
        for t in writes:
            for k, i in t.writers.items():
                deps[id(i)] = i
            for k, i in t.readers.items():
                deps[id(i)] = i
        for i in deps.values():
            if i is ins:
                continue
            if ins.eng == "pe" and i.eng == "pe" and not i.is_dma:
                continue
            ins.deps.append(i)
        for t in reads:
            t.readers[ins.chan] = ins
        for t in writes:
            t.writers[ins.chan] = ins
        self.instrs[ins.eng].append(ins)
        return ins

    def op(self, eng, fn, reads=(), writes=()):
        ins = Instr(eng, fn)
        return self._record(ins, list(reads), list(writes))

    def dma(self, q, out, in_, reads=(), writes=(), final=False, semtile=None, shared=False, **kw):
        reads = list(reads)
        writes = list(writes)
        if shared:
            if not hasattr(self, "shared_tile"):
                self.shared_tile = Tile("shared_small", None, "sb")
            semtile = self.shared_tile
        if semtile is None:
            for t in writes + reads:
                if t.space == "sb":
                    semtile = t
                    break
        assert semtile is not None
        if semtile.dsem is None:
            semtile.dsem = self.es.enter_context(self.nc.semaphore(f"d_{semtile.name}_{len(self.dsems)}"))
            self.dsems.append(semtile.dsem)
            self.dtiles = getattr(self, "dtiles", [])
            self.dtiles.append(semtile)
        ins = Instr(q, lambda e: e.dma_start(out=out, in_=in_, **kw))
        ins.is_dma = True
        ins.dtile = semtile
        semtile.dcount += 16
        ins.dval = semtile.dcount
        ins.chan = ("d", semtile.name)
        prev = semtile.last_dma
        self._record(ins, reads, writes)
        if prev is not None and all(d is not prev for d in ins.deps):
            ins.deps.append(prev)
        semtile.last_dma = ins
        if final:
            self.final.append(ins)
        return ins

    def emit(self):
        nc = self.nc
        fin = Instr("sp", lambda e: e.nop())
        fin.deps = list(self.final) + [t.last_dma for t in getattr(self, "dtiles", []) if t.last_dma is not None]
        self.n += 1
        fin.sval = self.n
        self.instrs["sp"].append(fin)
        for e in ENGS:
            for ins in self.instrs[e]:
                for d in ins.deps:
                    if not d.is_dma:
                        d.sig = True
        EPOCH = 30000
        esems = {}
        for e in ENGS:
            cnt = 0
            for ins in self.instrs[e]:
                if ins.is_dma:
                    continue
                if ins.sig:
                    cnt += 1
                    ins.sval = cnt
                else:
                    ins.sval = -1
            nsem = max(1, (cnt + EPOCH - 1) // EPOCH)
            esems[e] = [self.es.enter_context(nc.semaphore(f"e_{e}_{k}")) for k in range(nsem)]

        def sigof(d):
            if d.is_dma:
                return d.dtile.dsem, d.dval
            k = (d.sval - 1) // EPOCH
            return esems[d.eng][k], d.sval - k * EPOCH

        def body(ename):
            def run(eng):
                seen = {}
                for ins in self.instrs[ename]:
                    need = {}
                    for d in ins.deps:
                        sem, val = sigof(d)
                        key = id(sem)
                        if key not in need or need[key][1] < val:
                            need[key] = (sem, val)
                    for key, (sem, val) in need.items():
                        if seen.get(key, 0) >= val:
                            continue
                        eng.wait_ge(sem, val)
                        seen[key] = val
                    bi = ins.fn(eng)
                    if ins.is_dma:
                        bi.then_inc(ins.dtile.dsem, 16)
                    elif ins.sig:
                        sem, val = sigof(ins)
                        bi.then_inc(sem, 1)
            return run

        with nc.Block() as block:
            block.sync(body("sp"))
            block.scalar(body("act"))
            block.vector(body("dve"))
            block.gpsimd(body("pool"))
            block.tensor(body("pe"))
        self.es.close()


D = 2048
SEQ = 8192
BATCH = 2
NCORE = 8
NBLK = 32
TOK = 2048
KC = 16
D_SG = 1024
D_ATT = 1024
IN_COLS = 9216
C_U, C_V, C_Q, C_K, C_VV, C_GSG, C_GATT = 0, 1024, 2048, 3072, 4096, 5120, 7168
NEXP = 16
DEXP = 512
EPS = 1e-6
BIG = 30000.0
RLEN = 1792


def t5_bucket_np(n):
    n = np.asarray(n)
    nf = np.maximum(n, 16).astype(np.float32)
    large = 16 + (np.log(nf / np.float32(16)) / np.float32(np.log(8.0)) * np.float32(16)).astype(np.int32)
    large = np.minimum(large, 31)
    return np.where(n < 16, n, large)


def host_consts(j):
    c = {}
    c["ident"] = np.eye(128, dtype=np.float32)
    c["antiident"] = np.ascontiguousarray(np.eye(128, dtype=np.float32)[::-1])
    c["tril"] = np.tril(np.ones((128, 128), dtype=np.float32))
    i = np.arange(RLEN)
    d = 256 * j + 767 - i
    E = np.zeros((33, RLEN), dtype=np.float32)
    pos = d >= 0
    bk = t5_bucket_np(np.maximum(d, 0))
    E[bk[pos], i[pos]] = 1.0
    E[32, ~pos] = 1.0
    c["ebkt"] = E
    c["negbig8"] = np.full((1, 8), -BIG, dtype=np.float32)
    blk = np.zeros((3, 8, NBLK), dtype=np.float32)
    for s in range(8):
        own = 4 * s + j
        kb = np.arange(NBLK)
        blk[0, s] = np.where(kb < own, 0.0, -BIG)
        blk[1, s] = np.where(kb < own, -BIG, 0.0)
        blk[2, s] = np.where(kb < 4 * s - 2, 1.0, 0.0)
    c["blkc"] = blk.reshape(1, 3 * 8 * NBLK)
    return c


def build_program(stop_after=None, debug=False):
    nc = bass.Bass("TRN2", target_bir_lowering=False)
    S = Sched(nc)
    A = S.arena
    dbgset = set(debug) if debug else set()

    INSPEC = {
        "xs": [SEQ, D], "xo": [TOK, D], "cT": [128, KC], "w_ada": [D, 6 * D], "b_adaT": [128, 96],
        "n1T": [128, KC], "n2T": [128, KC], "fnw": [1, D], "w_in": [D, IN_COLS], "lnw": [1, D_SG],
        "lnb": [1, D_SG], "w_sp": [8, 128, 128], "b_spT": [128, 8], "relb": [32, 8], "w_osg": [D_SG, D],
        "w_oatt": [D_ATT, D], "w_o": [D, D], "w_rt": [D, 20], "w_eg": [NEXP, D, DEXP], "w_eu": [NEXP, D, DEXP],
        "w_ed": [NEXP, DEXP, D], "ident": [128, 128], "tril": [128, 128], "ebkt": [33, RLEN],
        "negbig8": [1, 8], "blkc": [1, 3 * 8 * NBLK], "antiident": [128, 128],
    }
    declared = {}

    class _In:
        def __getattr__(self, name):
            if name not in declared:
                declared[name] = nc.dram_tensor(name, list(INSPEC[name]), F32, kind="ExternalInput").ap()
            return declared[name]
    IN = _In()
    nc._declared_inputs = declared
    out_d = nc.dram_tensor("out", [TOK, D], F32, kind="ExternalOutput").ap()

    kT_d = S.dram("kT_d", [8, 128, SEQ], BF16, "ExternalOutput" if "kT_d" in dbgset else "Internal")
    v_d = S.dram("v_d", [SEQ, D_ATT], BF16, "ExternalOutput" if "v_d" in dbgset else "Internal")
    hT_d = S.dram("hT_d", [128, KC, TOK], BF16, "ExternalOutput" if "hT_d" in dbgset else "Internal")
    ysgT_d = S.dram("ysgT_d", [128, 8, TOK], BF16, "ExternalOutput" if "ysgT_d" in dbgset else "Internal")
    yattT_d = S.dram("yattT_d", [128, 8, TOK], BF16, "ExternalOutput" if "yattT_d" in dbgset else "Internal")
    gsg_d = S.dram("gsg_d", [128, KC, TOK], BF16, "ExternalOutput" if "gsg_d" in dbgset else "Internal")
    gatt_d = S.dram("gatt_d", [128, KC, TOK], BF16, "ExternalOutput" if "gatt_d" in dbgset else "Internal")
    mrgT_d = S.dram("mrgT_d", [128, KC, TOK], BF16, "ExternalOutput" if "mrgT_d" in dbgset else "Internal")
    x1_d = S.dram("x1_d", [TOK, D], F32, "ExternalOutput" if "x1_d" in dbgset else "Internal")
    h2T_d = S.dram("h2T_d", [128, KC, TOK], BF16, "ExternalOutput" if "h2T_d" in dbgset else "Internal")
    r_d = S.dram("r_d", [8, RLEN], F32, "ExternalOutput" if "r_d" in dbgset else "Internal")
    dbg_d = S.dram("dbg_d", [128, 4096], F32, "ExternalOutput") if debug else None

    def stop(name):
        return stop_after == name

    ident_f = A.alloc("ident_f", [128, 128], F32)
    ident_b = A.alloc("ident_b", [128, 128], BF16)
    ones_f = A.alloc("ones_f", [128, 128], F32)
    modT = A.alloc("modT", [128, 96], F32)
    g1s = A.alloc("g1s", [128, KC], F32)
    g2s = A.alloc("g2s", [128, KC], F32)
    ksum = A.alloc("ksum", [128, 8, NBLK], F32)
    S.dma("sp", ident_f.ap, IN.ident, writes=[ident_f], shared=True)
    S.dma("pool", ident_b.ap, IN.ident, writes=[ident_b])
    S.op("dve", lambda e: e.memset(ones_f.ap, 1.0), writes=[ones_f])
    S.op("dve", lambda e: e.memset(ksum.ap, 0.0), writes=[ksum])

    def wview(w2d, r0, nrows, c0, ncols):
        return w2d[r0:r0 + nrows, c0:c0 + ncols].rearrange("(k p) n -> p k n", p=128)

    def load_w(dst, w2d, r0, nrows, c0, ncols):
        S.dma("pool", dst.ap, wview(w2d, r0, nrows, c0, ncols), writes=[dst])

    mk0 = A.mark()
    c_sb = A.alloc("c_sb", [128, KC], F32)
    c_act = A.alloc("c_act", [128, KC], BF16)
    badaT = A.alloc("badaT", [128, 96], F32)
    n1T_sb = A.alloc("n1T_sb", [128, KC], F32)
    n2T_sb = A.alloc("n2T_sb", [128, KC], F32)
    S.dma("sp", c_sb.ap, IN.cT, writes=[c_sb], shared=True)
    S.dma("sp", badaT.ap, IN.b_adaT, writes=[badaT], shared=True)
    S.dma("sp", n1T_sb.ap, IN.n1T, writes=[n1T_sb], shared=True)
    S.dma("sp", n2T_sb.ap, IN.n2T, writes=[n2T_sb], shared=True)
    S.op("act", lambda e: e.activation(out=c_act.ap, in_=c_sb.ap, func=AF.Silu), reads=[c_sb], writes=[c_act])
    wada = [A.alloc(f"wada{i}", [128, KC, 512], BF16) for i in range(3)]
    NAD = 24
    for i in range(min(2, NAD)):
        load_w(wada[i % 3], IN.w_ada, 0, D, i * 512, 512)
    for i in range(NAD):
        if i + 2 < NAD:
            load_w(wada[(i + 2) % 3], IN.w_ada, 0, D, (i + 2) * 512, 512)
        wt = wada[i % 3]
        ps = S.ps()
        for mm in range(4):
            m = i * 4 + mm
            for kc in range(KC):
                S.op("pe", lambda e, ps=ps, wt=wt, mm=mm, kc=kc, m=m: e.matmul(
                    ps.ap[:, mm:mm + 1], lhsT=wt.ap[:, kc, mm * 128:(mm + 1) * 128], rhs=c_act.ap[:, kc:kc + 1],
                    start=(kc == 0), stop=(kc == KC - 1)), reads=[wt, c_act], writes=[ps])
        S.op("dve", lambda e, ps=ps, i=i: e.tensor_tensor(out=modT.ap[:, i * 4:i * 4 + 4], in0=ps.ap[:, 0:4],
                                                            in1=badaT.ap[:, i * 4:i * 4 + 4], op=ALU.add),
             reads=[ps, badaT], writes=[modT])
    S.op("dve", lambda e: e.scalar_tensor_tensor(out=g1s.ap, in0=modT.ap[:, 16:32], scalar=1.0, in1=n1T_sb.ap,
                                                 op0=ALU.add, op1=ALU.mult), reads=[modT, n1T_sb], writes=[g1s])
    S.op("dve", lambda e: e.scalar_tensor_tensor(out=g2s.ap, in0=modT.ap[:, 64:80], scalar=1.0, in1=n2T_sb.ap,
                                                 op0=ALU.add, op1=ALU.mult), reads=[modT, n2T_sb], writes=[g2s])
    SH1, G1, SH2, G2 = 0, 32, 48, 80
    A.release(mk0)

    def finish():
        S.emit()
        return nc

    if stop("p0"):
        S.dma("sp", dbg_d.ap[:, 0:96], modT.ap, reads=[modT], final=True)
        S.dma("sp", dbg_d.ap[:, 96:112], g1s.ap, reads=[g1s], final=True)
        return finish()

    def norm_group(x_src, row0, gs, sh_col, xst, xnb, junk, stat, hT):
        for tt in range(4):
            xt = xst[tt % len(xst)]
            S.dma("sp", xt.ap, x_src[row0 + tt * 128: row0 + (tt + 1) * 128, :], writes=[xt])
            ss = stat[tt]
            S.op("act", lambda e, xt=xt, ss=ss: e.activation(out=junk.ap, in_=xt.ap, func=AF.Square,
                                                               accum_out=ss.ap[:, 0:1]),
                 reads=[xt], writes=[junk, ss])
            S.op("act", lambda e, ss=ss: e.activation(out=ss.ap[:, 1:2], in_=ss.ap[:, 0:1], func=AF.Sqrt,
                                                      scale=1.0 / D, bias=EPS), reads=[ss], writes=[ss])
            S.op("dve", lambda e, ss=ss: e.reciprocal(out=ss.ap[:, 2:3], in_=ss.ap[:, 1:2]), reads=[ss], writes=[ss])
            S.op("act", lambda e, xt=xt, ss=ss, tt=tt: e.activation(out=xnb.ap[:, tt, :], in_=xt.ap, func=AF.Identity,
                                                                      scale=ss.ap[:, 2:3]),
                 reads=[xt, ss], writes=[xnb])
        for kc in range(KC):
            ps = S.ps()
            pb = ps.ap.bitcast(BF16)
            for tt in range(4):
                S.op("pe", lambda e, pb=pb, tt=tt, kc=kc: e.transpose(
                    out=pb[:, tt * 128:(tt + 1) * 128], in_=xnb.ap[:, tt, kc * 128:(kc + 1) * 128],
                    identity=ident_b.ap), reads=[xnb, ident_b], writes=[ps])
            S.op("dve", lambda e, pb=pb, kc=kc: e.tensor_scalar(
                out=hT.ap[:, kc, :], in0=pb[:, 0:512], scalar1=gs.ap[:, kc:kc + 1],
                scalar2=modT.ap[:, sh_col + kc:sh_col + kc + 1], op0=ALU.mult, op1=ALU.add),
                reads=[ps, gs, modT], writes=[hT])

    mkA = A.mark()
    wk = A.alloc("wk", [128, KC, 1024], BF16)
    wv = A.alloc("wv", [128, KC, 1024], BF16)
    for q4 in range(2):
        S.dma("pool", wk.ap[:, :, q4 * 512:(q4 + 1) * 512], wview(IN.w_in, 0, D, C_K + q4 * 512, 512), writes=[wk])
    for q4 in range(2):
        S.dma("pool", wv.ap[:, :, q4 * 512:(q4 + 1) * 512], wview(IN.w_in, 0, D, C_VV + q4 * 512, 512), writes=[wv])
    xst = [A.alloc(f"xst{i}", [128, D], F32) for i in range(3)]
    xnb = A.alloc("xnb", [128, 4, D], BF16)
    junk = A.alloc("junk", [128, D], BF16)
    stat = [A.alloc(f"stat{i}", [128, 4], F32) for i in range(4)]
    hTg = [A.alloc(f"hTg{i}", [128, KC, 512], BF16) for i in range(2)]
    kto = [A.alloc(f"kto{i}", [128, 8, 512], BF16) for i in range(2)]
    vo = [A.alloc(f"vo{i}", [128, 4, 1024], BF16) for i in range(2)]
    NGA = SEQ // 512
    if stop("pA_small"):
        NGA = 2
    for g in range(NGA):
        hT = hTg[g % 2]
        norm_group(IN.xs, g * 512, g1s, SH1, xst, xnb, junk, stat, hT)
        ko = kto[g % 2]
        for h in range(8):
            ps = S.ps()
            for kc in range(KC):
                S.op("pe", lambda e, ps=ps, h=h, kc=kc, hT=hT: e.matmul(
                    ps.ap, lhsT=wk.ap[:, kc, h * 128:(h + 1) * 128], rhs=hT.ap[:, kc, :],
                    start=(kc == 0), stop=(kc == KC - 1)), reads=[wk, hT], writes=[ps])
            S.op("act", lambda e, ps=ps, h=h, ko=ko: e.activation(out=ko.ap[:, h, :], in_=ps.ap, func=AF.Identity), reads=[ps], writes=[ko])
            S.op("dve", lambda e, ko=ko, h=h, g=g: e.tensor_reduce(
                out=ksum.ap[:, h, 2 * g:2 * g + 2], in_=ko.ap[:, h, :].rearrange("p (a b) -> p a b", a=2),
                axis=AX.X, op=ALU.add), reads=[ko], writes=[ksum])
        S.dma("sp", kT_d.ap[:, :, g * 512:(g + 1) * 512].rearrange("h d t -> d h t"), ko.ap,
              reads=[ko], writes=[kT_d])
        vt = vo[g % 2]
        for tt in range(4):
            for cc in range(2):
                ps = S.ps()
                for kc in range(KC):
                    S.op("pe", lambda e, ps=ps, tt=tt, cc=cc, kc=kc, hT=hT: e.matmul(
                        ps.ap, lhsT=hT.ap[:, kc, tt * 128:(tt + 1) * 128], rhs=wv.ap[:, kc, cc * 512:(cc + 1) * 512],
                        start=(kc == 0), stop=(kc == KC - 1)), reads=[wv, hT], writes=[ps])
                if cc == 0:
                    S.op("dve", lambda e, ps=ps, tt=tt, cc=cc, vt=vt: e.tensor_copy(
                        out=vt.ap[:, tt, cc * 512:(cc + 1) * 512], in_=ps.ap), reads=[ps], writes=[vt])
                else:
                    S.op("act", lambda e, ps=ps, tt=tt, cc=cc, vt=vt: e.activation(
                        out=vt.ap[:, tt, cc * 512:(cc + 1) * 512], in_=ps.ap, func=AF.Identity), reads=[ps], writes=[vt])
        S.dma("sp", v_d.ap[g * 512:(g + 1) * 512, :].rearrange("(t p) c -> p t c", p=128), vt.ap,
              reads=[vt], writes=[v_d])
    if stop("pA_small") or stop("pA"):
        S.dma("sp", dbg_d.ap[:, 0:256], ksum.ap.rearrange("p h n -> p (h n)"), reads=[ksum], final=True)
        S.dma("sp", dbg_d.ap[:, 512:608], modT.ap, reads=[modT], final=True)
        S.dma("sp", dbg_d.ap[:, 608:624], g1s.ap, reads=[g1s], final=True)
        fin = S.dma("sp", dbg_d.ap[0:1, 300:301], ones_f.ap[0:1, 0:1], reads=[ones_f, kT_d, v_d], final=True)
        return finish()
    A.release(mkA)
    QT = A.alloc("QT", [128, 8, TOK], BF16)
    mkC = A.mark()
    hTo = A.alloc("hTo", [128, KC, TOK], BF16)
    mkB = A.mark()
    xst = [A.alloc(f"xstB{i}", [128, D], F32) for i in range(2)]
    xnb = A.alloc("xnbB", [128, 4, D], BF16)
    junk = A.alloc("junkB", [128, D], BF16)
    stat = [A.alloc(f"statB{i}", [128, 4], F32) for i in range(4)]
    hTg1 = A.alloc("hTgB", [128, KC, 512], BF16)
    for g in range(4):
        norm_group(IN.xo, g * 512, g1s, SH1, xst, xnb, junk, stat, hTg1)
        S.op("pool", lambda e, g=g: e.tensor_copy(out=hTo.ap[:, :, g * 512:(g + 1) * 512], in_=hTg1.ap),
             reads=[hTg1], writes=[hTo])
    A.release(mkB)
    if stop("pB"):
        S.dma("sp", hT_d.ap, hTo.ap, reads=[hTo], writes=[hT_d], final=True)
        return finish()

    mk1 = A.mark()
    wsl = [A.alloc(f"wsl{i}", [128, KC, 512], BF16) for i in range(2)]
    gvz = A.alloc("gvz", [128, 16, D_SG], BF16)
    lnw_b = A.alloc("lnw_b", [128, D_SG], F32)
    lnb_b = A.alloc("lnb_b", [128, D_SG], F32)
    wmT = A.alloc("wmT", [128, 8, 128], BF16)
    bsT = A.alloc("bsT", [128, 8], F32)
    S.dma("sp", lnw_b.ap, IN.lnw.partition_broadcast(128), writes=[lnw_b], shared=True)
    S.dma("sp", lnb_b.ap, IN.lnb.partition_broadcast(128), writes=[lnb_b], shared=True)
    S.dma("sp", bsT.ap, IN.b_spT, writes=[bsT], shared=True)
    mkw = A.mark()
    tril_sb = A.alloc("tril_sb", [128, 128], F32)
    wsp_sb = A.alloc("wsp_sb", [128, 8, 128], F32)
    S.dma("sp", tril_sb.ap, IN.tril, writes=[tril_sb], shared=True)
    S.dma("sp", wsp_sb.ap, IN.w_sp.rearrange("g t s -> t g s"), writes=[wsp_sb], shared=True)
    for g in range(8):
        S.op("dve", lambda e, g=g: e.tensor_tensor(out=wsp_sb.ap[:, g, :], in0=wsp_sb.ap[:, g, :], in1=tril_sb.ap,
                                                   op=ALU.mult), reads=[wsp_sb, tril_sb], writes=[wsp_sb])
    for g2 in range(2):
        ps = S.ps()
        for gg in range(4):
            g = g2 * 4 + gg
            S.op("pe", lambda e, ps=ps, g=g, gg=gg: e.transpose(out=ps.ap[:, gg * 128:(gg + 1) * 128],
                                                                in_=wsp_sb.ap[:, g, :], identity=ident_f.ap),
                 reads=[wsp_sb, ident_f], writes=[ps])
        S.op("dve", lambda e, ps=ps, g2=g2: e.tensor_copy(
            out=wmT.ap[:, g2 * 4:(g2 + 1) * 4, :], in_=ps.ap.rearrange("p (a b) -> p a b", a=4)),
            reads=[ps], writes=[wmT])
    A.release(mkw)
    lstat = [A.alloc(f"lstat{i}", [128, 8], F32) for i in range(2)]
    vtmp = [A.alloc(f"vtmp{i}", [128, D_SG], F32) for i in range(1)]
    vnb = [A.alloc(f"vnb{i}", [128, D_SG], BF16) for i in range(2)]
    junk2 = A.alloc("junk2", [128, D_SG], BF16)
    gub = [A.alloc(f"gub{i}", [128, 512], F32) for i in range(2)]
    ysb = [A.alloc(f"ysb{i}", [128, 512], BF16) for i in range(2)]
    ysT = [A.alloc(f"ysT{i}", [128, 8, 128], BF16) for i in range(2)]

    def tm_linear(wt, tt, ps):
        for kc in range(KC):
            S.op("pe", lambda e, kc=kc: e.matmul(ps.ap, lhsT=hTo.ap[:, kc, tt * 128:(tt + 1) * 128],
                                                  rhs=wt.ap[:, kc, :], start=(kc == 0), stop=(kc == KC - 1)),
                 reads=[hTo, wt], writes=[ps])

    load_w(wsl[0], IN.w_in, 0, D, C_V, 512)
    load_w(wsl[1], IN.w_in, 0, D, C_V + 512, 512)
    for cc in range(2):
        for tt in range(16):
            ps = S.ps()
            tm_linear(wsl[cc], tt, ps)
            S.op("act", lambda e, ps=ps, tt=tt, cc=cc: e.activation(
                out=gvz.ap[:, tt, cc * 512:(cc + 1) * 512], in_=ps.ap, func=AF.Gelu), reads=[ps], writes=[gvz])
    load_w(wsl[0], IN.w_in, 0, D, C_U, 512)
    load_w(wsl[1], IN.w_in, 0, D, C_U + 512, 512)
    for tt in range(16):
        ls = lstat[tt % 2]
        vt = vtmp[0]
        vn = vnb[tt % 2]
        gv = gvz.ap[:, tt, :]
        S.op("act", lambda e, gv=gv, ls=ls: e.activation(out=junk2.ap, in_=gv, func=AF.Identity,
                                                          accum_out=ls.ap[:, 0:1]), reads=[gvz], writes=[junk2, ls])
        S.op("act", lambda e, gv=gv, ls=ls: e.activation(out=junk2.ap, in_=gv, func=AF.Square,
                                                          accum_out=ls.ap[:, 1:2]), reads=[gvz], writes=[junk2, ls])
        S.op("dve", lambda e, ls=ls: e.tensor_scalar(out=ls.ap[:, 2:3], in0=ls.ap[:, 0:1], scalar1=1.0 / D_SG,
                                                     scalar2=None, op0=ALU.mult), reads=[ls], writes=[ls])
        S.op("dve", lambda e, ls=ls: e.tensor_tensor(out=ls.ap[:, 3:4], in0=ls.ap[:, 2:3], in1=ls.ap[:, 2:3],
                                                     op=ALU.mult), reads=[ls], writes=[ls])
        S.op("dve", lambda e, ls=ls: e.scalar_tensor_tensor(out=ls.ap[:, 4:5], in0=ls.ap[:, 1:2], scalar=1.0 / D_SG,
                                                            in1=ls.ap[:, 3:4], op0=ALU.mult, op1=ALU.subtract),
             reads=[ls], writes=[ls])
        S.op("act", lambda e, ls=ls: e.activation(out=ls.ap[:, 5:6], in_=ls.ap[:, 4:5], func=AF.Sqrt, bias=EPS),
             reads=[ls], writes=[ls])
        S.op("dve", lambda e, ls=ls: e.reciprocal(out=ls.ap[:, 6:7], in_=ls.ap[:, 5:6]), reads=[ls], writes=[ls])
        S.op("dve", lambda e, gv=gv, ls=ls, vt=vt: e.tensor_scalar(
            out=vt.ap, in0=gv, scalar1=ls.ap[:, 2:3], scalar2=ls.ap[:, 6:7], op0=ALU.subtract, op1=ALU.mult),
            reads=[gvz, ls], writes=[vt])
        S.op("pool", lambda e, vt=vt: e.tensor_tensor(out=vt.ap, in0=vt.ap, in1=lnw_b.ap, op=ALU.mult),
             reads=[vt, lnw_b], writes=[vt])
        S.op("dve", lambda e, vt=vt, vn=vn: e.tensor_tensor(out=vn.ap, in0=vt.ap, in1=lnb_b.ap, op=ALU.add),
             reads=[vt, lnb_b], writes=[vn])
        for g2 in range(2):
            ps = S.ps()
            for gg in range(4):
                g = g2 * 4 + gg
                S.op("pe", lambda e, ps=ps, g=g, gg=gg, vn=vn: e.matmul(
                    ps.ap[:, gg * 128:(gg + 1) * 128], lhsT=wmT.ap[:, g, :], rhs=vn.ap[:, g * 128:(g + 1) * 128],
                    start=True, stop=True), reads=[wmT, vn], writes=[ps])
            for gg in range(4):
                g = g2 * 4 + gg
                S.op("act", lambda e, ps=ps, g=g, gg=gg, tt=tt: e.activation(
                    out=gvz.ap[:, tt, g * 128:(g + 1) * 128], in_=ps.ap[:, gg * 128:(gg + 1) * 128],
                    func=AF.Identity, bias=bsT.ap[:, g:g + 1]), reads=[ps, bsT], writes=[gvz])
    for tt in range(16):
        yT = ysT[tt % 2]
        for cc in range(2):
            ps = S.ps()
            tm_linear(wsl[cc], tt, ps)
            gu = gub[cc]
            ys = ysb[cc]
            S.op("act", lambda e, ps=ps, gu=gu: e.activation(out=gu.ap, in_=ps.ap, func=AF.Gelu),
                 reads=[ps], writes=[gu])
            S.op("dve", lambda e, gu=gu, ys=ys, tt=tt, cc=cc: e.tensor_tensor(
                out=ys.ap, in0=gu.ap, in1=gvz.ap[:, tt, cc * 512:(cc + 1) * 512], op=ALU.mult),
                reads=[gu, gvz], writes=[ys])
            ps2 = S.ps()
            pb = ps2.ap.bitcast(BF16)
            for q in range(4):
                S.op("pe", lambda e, pb=pb, q=q, ys=ys: e.transpose(
                    out=pb[:, q * 128:(q + 1) * 128], in_=ys.ap[:, q * 128:(q + 1) * 128], identity=ident_b.ap),
                    reads=[ys, ident_b], writes=[ps2])
            S.op("dve", lambda e, pb=pb, cc=cc, yT=yT: e.tensor_copy(
                out=yT.ap[:, cc * 4:(cc + 1) * 4, :], in_=pb[:, 0:512].rearrange("p (a b) -> p a b", a=4)),
                reads=[ps2], writes=[yT])
        S.dma("sp", ysgT_d.ap[:, :, tt * 128:(tt + 1) * 128], yT.ap, reads=[yT], writes=[ysgT_d])
    A.release(mk1)
    if stop("pC1"):
        S.dma("sp", dbg_d.ap[0:1, 300:301], ones_f.ap[0:1, 0:1], reads=[ones_f, ysgT_d], final=True)
        return finish()

    mk2 = A.mark()
    wsl = [A.alloc(f"wslq{i}", [128, KC, 512], BF16) for i in range(3)]
    gst = [A.alloc(f"gst{i}", [128, 4, TOK], BF16) for i in range(2)]
    cols = [C_Q, C_Q + 512] + [C_GSG + i * 512 for i in range(4)] + [C_GATT + i * 512 for i in range(4)]
    for i in range(2):
        load_w(wsl[i], IN.w_in, 0, D, cols[i], 512)
    for i, c0 in enumerate(cols):
        if i + 2 < len(cols):
            load_w(wsl[(i + 2) % 3], IN.w_in, 0, D, cols[i + 2], 512)
        wt = wsl[i % 3]
        stg = gst[i % 2]
        for cl in range(4):
            for tg in range(4):
                ps = S.ps()
                for kc in range(KC):
                    S.op("pe", lambda e, ps=ps, wt=wt, cl=cl, tg=tg, kc=kc: e.matmul(
                        ps.ap, lhsT=wt.ap[:, kc, cl * 128:(cl + 1) * 128], rhs=hTo.ap[:, kc, tg * 512:(tg + 1) * 512],
                        start=(kc == 0), stop=(kc == KC - 1)), reads=[wt, hTo], writes=[ps])
                if i < 2:
                    h = i * 4 + cl
                    S.op("act", lambda e, ps=ps, h=h, tg=tg: e.activation(
                        out=QT.ap[:, h, tg * 512:(tg + 1) * 512], in_=ps.ap, func=AF.Identity, scale=float(128 ** -0.5)),
                        reads=[ps], writes=[QT])
                else:
                    S.op("act", lambda e, ps=ps, cl=cl, tg=tg, stg=stg: e.activation(
                        out=stg.ap[:, cl, tg * 512:(tg + 1) * 512], in_=ps.ap, func=AF.Sigmoid),
                        reads=[ps], writes=[stg])
        if i >= 2:
            dst = gsg_d if i < 6 else gatt_d
            c4 = (i - 2) % 4
            S.dma("sp", dst.ap[:, c4 * 4:(c4 + 1) * 4, :], stg.ap, reads=[stg], writes=[dst])
    A.release(mkC)
    if stop("pC"):
        S.dma("sp", dbg_d.ap[0:1, 300:301], ones_f.ap[0:1, 0:1], reads=[ones_f, gsg_d, gatt_d], final=True)
        S.dma("sp", hT_d.ap[:, 0:8, :], QT.ap, reads=[QT], writes=[hT_d], final=True)
        return finish()
    mkD = A.mark()
    S.ps_pool = [0, 1, 2, 3, 4, 5]
    blk_b = A.alloc("blk_b", [128, 3, 8, NBLK], F32)
    b31_b = A.alloc("b31_b", [128, 8], F32)
    khi = A.alloc("khi", [128, 8, NBLK], BF16)
    S.dma("sp", blk_b.ap.rearrange("p a s n -> p (a s n)"), IN.blkc.partition_broadcast(128), writes=[blk_b], shared=True)
    S.dma("sp", b31_b.ap, IN.relb[31:32, :].partition_broadcast(128), writes=[b31_b], shared=True)
    S.op("dve", lambda e: e.tensor_copy(out=khi.ap, in_=ksum.ap), reads=[ksum], writes=[khi])
    mkr = A.mark()
    raug = A.alloc("raug", [33, 8], F32)
    e_sb = A.alloc("e_sb", [33, RLEN], F32)
    r_sb = A.alloc("r_sb", [8, RLEN], F32)
    S.dma("sp", raug.ap[0:32, :], IN.relb, writes=[raug], shared=True)
    S.dma("sp", raug.ap[32:33, :], IN.negbig8, writes=[raug], shared=True)
    S.dma("sp", e_sb.ap, IN.ebkt, writes=[e_sb], shared=True)
    for n0 in range(0, RLEN, 512):
        n = min(512, RLEN - n0)
        ps = S.ps()
        S.op("pe", lambda e, ps=ps, n0=n0, n=n: e.matmul(ps.ap[0:8, 0:n], lhsT=raug.ap, rhs=e_sb.ap[:, n0:n0 + n],
                                                         start=True, stop=True), reads=[raug, e_sb], writes=[ps])
        S.op("dve", lambda e, ps=ps, n0=n0, n=n: e.tensor_copy(out=r_sb.ap[:, n0:n0 + n], in_=ps.ap[0:8, 0:n]),
             reads=[ps], writes=[r_sb])
    S.dma("sp", r_d.ap, r_sb.ap, reads=[r_sb], writes=[r_d])
    A.release(mkr)
    KT1 = A.alloc("KT1", [128, SEQ], BF16)
    Vh = [A.alloc(f"Vh{i}", [128, 64, 128], BF16) for i in range(2)]
    Bh1 = A.alloc("Bh1", [128, 2, 1536], F32)
    Sb2 = [A.alloc(f"Sb{i}", [128, SEQ], F32) for i in range(2)]
    Pb = [A.alloc(f"Pb{i}", [128, 1024], BF16) for i in range(2)]
    PTb = [A.alloc(f"PTb{i}", [128, 8, 128], BF16) for i in range(2)]
    yTs = [A.alloc(f"yTs{i}", [128, TOK], BF16) for i in range(2)]
    ob = [A.alloc(f"ob{i}", [128, 128], BF16) for i in range(2)]
    sm = [A.alloc(f"sm{i}", [128, 160], F32) for i in range(2)]
    OPS = [S.psum[6], S.psum[7]]
    Brev1 = A.alloc("Brev1", [128, 1536], F32)
    antiI = A.alloc("antiI", [128, 128], F32)
    S.dma("sp", antiI.ap, IN.antiident, writes=[antiI], shared=True)

    def load_head(h):
        kt, vv, bb = KT1, Vh[h % 2], Bh1
        S.dma("sp", kt.ap, kT_d.ap[h], reads=[kT_d], writes=[kt])
        for hf in range(2):
            brv = Brev1
            src = bass.AP(r_d.ap.tensor, h * RLEN + 128 - 128 * hf, [[1, 128], [1, 1536]])
            S.dma("sp", brv.ap, src, reads=[r_d], writes=[brv])
            for wc in range(3):
                ps = S.ps()
                S.op("pe", lambda e, ps=ps, brv=brv, wc=wc: e.matmul(
                    ps.ap, lhsT=antiI.ap, rhs=brv.ap[:, wc * 512:(wc + 1) * 512], start=True, stop=True),
                    reads=[antiI, brv], writes=[ps])
                S.op("dve", lambda e, ps=ps, bb=bb, hf=hf, wc=wc: e.tensor_copy(
                    out=bb.ap[:, hf, wc * 512:(wc + 1) * 512], in_=ps.ap), reads=[ps], writes=[bb])
        for q4 in range(4):
            S.dma("sp", vv.ap[:, q4 * 16:(q4 + 1) * 16, :],
                  v_d.ap[q4 * 2048:(q4 + 1) * 2048, h * 128:(h + 1) * 128].rearrange("(n p) d -> p n d", p=128),
                  reads=[v_d], writes=[vv])

    pcs = [Tile(f"pcs{i}", None, "sb") for i in range(4)]
    wb = []
    npc = 0
    for eidx in range(NEXP):
        row = []
        for kind, (src, shp) in enumerate(((IN.w_eg, [D, DEXP]), (IN.w_eu, [D, DEXP]), (IN.w_ed, [DEXP, D]))):
            t = S.dram(f"wb_{eidx}_{kind}", shp, BF16)
            if kind < 2:
                sv = src[eidx].rearrange("(a b) n -> a (b n)", b=4)
                dv_ = t.ap.rearrange("(a b) n -> a (b n)", b=4)
            else:
                sv, dv_ = src[eidx], t.ap
            S.dma("pool", dv_, sv, writes=[t], semtile=pcs[npc % 4])
            npc += 1
            row.append(t)
        wb.append(row)

    iters = [(h, s, hf) for h in range(8) for s in range(8) for hf in range(2)]
    segc = [0]

    def stage1(idx):
        h, s, hf = iters[idx]
        if s == 0 and hf == 0:
            load_head(h)
        kt, bb = KT1, Bh1
        nblk = 4 * s + 4
        nch = 2 * s + 2
        t0 = s * 256 + hf * 128
        w = sm[idx % 2]
        Sb = Sb2[idx % 2]
        q_l = QT.ap[:, h, t0:t0 + 128]
        ps = S.ps()
        S.op("pe", lambda e, ps=ps: e.matmul(ps.ap[:, 0:NBLK], lhsT=q_l, rhs=khi.ap[:, h, :], start=True, stop=True),
             reads=[QT, khi], writes=[ps])
        S.op("dve", lambda e, ps=ps: e.tensor_tensor(out=w.ap[:, 0:32], in0=ps.ap[:, 0:NBLK], in1=blk_b.ap[:, 0, s, :],
                                                      op=ALU.add), reads=[ps, blk_b], writes=[w])
        S.op("dve", lambda e: e.max(out=w.ap[:, 32:40], in_=w.ap[:, 0:32]), reads=[w], writes=[w])
        S.op("dve", lambda e: e.tensor_scalar(out=w.ap[:, 40:72], in0=w.ap[:, 0:32], scalar1=w.ap[:, 34:35],
                                              scalar2=None, op0=ALU.is_lt), reads=[w], writes=[w])
        S.op("dve", lambda e: e.tensor_tensor(out=w.ap[:, 40:72], in0=w.ap[:, 40:72], in1=blk_b.ap[:, 1, s, :],
                                              op=ALU.mult), reads=[w, blk_b], writes=[w])
        S.op("dve", lambda e: e.scalar_tensor_tensor(
            out=w.ap[:, 40:72], in0=blk_b.ap[:, 2, s, :], scalar=b31_b.ap[:, h:h + 1], in1=w.ap[:, 40:72],
            op0=ALU.mult, op1=ALU.add), reads=[w, blk_b, b31_b], writes=[w])
        for c in range(nch):
            ps = S.ps()
            S.op("pe", lambda e, ps=ps, c=c: e.matmul(ps.ap, lhsT=q_l, rhs=kt.ap[:, c * 512:(c + 1) * 512],
                                                      start=True, stop=True), reads=[QT, kt], writes=[ps])
            wc = c - (2 * s - 1)
            if wc >= 0:
                S.op("dve", lambda e, ps=ps, c=c, wc=wc: e.tensor_tensor(
                    out=Sb.ap[:, c * 512:(c + 1) * 512], in0=ps.ap, in1=bb.ap[:, hf, wc * 512:(wc + 1) * 512],
                    op=ALU.add), reads=[ps, bb], writes=[Sb])
            elif c % 2 == 0:
                S.op("act", lambda e, ps=ps, c=c: e.activation(out=Sb.ap[:, c * 512:(c + 1) * 512], in_=ps.ap,
                                                                func=AF.Identity), reads=[ps], writes=[Sb])
            else:
                S.op("dve", lambda e, ps=ps, c=c: e.tensor_copy(out=Sb.ap[:, c * 512:(c + 1) * 512], in_=ps.ap),
                     reads=[ps], writes=[Sb])
        S.op("dve", lambda e: e.tensor_reduce(
            out=w.ap[:, 72:72 + nblk], in_=Sb.ap[:, 0:nblk * 256].rearrange("p (n k) -> p n k", k=256),
            axis=AX.X, op=ALU.max), reads=[Sb], writes=[w])
        S.op("dve", lambda e: e.tensor_tensor(out=w.ap[:, 72:72 + nblk], in0=w.ap[:, 72:72 + nblk],
                                              in1=w.ap[:, 40:40 + nblk], op=ALU.add), reads=[w], writes=[w])
        S.op("dve", lambda e: e.tensor_reduce(out=w.ap[:, 136:137], in_=w.ap[:, 72:72 + nblk],
                                              axis=AX.X, op=ALU.max, negate=True), reads=[w], writes=[w])
        S.op("dve", lambda e: e.tensor_scalar(out=w.ap[:, 72:72 + nblk], in0=w.ap[:, 40:40 + nblk],
                                              scalar1=w.ap[:, 136:137], scalar2=None, op0=ALU.add),
             reads=[w], writes=[w])

    def stage2(idx):
        h, s, hf = iters[idx]
        vv = Vh[h % 2]
        yT = yTs[h % 2]
        nblk = 4 * s + 4
        t0 = s * 256 + hf * 128
        w = sm[idx % 2]
        Sb = Sb2[idx % 2]
        ops = OPS[idx % 2]
        nseg = s + 1
        for sg in range(nseg):
            pbuf = Pb[segc[0] % 2]
            ptb = PTb[segc[0] % 2]
            segc[0] += 1
            for b4 in range(4):
                n = sg * 4 + b4
                S.op("act", lambda e, pbuf=pbuf, b4=b4, n=n: e.activation(
                    out=pbuf.ap[:, b4 * 256:(b4 + 1) * 256], in_=Sb.ap[:, n * 256:(n + 1) * 256], func=AF.Exp,
                    bias=w.ap[:, 72 + n:73 + n], accum_out=w.ap[:, 104 + n:105 + n]),
                    reads=[Sb, w], writes=[pbuf, w])
            ps = S.ps()
            pb = ps.ap.bitcast(BF16)
            for k8 in range(8):
                S.op("pe", lambda e, pb=pb, k8=k8, pbuf=pbuf: e.transpose(
                    out=pb[:, k8 * 128:(k8 + 1) * 128], in_=pbuf.ap[:, k8 * 128:(k8 + 1) * 128],
                    identity=ident_b.ap), reads=[pbuf, ident_b], writes=[ps])
            if sg % 2 == 0:
                S.op("dve", lambda e, pb=pb, ptb=ptb: e.tensor_copy(
                    out=ptb.ap, in_=pb.rearrange("p (a b) -> p a b", a=8)), reads=[ps], writes=[ptb])
            else:
                S.op("act", lambda e, pb=pb, ptb=ptb: e.activation(
                    out=ptb.ap, in_=pb.rearrange("p (a b) -> p a b", a=8), func=AF.Identity),
                    reads=[ps], writes=[ptb])
            for k8 in range(8):
                ktile = sg * 8 + k8
                S.op("pe", lambda e, ptb=ptb, k8=k8, ktile=ktile, sg=sg: e.matmul(
                    ops.ap[:, 0:128], lhsT=ptb.ap[:, k8, :], rhs=vv.ap[:, ktile, :],
                    start=(sg == 0 and k8 == 0), stop=(sg == nseg - 1 and k8 == 7)),
                    reads=[ptb, vv], writes=[ops])
        S.op("dve", lambda e: e.tensor_reduce(out=w.ap[:, 137:138], in_=w.ap[:, 104:104 + nblk],
                                              axis=AX.X, op=ALU.add), reads=[w], writes=[w])
        S.op("dve", lambda e: e.reciprocal(out=w.ap[:, 138:139], in_=w.ap[:, 137:138]), reads=[w], writes=[w])
        o_sb = ob[idx % 2]
        S.op("dve", lambda e: e.tensor_scalar(out=o_sb.ap, in0=ops.ap[:, 0:128], scalar1=w.ap[:, 138:139], scalar2=None,
                                              op0=ALU.mult), reads=[ops, w], writes=[o_sb])
        ps = S.ps()
        pb = ps.ap.bitcast(BF16)
        S.op("pe", lambda e, pb=pb: e.transpose(out=pb[:, 0:128], in_=o_sb.ap, identity=ident_b.ap),
             reads=[o_sb, ident_b], writes=[ps])
        S.op("act", lambda e, pb=pb: e.activation(out=yT.ap[:, t0:t0 + 128], in_=pb[:, 0:128], func=AF.Identity),
             reads=[ps], writes=[yT])
        if s == 7 and hf == 1:
            S.dma("sp", yattT_d.ap[:, h, :], yT.ap, reads=[yT], writes=[yattT_d])

    NIT = len(iters)
    stage1(0)
    for idx in range(NIT):
        if idx + 1 < NIT:
            stage1(idx + 1)
        stage2(idx)
    S.ps_pool = list(range(8))
    A.release(mkD)
    A.release(mkC - 0)
    if stop("pD"):
        S.dma("sp", dbg_d.ap[0:1, 300:301], ones_f.ap[0:1, 0:1], reads=[ones_f, yattT_d], final=True)
        return finish()
    A.release(0 + ksum.hi)
    mkE = A.mark()
    wos = [A.alloc(f"wos{i}", [128, 8, 512], BF16) for i in range(2)]
    woa = [A.alloc(f"woa{i}", [128, 8, 512], BF16) for i in range(2)]
    ysg_t = [A.alloc(f"ysg_t{i}", [128, 8, 512], BF16) for i in range(2)]
    yat_t = [A.alloc(f"yat_t{i}", [128, 8, 512], BF16) for i in range(2)]
    gs_t = [A.alloc(f"gs_t{i}", [128, 4, 512], BF16) for i in range(2)]
    ga_t = [A.alloc(f"ga_t{i}", [128, 4, 512], BF16) for i in range(2)]
    t1 = [A.alloc(f"t1_{i}", [128, 512], F32) for i in range(2)]
    t2 = [A.alloc(f"t2_{i}", [128, 512], F32) for i in range(2)]
    mst = [A.alloc(f"mst{i}", [128, 4, 512], BF16) for i in range(2)]
    load_w(wos[0], IN.w_osg, 0, D_SG, 0, 512)
    load_w(woa[0], IN.w_oatt, 0, D_ATT, 0, 512)
    k = 0
    for cg in range(4):
        if cg + 1 < 4:
            load_w(wos[(cg + 1) % 2], IN.w_osg, 0, D_SG, (cg + 1) * 512, 512)
            load_w(woa[(cg + 1) % 2], IN.w_oatt, 0, D_ATT, (cg + 1) * 512, 512)
        ws, wa = wos[cg % 2], woa[cg % 2]
        for tg in range(4):
            ys, ya, gs, ga, ms = ysg_t[k % 2], yat_t[k % 2], gs_t[k % 2], ga_t[k % 2], mst[k % 2]
            k += 1
            tsl = slice(tg * 512, (tg + 1) * 512)
            S.dma("sp", ys.ap, ysgT_d.ap[:, :, tsl], reads=[ysgT_d], writes=[ys])
            S.dma("sp", ya.ap, yattT_d.ap[:, :, tsl], reads=[yattT_d], writes=[ya])
            S.dma("sp", gs.ap, gsg_d.ap[:, cg * 4:(cg + 1) * 4, tsl], reads=[gsg_d], writes=[gs])
            S.dma("sp", ga.ap, gatt_d.ap[:, cg * 4:(cg + 1) * 4, tsl], reads=[gatt_d], writes=[ga])
            for cl in range(4):
                ps1 = S.ps()
                for kk in range(8):
                    S.op("pe", lambda e, ps1=ps1, kk=kk, cl=cl, ws=ws, ys=ys: e.matmul(
                        ps1.ap, lhsT=ws.ap[:, kk, cl * 128:(cl + 1) * 128], rhs=ys.ap[:, kk, :],
                        start=(kk == 0), stop=(kk == 7)), reads=[ws, ys], writes=[ps1])
                ps2 = S.ps()
                for kk in range(8):
                    S.op("pe", lambda e, ps2=ps2, kk=kk, cl=cl, wa=wa, ya=ya: e.matmul(
                        ps2.ap, lhsT=wa.ap[:, kk, cl * 128:(cl + 1) * 128], rhs=ya.ap[:, kk, :],
                        start=(kk == 0), stop=(kk == 7)), reads=[wa, ya], writes=[ps2])
                a1, a2 = t1[cl % 2], t2[cl % 2]
                S.op("dve", lambda e, ps1=ps1, a1=a1, gs=gs, cl=cl: e.tensor_tensor(
                    out=a1.ap, in0=ps1.ap, in1=gs.ap[:, cl, :], op=ALU.mult), reads=[ps1, gs], writes=[a1])
                S.op("dve", lambda e, ps2=ps2, a2=a2, ga=ga, cl=cl: e.tensor_tensor(
                    out=a2.ap, in0=ps2.ap, in1=ga.ap[:, cl, :], op=ALU.mult), reads=[ps2, ga], writes=[a2])
                S.op("pool", lambda e, a1=a1, a2=a2, ms=ms, cl=cl: e.tensor_tensor(
                    out=ms.ap[:, cl, :], in0=a1.ap, in1=a2.ap, op=ALU.add), reads=[a1, a2], writes=[ms])
            S.dma("sp", mrgT_d.ap[:, cg * 4:(cg + 1) * 4, tsl], ms.ap, reads=[ms], writes=[mrgT_d])
    A.release(mkE)
    if stop("pE1"):
        S.dma("sp", dbg_d.ap[0:1, 300:301], ones_f.ap[0:1, 0:1], reads=[ones_f, mrgT_d], final=True)
        return finish()

    def bcast_row(dst, col0, dt_tmp):
        for c in range(KC):
            dg = dt_tmp[c % 2]
            S.op("dve", lambda e, dg=dg, c=c: e.tensor_scalar(out=dg.ap, in0=ident_f.ap,
                                                               scalar1=modT.ap[:, col0 + c:col0 + c + 1], scalar2=None,
                                                               op0=ALU.mult), reads=[ident_f, modT], writes=[dg])
            ps = S.ps()
            S.op("pe", lambda e, ps=ps, dg=dg: e.matmul(ps.ap[:, 0:128], lhsT=ones_f.ap, rhs=dg.ap, start=True, stop=True),
                 reads=[ones_f, dg], writes=[ps])
            S.op("act", lambda e, ps=ps, c=c: e.activation(out=dst.ap[:, c * 128:(c + 1) * 128], in_=ps.ap[:, 0:128],
                                                            func=AF.Identity), reads=[ps], writes=[dst])

    mkE2 = A.mark()
    wo_sb = A.alloc("wo_sb", [128, KC, D], BF16)
    g1_b = A.alloc("g1_b", [128, D], F32)
    dtmp = [A.alloc(f"dtmp{i}", [128, 128], F32) for i in range(2)]
    for q4 in range(4):
        S.dma("pool", wo_sb.ap[:, :, q4 * 512:(q4 + 1) * 512], wview(IN.w_o, 0, D, q4 * 512, 512), writes=[wo_sb])
    bcast_row(g1_b, G1, dtmp)
    mrg_t = [A.alloc(f"mrg_t{i}", [128, KC, 512], BF16) for i in range(2)]
    xt2 = [A.alloc(f"xt2_{i}", [128, D], F32) for i in range(2)]
    x1t = [A.alloc(f"x1t{i}", [128, D], F32) for i in range(2)]
    xnb = A.alloc("xnbE", [128, 4, D], BF16)
    junk = A.alloc("junkE", [128, D], BF16)
    stat = [A.alloc(f"statE{i}", [128, 4], F32) for i in range(4)]
    h2g = A.alloc("h2g", [128, KC, 512], BF16)
    pp = A.alloc("ppE", [128, 512], F32)

    def norm_tile(xt, ss, xnb, tt, junk):
        S.op("act", lambda e: e.activation(out=junk.ap, in_=xt.ap, func=AF.Square, accum_out=ss.ap[:, 0:1]),
             reads=[xt], writes=[junk, ss])
        S.op("act", lambda e: e.activation(out=ss.ap[:, 1:2], in_=ss.ap[:, 0:1], func=AF.Sqrt, scale=1.0 / D, bias=EPS),
             reads=[ss], writes=[ss])
        S.op("dve", lambda e: e.reciprocal(out=ss.ap[:, 2:3], in_=ss.ap[:, 1:2]), reads=[ss], writes=[ss])
        S.op("act", lambda e: e.activation(out=xnb.ap[:, tt, :], in_=xt.ap, func=AF.Identity, scale=ss.ap[:, 2:3]),
             reads=[xt, ss], writes=[xnb])

    def transpose_group(xnb, gs, sh_col, hT):
        for kc in range(KC):
            ps = S.ps()
            pb = ps.ap.bitcast(BF16)
            for tt in range(4):
                S.op("pe", lambda e, pb=pb, tt=tt, kc=kc: e.transpose(
                    out=pb[:, tt * 128:(tt + 1) * 128], in_=xnb.ap[:, tt, kc * 128:(kc + 1) * 128],
                    identity=ident_b.ap), reads=[xnb, ident_b], writes=[ps])
            S.op("dve", lambda e, pb=pb, kc=kc: e.tensor_scalar(
                out=hT.ap[:, kc, :], in0=pb[:, 0:512], scalar1=gs.ap[:, kc:kc + 1],
                scalar2=modT.ap[:, sh_col + kc:sh_col + kc + 1], op0=ALU.mult, op1=ALU.add),
                reads=[ps, gs, modT], writes=[hT])

    for tg in range(4):
        mt = mrg_t[tg % 2]
        S.dma("sp", mt.ap, mrgT_d.ap[:, :, tg * 512:(tg + 1) * 512], reads=[mrgT_d], writes=[mt])
        for tt in range(4):
            r0 = tg * 512 + tt * 128
            xt = xt2[tt % 2]
            x1 = x1t[tt % 2]
            S.dma("sp", xt.ap, IN.xo[r0:r0 + 128, :], writes=[xt])
            for cc in range(4):
                ps = S.ps()
                for kc in range(KC):
                    S.op("pe", lambda e, ps=ps, kc=kc, tt=tt, cc=cc, mt=mt: e.matmul(
                        ps.ap, lhsT=mt.ap[:, kc, tt * 128:(tt + 1) * 128], rhs=wo_sb.ap[:, kc, cc * 512:(cc + 1) * 512],
                        start=(kc == 0), stop=(kc == KC - 1)), reads=[mt, wo_sb], writes=[ps])
                S.op("dve", lambda e, ps=ps, cc=cc: e.tensor_tensor(out=pp.ap, in0=ps.ap, in1=g1_b.ap[:, cc * 512:(cc + 1) * 512],
                                                                    op=ALU.mult), reads=[ps, g1_b], writes=[pp])
                S.op("pool", lambda e, cc=cc, xt=xt, x1=x1: e.tensor_tensor(
                    out=x1.ap[:, cc * 512:(cc + 1) * 512], in0=pp.ap, in1=xt.ap[:, cc * 512:(cc + 1) * 512], op=ALU.add),
                    reads=[pp, xt], writes=[x1])
            S.dma("sp", x1_d.ap[r0:r0 + 128, :], x1.ap, reads=[x1], writes=[x1_d])
            norm_tile(x1, stat[tt], xnb, tt, junk)
        transpose_group(xnb, g2s, SH2, h2g)
        S.dma("sp", h2T_d.ap[:, :, tg * 512:(tg + 1) * 512], h2g.ap, reads=[h2g], writes=[h2T_d])
    A.release(mkE2)
    if stop("pE2"):
        S.dma("sp", dbg_d.ap[0:1, 300:301], ones_f.ap[0:1, 0:1], reads=[ones_f, x1_d, h2T_d], final=True)
        return finish()

    g2_b = A.alloc("g2_b", [128, D], BF16)
    mkg = A.mark()
    g2_f = A.alloc("g2_f", [128, D], F32)
    dtmp = [A.alloc(f"dtmpF{i}", [128, 128], F32) for i in range(2)]
    bcast_row(g2_f, G2, dtmp)
    S.op("dve", lambda e: e.tensor_copy(out=g2_b.ap, in_=g2_f.ap), reads=[g2_f], writes=[g2_b])
    A.release(mkg)
    wr_f = A.alloc("wr_f", [128, KC, 20], F32)
    wr_b = A.alloc("wr_b", [128, KC, 20], BF16)
    S.dma("sp", wr_f.ap, IN.w_rt.rearrange("(k p) n -> p k n", p=128), writes=[wr_f], shared=True)
    S.op("dve", lambda e: e.tensor_copy(out=wr_b.ap, in_=wr_f.ap), reads=[wr_f], writes=[wr_b])
    h2h = A.alloc("h2h", [128, KC, 1024], BF16)
    accb = [A.alloc(f"accb{i}", [128, D], F32) for i in range(8)]
    acc = [[Tile(f"acc{t}_{c}", accb[t].ap[:, c * 512:(c + 1) * 512], "sb") for c in range(4)] for t in range(8)]
    ring = [A.alloc(f"ering{i}", [128, KC, 512], BF16) for i in range(4)]
    scr = A.alloc("scr", [128, D], F32)
    s1v = scr.ap.bitcast(BF16).rearrange("p (a b) -> p a b", a=8)[:, 0:8, 0:512] if False else None
    s1_full = scr.ap.bitcast(BF16)
    ATb = A.alloc("ATb", [128, 4, 1024], BF16)
    gts = A.alloc("gts", [128, 8, 16], F32)
    rw = A.alloc("rw", [128, 64], F32)
    pad8 = A.alloc("pad8", [128, 8], F32)
    fst = [A.alloc(f"fst{i}", [128, 4], F32) for i in range(2)]
    junkF = A.alloc("junkF", [128, 512], BF16)
    S.op("dve", lambda e: e.memset(pad8.ap, -BIG), writes=[pad8])

    def w2view(eidx, c0, ncols):
        return IN.w_ed[eidx][:, c0:c0 + ncols].rearrange("(k p) n -> p k n", p=128)

    for half in range(2):
        S.dma("sp", h2h.ap, h2T_d.ap[:, :, half * 1024:(half + 1) * 1024], reads=[h2T_d], writes=[h2h])
        for tt in range(8):
            r0 = half * 1024 + tt * 128
            S.dma("sp", accb[tt].ap, x1_d.ap[r0:r0 + 128, :], reads=[x1_d], writes=acc[tt])
        for tt in range(8):
            ps = S.ps()
            for kc in range(KC):
                S.op("pe", lambda e, ps=ps, kc=kc, tt=tt: e.matmul(
                    ps.ap[:, 0:20], lhsT=h2h.ap[:, kc, tt * 128:(tt + 1) * 128], rhs=wr_b.ap[:, kc, :],
                    start=(kc == 0), stop=(kc == KC - 1)), reads=[h2h, wr_b], writes=[ps])
            R_ = rw.ap
            dv = lambda fn, rd=(rw,), wr=(rw,): S.op("dve", fn, reads=list(rd), writes=list(wr))
            S.op("dve", lambda e, ps=ps: e.tensor_copy(out=R_[:, 0:20], in_=ps.ap[:, 0:20]), reads=[ps], writes=[rw])
            dv(lambda e: e.tensor_reduce(out=R_[:, 20:21], in_=R_[:, 0:4], axis=AX.X, op=ALU.max, negate=True))
            dv(lambda e: e.tensor_scalar(out=R_[:, 21:25], in0=R_[:, 0:4], scalar1=R_[:, 20:21], scalar2=0.0,
                                         op0=ALU.add, op1=ALU.is_ge))
            S.op("act", lambda e: e.activation(out=R_[:, 25:29], in_=R_[:, 0:4], func=AF.Exp, bias=R_[:, 20:21],
                                               accum_out=R_[:, 29:30]), reads=[rw], writes=[rw])
            dv(lambda e: e.reciprocal(out=R_[:, 30:31], in_=R_[:, 29:30]))
            dv(lambda e: e.tensor_scalar(out=R_[:, 31:35], in0=R_[:, 4:8], scalar1=R_[:, 21:22], scalar2=None, op0=ALU.mult))
            for g in range(1, 4):
                dv(lambda e, g=g: e.scalar_tensor_tensor(out=R_[:, 31:35], in0=R_[:, 4 + 4 * g:8 + 4 * g],
                                                         scalar=R_[:, 21 + g:22 + g], in1=R_[:, 31:35],
                                                         op0=ALU.mult, op1=ALU.add))
            S.op("dve", lambda e: e.tensor_copy(out=pad8.ap[:, 0:4], in_=R_[:, 31:35]), reads=[rw], writes=[pad8])
            S.op("dve", lambda e: e.max(out=R_[:, 35:43], in_=pad8.ap), reads=[pad8], writes=[rw])
            dv(lambda e: e.tensor_tensor(out=R_[:, 43:44], in0=R_[:, 36:37], in1=R_[:, 35:36], op=ALU.subtract))
            S.op("act", lambda e: e.activation(out=R_[:, 44:45], in_=R_[:, 43:44], func=AF.Exp), reads=[rw], writes=[rw])
            dv(lambda e: e.tensor_scalar(out=R_[:, 45:46], in0=R_[:, 44:45], scalar1=1.0, scalar2=None, op0=ALU.add))
            dv(lambda e: e.reciprocal(out=R_[:, 46:47], in_=R_[:, 45:46]))
            dv(lambda e: e.tensor_tensor(out=R_[:, 47:48], in0=R_[:, 44:45], in1=R_[:, 46:47], op=ALU.mult))
            dv(lambda e: e.tensor_scalar(out=R_[:, 48:52], in0=R_[:, 31:35], scalar1=R_[:, 35:36], scalar2=R_[:, 46:47],
                                         op0=ALU.is_ge, op1=ALU.mult))
            dv(lambda e: e.tensor_scalar(out=R_[:, 52:56], in0=R_[:, 31:35], scalar1=R_[:, 36:37], scalar2=R_[:, 47:48],
                                         op0=ALU.is_equal, op1=ALU.mult))
            dv(lambda e: e.tensor_tensor(out=R_[:, 56:60], in0=R_[:, 48:52], in1=R_[:, 52:56], op=ALU.add))
            dv(lambda e: e.tensor_scalar(out=R_[:, 56:60], in0=R_[:, 56:60], scalar1=R_[:, 30:31], scalar2=None, op0=ALU.mult))
            for g in range(4):
                S.op("dve", lambda e, g=g, tt=tt: e.tensor_scalar(out=gts.ap[:, tt, 4 * g:4 * g + 4], in0=R_[:, 56:60],
                                                                   scalar1=R_[:, 21 + g:22 + g], scalar2=None, op0=ALU.mult),
                     reads=[rw], writes=[gts])
        stage = 0

        def issue(st):
            eidx, kind = st // 3, st % 3
            sl = ring[st % 4]
            wt_ = wb[eidx][kind]
            if kind < 2:
                S.dma("sp", sl.ap, wt_.ap.rearrange("(k p) n -> p k n", p=128), reads=[wt_], writes=[sl])
            else:
                v = sl.ap.rearrange("p k n -> p (k n)").rearrange("p (k n) -> p k n", k=4)
                S.dma("sp", v, wt_.ap.rearrange("(k p) n -> p k n", p=128), reads=[wt_], writes=[sl])
                for k4 in range(4):
                    S.op("pool", lambda e, v=v, k4=k4: e.tensor_tensor(
                        out=v[:, k4, :], in0=v[:, k4, :], in1=g2_b.ap, op=ALU.mult),
                        reads=[sl, g2_b], writes=[sl])

        NST = NEXP * 3
        for st in range(min(3, NST)):
            issue(st)
        for eidx in range(NEXP):
            for kind in range(3):
                st = eidx * 3 + kind
                if st + 3 < NST:
                    issue(st + 3)
                sl = ring[st % 4]
                if kind == 0:
                    for tg in range(2):
                        for fc in range(4):
                            ps = S.ps()
                            for kc in range(KC):
                                S.op("pe", lambda e, ps=ps, kc=kc, fc=fc, tg=tg, sl=sl: e.matmul(
                                    ps.ap, lhsT=sl.ap[:, kc, fc * 128:(fc + 1) * 128], rhs=h2h.ap[:, kc, tg * 512:(tg + 1) * 512],
                                    start=(kc == 0), stop=(kc == KC - 1)), reads=[sl, h2h], writes=[ps])
                            o0 = (tg * 4 + fc) * 512
                            S.op("act", lambda e, ps=ps, o0=o0: e.activation(out=s1_full[:, o0:o0 + 512], in_=ps.ap,
                                                                              func=AF.Silu), reads=[ps], writes=[scr])
                elif kind == 1:
                    for tg in range(2):
                        for fc in range(4):
                            ps = S.ps()
                            for kc in range(KC):
                                S.op("pe", lambda e, ps=ps, kc=kc, fc=fc, tg=tg, sl=sl: e.matmul(
                                    ps.ap, lhsT=sl.ap[:, kc, fc * 128:(fc + 1) * 128], rhs=h2h.ap[:, kc, tg * 512:(tg + 1) * 512],
                                    start=(kc == 0), stop=(kc == KC - 1)), reads=[sl, h2h], writes=[ps])
                            o0 = (tg * 4 + fc) * 512
                            S.op("dve", lambda e, ps=ps, o0=o0, fc=fc, tg=tg: e.tensor_tensor(
                                out=ATb.ap[:, fc, tg * 512:(tg + 1) * 512], in0=ps.ap, in1=s1_full[:, o0:o0 + 512], op=ALU.mult),
                                reads=[ps, scr], writes=[ATb])
                else:
                    v = sl.ap.rearrange("p k n -> p (k n)").rearrange("p (k n) -> p k n", k=4)
                    for tt in range(8):
                        for cc in range(4):
                            ps = S.ps()
                            for fc in range(4):
                                S.op("pe", lambda e, ps=ps, fc=fc, tt=tt, cc=cc, v=v: e.matmul(
                                    ps.ap, lhsT=ATb.ap[:, fc, tt * 128:(tt + 1) * 128], rhs=v[:, fc, cc * 512:(cc + 1) * 512],
                                    start=(fc == 0), stop=(fc == 3)), reads=[ATb, sl], writes=[ps])
                            a = acc[tt][cc]
                            S.op("dve", lambda e, ps=ps, a=a, tt=tt, eidx=eidx: e.scalar_tensor_tensor(
                                out=a.ap, in0=ps.ap, scalar=gts.ap[:, tt, eidx:eidx + 1], in1=a.ap, op0=ALU.mult, op1=ALU.add),
                                reads=[ps, gts, a], writes=[a])
        S.dma("sp", scr.ap, IN.fnw.partition_broadcast(128), writes=[scr])
        for tt in range(8):
            ss = fst[tt % 2]
            r0 = half * 1024 + tt * 128
            for cc in range(4):
                S.op("act", lambda e, tt=tt, cc=cc, ss=ss: e.activation(
                    out=junkF.ap, in_=acc[tt][cc].ap, func=AF.Square, accum_out=ss.ap[:, cc:cc + 1]),
                    reads=[acc[tt][cc]], writes=[junkF, ss])
            S.op("dve", lambda e, ss=ss: e.tensor_reduce(out=ss.ap[:, 0:1], in_=ss.ap[:, 0:4], axis=AX.X, op=ALU.add),
                 reads=[ss], writes=[ss])
            S.op("act", lambda e, ss=ss: e.activation(out=ss.ap[:, 1:2], in_=ss.ap[:, 0:1], func=AF.Sqrt, scale=1.0 / D,
                                                      bias=EPS), reads=[ss], writes=[ss])
            S.op("dve", lambda e, ss=ss: e.reciprocal(out=ss.ap[:, 2:3], in_=ss.ap[:, 1:2]), reads=[ss], writes=[ss])
            for cc in range(4):
                a = acc[tt][cc]
                S.op("dve", lambda e, a=a, cc=cc, ss=ss: e.scalar_tensor_tensor(
                    out=a.ap, in0=a.ap, scalar=ss.ap[:, 2:3], in1=scr.ap[:, cc * 512:(cc + 1) * 512],
                    op0=ALU.mult, op1=ALU.mult), reads=[a, ss, scr], writes=[a])
            S.dma("sp", out_d[r0:r0 + 128, :], accb[tt].ap, reads=acc[tt], semtile=accb[tt], final=True)
    return finish()


def host_prepare(inp):
    f = lambda a: np.ascontiguousarray(np.asarray(a, dtype=np.float32))
    x = f(inp["x"])
    shared = {
        "w_ada": f(inp["w_ada"][0]),
        "b_adaT": f(np.asarray(inp["b_ada"][0]).reshape(96, 128).T),
        "n1T": f(np.asarray(inp["norm1_w"][0]).reshape(KC, 128).T),
        "n2T": f(np.asarray(inp["norm2_w"][0]).reshape(KC, 128).T),
        "fnw": f(np.asarray(inp["final_norm_w"]).reshape(1, D)),
        "w_in": f(inp["w_in"][0]),
        "lnw": f(np.asarray(inp["sg_ln_w"][0]).reshape(1, D_SG)),
        "lnb": f(np.asarray(inp["sg_ln_b"][0]).reshape(1, D_SG)),
        "w_sp": f(inp["w_spatial"][0]),
        "b_spT": f(np.asarray(inp["b_spatial"][0]).T),
        "relb": f(inp["rel_bias"]),
        "w_osg": f(inp["w_out_sg"][0]),
        "w_oatt": f(inp["w_out_att"][0]),
        "w_o": f(inp["w_o"][0]),
        "w_rt": f(np.concatenate([np.asarray(inp["w_router_group"][0]), np.asarray(inp["w_router_expert"][0])], axis=1)),
        "w_eg": f(inp["w_exp_gate"][0]),
        "w_eu": f(inp["w_exp_up"][0]),
        "w_ed": f(inp["w_exp_down"][0]),
    }
    maps = []
    for core in range(NCORE):
        b, j = core // 4, core % 4
        m = dict(shared)
        m["xs"] = x[b]
        xb = x[b].reshape(NBLK, 256, D)
        m["xo"] = np.ascontiguousarray(xb[j::4].reshape(TOK, D))
        m["cT"] = f(np.asarray(inp["c"][b]).reshape(KC, 128).T)
        m.update(host_consts(j))
        maps.append(m)
    return maps


def assemble(outs):
    res = np.zeros((BATCH, SEQ, D), dtype=np.float32)
    for core in range(NCORE):
        b, j = core // 4, core % 4
        res[b].reshape(NBLK, 256, D)[j::4] = outs[core].reshape(8, 256, D)
    return res


_NC_CACHE = {}


def kernel(**inputs):
    if "nc" not in _NC_CACHE:
        _NC_CACHE["nc"] = build_program()
    nc = _NC_CACHE["nc"]
    maps = host_prepare(inputs)
    maps = [{k: m[k] for k in nc._declared_inputs} for m in maps]
    res = run_bass_kernel_spmd(nc, maps, core_ids=list(range(NCORE)))
    return assemble([np.asarray(r["out"]) for r in res.results])
```

```python
from contextlib import ExitStack
import numpy as np
import concourse.bass as bass
import concourse.mybir as mybir
from concourse.bass_utils import run_bass_kernel_spmd

F32 = mybir.dt.float32
BF16 = mybir.dt.bfloat16
I32 = mybir.dt.int32
ALU = mybir.AluOpType
AF = mybir.ActivationFunctionType
AX = mybir.AxisListType

ENGS = ("pe", "act", "dve", "pool", "sp")


class Tile:
    def __init__(self, name, ap, space):
        self.name = name
        self.ap = ap
        self.space = space
        self.writers = {}
        self.readers = {}
        self.dsem = None
        self.dcount = 0
        self.last_dma = None

    def __getitem__(self, k):
        return self.ap[k]


class Instr:
    __slots__ = ("eng", "fn", "deps", "sig", "sval", "is_dma", "dtile", "dval", "chan")

    def __init__(self, eng, fn):
        self.eng = eng
        self.fn = fn
        self.deps = []
        self.sig = False
        self.sval = 0
        self.is_dma = False
        self.dtile = None
        self.dval = 0
        self.chan = eng


class Arena:
    def __init__(self, sched, ap, words):
        self.S = sched
        self.ap = ap
        self.words = words
        self.top = 0
        self.grave = []
        self.live = {}

    def alloc(self, name, shape, dtype):
        assert shape[0] <= 128
        n = 1
        for s in shape[1:]:
            n *= s
        bpe = 2 if dtype == BF16 else 4
        w = (n * bpe + 3) // 4
        w = (w + 7) // 8 * 8
        lo = self.top
        hi = lo + w
        assert hi <= self.words, f"SBUF arena overflow allocating {name}: {hi} > {self.words}"
        self.top = hi
        v = self.ap[0:shape[0], lo:lo + (n * bpe + 3) // 4]
        if dtype != F32:
            v = v.bitcast(dtype)
        if len(shape) == 3:
            v = v.rearrange("p (a b) -> p a b", a=shape[1])
        elif len(shape) == 4:
            v = v.rearrange("p (a b c) -> p a b c", a=shape[1], b=shape[2])
        t = Tile(name, v, "sb")
        t.lo, t.hi = lo, hi
        for (glo, ghi, gw, gr) in self.grave:
            if glo < hi and lo < ghi:
                for k, i in gw.items():
                    _merge(t.readers, k, i)
                for k, i in gr.items():
                    _merge(t.readers, k, i)
        self.live[name] = t
        return t

    def mark(self):
        return self.top

    def release(self, mark):
        for name in list(self.live):
            t = self.live[name]
            if t.lo >= mark:
                self.grave.append((t.lo, t.hi, dict(t.writers), dict(t.readers)))
                del self.live[name]
        self.top = mark


def subtile(parent, name, ap):
    t = Tile(name, ap, "sb")
    t.readers = dict(parent.readers)
    t.writers = dict(parent.writers)
    return t


def _merge(d, k, ins):
    old = d.get(k)
    if old is None or _order(ins) >= _order(old):
        d[k] = ins


_ctr = [0]


def _order(ins):
    return ins.sval


class Sched:
    def __init__(self, nc, arena_words=50688):
        self.nc = nc
        self.es = ExitStack()
        self.instrs = {e: [] for e in ENGS}
        self.n = 0
        self.final = []
        self.dsems = []
        arena_t = self.es.enter_context(nc.sbuf_tensor("arena", [128, arena_words], F32))
        self.arena = Arena(self, arena_t[:, :], arena_words)
        self.psum = []
        for i in range(8):
            p = self.es.enter_context(nc.psum_tensor(f"psb{i}", [128, 512], F32))
            self.psum.append(Tile(f"ps{i}", p[:, :], "ps"))
        self.ps_i = 0
        self.dram_tiles = {}

    def ps(self):
        pool = getattr(self, "ps_pool", None) or list(range(8))
        t = self.psum[pool[self.ps_i % len(pool)]]
        self.ps_i += 1
        return t

    def dram(self, name, shape, dtype, kind="Internal"):
        h = self.nc.dram_tensor(name, list(shape), dtype, kind=kind)
        t = Tile(name, h.ap(), "dram")
        self.dram_tiles[name] = t
        return t

    def _record(self, ins, reads, writes):
        self.n += 1
        ins.sval = self.n
        deps = {}
        for t in reads:
            for k, i in t.writers.items():
                deps[id(i)] = i
        for t in writes:
            for k, i in t.writers.items():
                deps[id(i)] = i
            for k, i in t.readers.items():
                deps[id(i)] = i
        for i in deps.values():
            if i is ins:
                continue
            if ins.eng == "pe" and i.eng == "pe" and not i.is_dma:
                continue
            ins.deps.append(i)
        for t in reads:
            t.readers[ins.chan] = ins
        for t in writes:
            t.writers[ins.chan] = ins
        self.instrs[ins.eng].append(ins)
        return ins

    def op(self, eng, fn, reads=(), writes=()):
        ins = Instr(eng, fn)
        return self._record(ins, list(reads), list(writes))

    def dma(self, q, out, in_, reads=(), writes=(), final=False, semtile=None, shared=False, **kw):
        reads = list(reads)
        writes = list(writes)
        if shared:
            if not hasattr(self, "shared_tile"):
                self.shared_tile = Tile("shared_small", None, "sb")
            semtile = self.shared_tile
        if semtile is None:
            for t in writes + reads:
                if t.space == "sb":
                    semtile = t
                    break
        assert semtile is not None
        if semtile.dsem is None:
            semtile.dsem = self.es.enter_context(self.nc.semaphore(f"d_{semtile.name}_{len(self.dsems)}"))
            self.dsems.append(semtile.dsem)
            self.dtiles = getattr(self, "dtiles", [])
            self.dtiles.append(semtile)
        ins = Instr(q, lambda e: e.dma_start(out=out, in_=in_, **kw))
        ins.is_dma = True
        ins.dtile = semtile
        semtile.dcount += 16
        ins.dval = semtile.dcount
        ins.chan = ("d", semtile.name)
        prev = semtile.last_dma
        self._record(ins, reads, writes)
        if prev is not None and all(d is not prev for d in ins.deps):
            ins.deps.append(prev)
        semtile.last_dma = ins
        if final:
            self.final.append(ins)
        return ins

    def emit(self):
        nc = self.nc
        fin = Instr("sp", lambda e: e.nop())
        fin.deps = list(self.final) + [t.last_dma for t in getattr(self, "dtiles", []) if t.last_dma is not None]
        self.n += 1
        fin.sval = self.n
        self.instrs["sp"].append(fin)
        for e in ENGS:
            for ins in self.instrs[e]:
                for d in ins.deps:
                    if not d.is_dma:
                        d.sig = True
        EPOCH = 30000
        esems = {}
        for e in ENGS:
            cnt = 0
            for ins in self.instrs[e]:
                if ins.is_dma:
                    continue
                if ins.sig:
                    cnt += 1
                    ins.sval = cnt
                else:
                    ins.sval = -1
            nsem = max(1, (cnt + EPOCH - 1) // EPOCH)
            esems[e] = [self.es.enter_context(nc.semaphore(f"e_{e}_{k}")) for k in range(nsem)]

        def sigof(d):
            if d.is_dma:
                return d.dtile.dsem, d.dval
            k = (d.sval - 1) // EPOCH
            return esems[d.eng][k], d.sval - k * EPOCH

        def body(ename):
            def run(eng):
                seen = {}
                for ins in self.instrs[ename]:
                    need = {}
                    for d in ins.deps:
                        sem, val = sigof(d)
                        key = id(sem)
                        if key not in need or need[key][1] < val:
                            need[key] = (sem, val)
                    for key, (sem, val) in need.items():
                        if seen.get(key, 0) >= val:
                            continue
                        eng.wait_ge(sem, val)
                        seen[key] = val
                    bi = ins.fn(eng)
                    if ins.is_dma:
                        bi.then_inc(ins.dtile.dsem, 16)
                    elif ins.sig:
                        sem, val = sigof(ins)
                        bi.then_inc(sem, 1)
            return run

        with nc.Block() as block:
            block.sync(body("sp"))
            block.scalar(body("act"))
            block.vector(body("dve"))
            block.gpsimd(body("pool"))
            block.tensor(body("pe"))
        self.es.close()


D = 2048
SEQ = 8192
BATCH = 2
NCORE = 8
NBLK = 32
TOK = 2048
KC = 16
D_SG = 1024
D_ATT = 1024
IN_COLS = 9216
C_U, C_V, C_Q, C_K, C_VV, C_GSG, C_GATT = 0, 1024, 2048, 3072, 4096, 5120, 7168
NEXP = 16
DEXP = 512
EPS = 1e-6
BIG = 30000.0
RLEN = 1792


def t5_bucket_np(n):
    n = np.asarray(n)
    nf = np.maximum(n, 16).astype(np.float32)
    large = 16 + (np.log(nf / np.float32(16)) / np.float32(np.log(8.0)) * np.float32(16)).astype(np.int32)
    large = np.minimum(large, 31)
    return np.where(n < 16, n, large)


def host_consts(j):
    c = {}
    c["ident"] = np.eye(128, dtype=np.float32)
    c["antiident"] = np.ascontiguousarray(np.eye(128, dtype=np.float32)[::-1])
    c["tril"] = np.tril(np.ones((128, 128), dtype=np.float32))
    i = np.arange(RLEN)
    d = 256 * j + 767 - i
    E = np.zeros((33, RLEN), dtype=np.float32)
    pos = d >= 0
    bk = t5_bucket_np(np.maximum(d, 0))
    E[bk[pos], i[pos]] = 1.0
    E[32, ~pos] = 1.0
    c["ebkt"] = E
    c["negbig8"] = np.full((1, 8), -BIG, dtype=np.float32)
    blk = np.zeros((3, 8, NBLK), dtype=np.float32)
    for s in range(8):
        own = 4 * s + j
        kb = np.arange(NBLK)
        blk[0, s] = np.where(kb < own, 0.0, -BIG)
        blk[1, s] = np.where(kb < own, -BIG, 0.0)
        blk[2, s] = np.where(kb < 4 * s - 2, 1.0, 0.0)
    c["blkc"] = blk.reshape(1, 3 * 8 * NBLK)
    return c


def build_program(stop_after=None, debug=False):
    nc = bass.Bass("TRN2", target_bir_lowering=False)
    S = Sched(nc)
    A = S.arena
    dbgset = set(debug) if debug else set()

    INSPEC = {
        "xs": [SEQ, D], "xo": [TOK, D], "cT": [128, KC], "w_ada": [D, 6 * D], "b_adaT": [128, 96],
        "n1T": [128, KC], "n2T": [128, KC], "fnw": [1, D], "w_in": [D, IN_COLS], "lnw": [1, D_SG],
        "lnb": [1, D_SG], "w_sp": [8, 128, 128], "b_spT": [128, 8], "relb": [32, 8], "w_osg": [D_SG, D],
        "w_oatt": [D_ATT, D], "w_o": [D, D], "w_rt": [D, 20], "w_eg": [NEXP, D, DEXP], "w_eu": [NEXP, D, DEXP],
        "w_ed": [NEXP, DEXP, D], "ident": [128, 128], "tril": [128, 128], "ebkt": [33, RLEN],
        "negbig8": [1, 8], "blkc": [1, 3 * 8 * NBLK], "antiident": [128, 128],
    }
    declared = {}

    class _In:
        def __getattr__(self, name):
            if name not in declared:
                declared[name] = nc.dram_tensor(name, list(INSPEC[name]), F32, kind="ExternalInput").ap()
            return declared[name]
    IN = _In()
    nc._declared_inputs = declared
    out_d = nc.dram_tensor("out", [TOK, D], F32, kind="ExternalOutput").ap()

    kT_d = S.dram("kT_d", [8, 128, SEQ], BF16, "ExternalOutput" if "kT_d" in dbgset else "Internal")
    v_d = S.dram("v_d", [SEQ, D_ATT], BF16, "ExternalOutput" if "v_d" in dbgset else "Internal")
    hT_d = S.dram("hT_d", [128, KC, TOK], BF16, "ExternalOutput" if "hT_d" in dbgset else "Internal")
    ysgT_d = S.dram("ysgT_d", [128, 8, TOK], BF16, "ExternalOutput" if "ysgT_d" in dbgset else "Internal")
    yattT_d = S.dram("yattT_d", [128, 8, TOK], BF16, "ExternalOutput" if "yattT_d" in dbgset else "Internal")
    gsg_d = S.dram("gsg_d", [128, KC, TOK], BF16, "ExternalOutput" if "gsg_d" in dbgset else "Internal")
    gatt_d = S.dram("gatt_d", [128, KC, TOK], BF16, "ExternalOutput" if "gatt_d" in dbgset else "Internal")
    mrgT_d = S.dram("mrgT_d", [128, KC, TOK], BF16, "ExternalOutput" if "mrgT_d" in dbgset else "Internal")
    x1_d = S.dram("x1_d", [TOK, D], F32, "ExternalOutput" if "x1_d" in dbgset else "Internal")
    h2T_d = S.dram("h2T_d", [128, KC, TOK], BF16, "ExternalOutput" if "h2T_d" in dbgset else "Internal")
    r_d = S.dram("r_d", [8, RLEN], F32, "ExternalOutput" if "r_d" in dbgset else "Internal")
    dbg_d = S.dram("dbg_d", [128, 4096], F32, "ExternalOutput") if debug else None

    def stop(name):
        return stop_after == name

    ident_f = A.alloc("ident_f", [128, 128], F32)
    ident_b = A.alloc("ident_b", [128, 128], BF16)
    ones_f = A.alloc("ones_f", [128, 128], F32)
    modT = A.alloc("modT", [128, 96], F32)
    g1s = A.alloc("g1s", [128, KC], F32)
    g2s = A.alloc("g2s", [128, KC], F32)
    ksum = A.alloc("ksum", [128, 8, NBLK], F32)
    S.dma("sp", ident_f.ap, IN.ident, writes=[ident_f], shared=True)
    S.dma("pool", ident_b.ap, IN.ident, writes=[ident_b])
    S.op("dve", lambda e: e.memset(ones_f.ap, 1.0), writes=[ones_f])
    S.op("dve", lambda e: e.memset(ksum.ap, 0.0), writes=[ksum])

    def wview(w2d, r0, nrows, c0, ncols):
        return w2d[r0:r0 + nrows, c0:c0 + ncols].rearrange("(k p) n -> p k n", p=128)

    def load_w(dst, w2d, r0, nrows, c0, ncols):
        S.dma("pool", dst.ap, wview(w2d, r0, nrows, c0, ncols), writes=[dst])

    mk0 = A.mark()
    c_sb = A.alloc("c_sb", [128, KC], F32)
    c_act = A.alloc("c_act", [128, KC], BF16)
    badaT = A.alloc("badaT", [128, 96], F32)
    n1T_sb = A.alloc("n1T_sb", [128, KC], F32)
    n2T_sb = A.alloc("n2T_sb", [128, KC], F32)
    S.dma("sp", c_sb.ap, IN.cT, writes=[c_sb], shared=True)
    S.dma("sp", badaT.ap, IN.b_adaT, writes=[badaT], shared=True)
    S.dma("sp", n1T_sb.ap, IN.n1T, writes=[n1T_sb], shared=True)
    S.dma("sp", n2T_sb.ap, IN.n2T, writes=[n2T_sb], shared=True)
    S.op("act", lambda e: e.activation(out=c_act.ap, in_=c_sb.ap, func=AF.Silu), reads=[c_sb], writes=[c_act])
    wada = [A.alloc(f"wada{i}", [128, KC, 512], BF16) for i in range(3)]
    NAD = 24
    for i in range(min(2, NAD)):
        load_w(wada[i % 3], IN.w_ada, 0, D, i * 512, 512)
    for i in range(NAD):
        if i + 2 < NAD:
            load_w(wada[(i + 2) % 3], IN.w_ada, 0, D, (i + 2) * 512, 512)
        wt = wada[i % 3]
        ps = S.ps()
        for mm in range(4):
            m = i * 4 + mm
            for kc in range(KC):
                S.op("pe", lambda e, ps=ps, wt=wt, mm=mm, kc=kc, m=m: e.matmul(
                    ps.ap[:, mm:mm + 1], lhsT=wt.ap[:, kc, mm * 128:(mm + 1) * 128], rhs=c_act.ap[:, kc:kc + 1],
                    start=(kc == 0), stop=(kc == KC - 1)), reads=[wt, c_act], writes=[ps])
        S.op("dve", lambda e, ps=ps, i=i: e.tensor_tensor(out=modT.ap[:, i * 4:i * 4 + 4], in0=ps.ap[:, 0:4],
                                                            in1=badaT.ap[:, i * 4:i * 4 + 4], op=ALU.add),
             reads=[ps, badaT], writes=[modT])
    S.op("dve", lambda e: e.scalar_tensor_tensor(out=g1s.ap, in0=modT.ap[:, 16:32], scalar=1.0, in1=n1T_sb.ap,
                                                 op0=ALU.add, op1=ALU.mult), reads=[modT, n1T_sb], writes=[g1s])
    S.op("dve", lambda e: e.scalar_tensor_tensor(out=g2s.ap, in0=modT.ap[:, 64:80], scalar=1.0, in1=n2T_sb.ap,
                                                 op0=ALU.add, op1=ALU.mult), reads=[modT, n2T_sb], writes=[g2s])
    SH1, G1, SH2, G2 = 0, 32, 48, 80
    A.release(mk0)

    def finish():
        S.emit()
        return nc

    if stop("p0"):
        S.dma("sp", dbg_d.ap[:, 0:96], modT.ap, reads=[modT], final=True)
        S.dma("sp", dbg_d.ap[:, 96:112], g1s.ap, reads=[g1s], final=True)
        return finish()

    def norm_partA(x_src, row0, xst, xnb, junk, stat):
        for tt in range(4):
            xt = xst[tt % len(xst)]
            S.dma("sp", xt.ap, x_src[row0 + tt * 128: row0 + (tt + 1) * 128, :], writes=[xt])
            ss = stat[tt]
            S.op("act", lambda e, xt=xt, ss=ss: e.activation(out=junk.ap, in_=xt.ap, func=AF.Square,
                                                               accum_out=ss.ap[:, 0:1]),
                 reads=[xt], writes=[junk, ss])
            S.op("act", lambda e, ss=ss: e.activation(out=ss.ap[:, 1:2], in_=ss.ap[:, 0:1], func=AF.Sqrt,
                                                      scale=1.0 / D, bias=EPS), reads=[ss], writes=[ss])
            S.op("dve", lambda e, ss=ss: e.reciprocal(out=ss.ap[:, 2:3], in_=ss.ap[:, 1:2]), reads=[ss], writes=[ss])
            S.op("act", lambda e, xt=xt, ss=ss, tt=tt: e.activation(out=xnb.ap[:, tt, :], in_=xt.ap, func=AF.Identity,
                                                                      scale=ss.ap[:, 2:3]),
                 reads=[xt, ss], writes=[xnb])

    def norm_partB(xnb, gs, sh_col, hT):
        for kc in range(KC):
            ps = S.ps()
            pb = ps.ap.bitcast(BF16)
            for tt in range(4):
                S.op("pe", lambda e, pb=pb, tt=tt, kc=kc: e.transpose(
                    out=pb[:, tt * 128:(tt + 1) * 128], in_=xnb.ap[:, tt, kc * 128:(kc + 1) * 128],
                    identity=ident_b.ap), reads=[xnb, ident_b], writes=[ps])
            S.op("dve", lambda e, pb=pb, kc=kc: e.tensor_scalar(
                out=hT.ap[:, kc, :], in0=pb[:, 0:512], scalar1=gs.ap[:, kc:kc + 1],
                scalar2=modT.ap[:, sh_col + kc:sh_col + kc + 1], op0=ALU.mult, op1=ALU.add),
                reads=[ps, gs, modT], writes=[hT])

    def norm_group(x_src, row0, gs, sh_col, xst, xnb, junk, stat, hT):
        norm_partA(x_src, row0, xst, xnb, junk, stat)
        norm_partB(xnb, gs, sh_col, hT)

    mkA = A.mark()
    wk = A.alloc("wk", [128, KC, 1024], BF16)
    wv = A.alloc("wv", [128, KC, 1024], BF16)
    for q4 in range(2):
        S.dma("pool", wk.ap[:, :, q4 * 512:(q4 + 1) * 512], wview(IN.w_in, 0, D, C_K + q4 * 512, 512), writes=[wk])
    for q4 in range(2):
        S.dma("pool", wv.ap[:, :, q4 * 512:(q4 + 1) * 512], wview(IN.w_in, 0, D, C_VV + q4 * 512, 512), writes=[wv])
    xst = [A.alloc(f"xst{i}", [128, D], F32) for i in range(2)]
    xnb = A.alloc("xnb", [128, 4, D], BF16)
    junk = A.alloc("junk", [128, D], BF16)
    stat = [A.alloc(f"stat{i}", [128, 4], F32) for i in range(4)]
    hTg = [A.alloc(f"hTg{i}", [128, KC, 512], BF16) for i in range(2)]
    kto = [A.alloc(f"kto{i}", [128, 8, 512], BF16) for i in range(2)]
    vo = [A.alloc(f"vo{i}", [128, 4, 1024], BF16) for i in range(2)]
    NGA = SEQ // 512
    if stop("pA_small"):
        NGA = 2
    xnbs = [xnb, A.alloc("xnb2", [128, 4, D], BF16)]
    norm_partA(IN.xs, 0, xst, xnbs[0], junk, stat)
    norm_partB(xnbs[0], g1s, SH1, hTg[0])
    for g in range(NGA):
        hT = hTg[g % 2]
        if g + 1 < NGA:
            norm_partA(IN.xs, (g + 1) * 512, xst, xnbs[(g + 1) % 2], junk, stat)
        ko = kto[g % 2]
        for h in range(8):
            ps = S.ps()
            for kc in range(KC):
                S.op("pe", lambda e, ps=ps, h=h, kc=kc, hT=hT: e.matmul(
                    ps.ap, lhsT=wk.ap[:, kc, h * 128:(h + 1) * 128], rhs=hT.ap[:, kc, :],
                    start=(kc == 0), stop=(kc == KC - 1)), reads=[wk, hT], writes=[ps])
            S.op("act", lambda e, ps=ps, h=h, ko=ko: e.activation(out=ko.ap[:, h, :], in_=ps.ap, func=AF.Identity), reads=[ps], writes=[ko])
            S.op("dve", lambda e, ko=ko, h=h, g=g: e.tensor_reduce(
                out=ksum.ap[:, h, 2 * g:2 * g + 2], in_=ko.ap[:, h, :].rearrange("p (a b) -> p a b", a=2),
                axis=AX.X, op=ALU.add), reads=[ko], writes=[ksum])
        S.dma("sp", kT_d.ap[:, :, g * 512:(g + 1) * 512].rearrange("h d t -> d h t"), ko.ap,
              reads=[ko], writes=[kT_d])
        vt = vo[g % 2]
        for tt in range(4):
            for cc in range(2):
                ps = S.ps()
                for kc in range(KC):
                    S.op("pe", lambda e, ps=ps, tt=tt, cc=cc, kc=kc, hT=hT: e.matmul(
                        ps.ap, lhsT=hT.ap[:, kc, tt * 128:(tt + 1) * 128], rhs=wv.ap[:, kc, cc * 512:(cc + 1) * 512],
                        start=(kc == 0), stop=(kc == KC - 1)), reads=[wv, hT], writes=[ps])
                if cc == 0:
                    S.op("dve", lambda e, ps=ps, tt=tt, cc=cc, vt=vt: e.tensor_copy(
                        out=vt.ap[:, tt, cc * 512:(cc + 1) * 512], in_=ps.ap), reads=[ps], writes=[vt])
                else:
                    S.op("act", lambda e, ps=ps, tt=tt, cc=cc, vt=vt: e.activation(
                        out=vt.ap[:, tt, cc * 512:(cc + 1) * 512], in_=ps.ap, func=AF.Identity), reads=[ps], writes=[vt])
        S.dma("sp", v_d.ap[g * 512:(g + 1) * 512, :].rearrange("(t p) c -> p t c", p=128), vt.ap,
              reads=[vt], writes=[v_d])
        if g + 1 < NGA:
            norm_partB(xnbs[(g + 1) % 2], g1s, SH1, hTg[(g + 1) % 2])
    if stop("pA_small") or stop("pA"):
        S.dma("sp", dbg_d.ap[:, 0:256], ksum.ap.rearrange("p h n -> p (h n)"), reads=[ksum], final=True)
        S.dma("sp", dbg_d.ap[:, 512:608], modT.ap, reads=[modT], final=True)
        S.dma("sp", dbg_d.ap[:, 608:624], g1s.ap, reads=[g1s], final=True)
        fin = S.dma("sp", dbg_d.ap[0:1, 300:301], ones_f.ap[0:1, 0:1], reads=[ones_f, kT_d, v_d], final=True)
        return finish()
    A.release(mkA)
    QT = A.alloc("QT", [128, 8, TOK], BF16)
    mkC = A.mark()
    hTo = A.alloc("hTo", [128, KC, TOK], BF16)
    mkB = A.mark()
    xst = [A.alloc(f"xstB{i}", [128, D], F32) for i in range(2)]
    xnb = A.alloc("xnbB", [128, 4, D], BF16)
    junk = A.alloc("junkB", [128, D], BF16)
    stat = [A.alloc(f"statB{i}", [128, 4], F32) for i in range(4)]
    hTg1 = A.alloc("hTgB", [128, KC, 512], BF16)
    for g in range(4):
        norm_group(IN.xo, g * 512, g1s, SH1, xst, xnb, junk, stat, hTg1)
        S.op("pool", lambda e, g=g: e.tensor_copy(out=hTo.ap[:, :, g * 512:(g + 1) * 512], in_=hTg1.ap),
             reads=[hTg1], writes=[hTo])
    A.release(mkB)
    if stop("pB"):
        S.dma("sp", hT_d.ap, hTo.ap, reads=[hTo], writes=[hT_d], final=True)
        return finish()

    mk1 = A.mark()
    wsl = [A.alloc(f"wsl{i}", [128, KC, 512], BF16) for i in range(2)]
    gvz = A.alloc("gvz", [128, 16, D_SG], BF16)
    lnw_b = A.alloc("lnw_b", [128, D_SG], F32)
    lnb_b = A.alloc("lnb_b", [128, D_SG], F32)
    wmT = A.alloc("wmT", [128, 8, 128], BF16)
    bsT = A.alloc("bsT", [128, 8], F32)
    S.dma("sp", lnw_b.ap, IN.lnw.partition_broadcast(128), writes=[lnw_b], shared=True)
    S.dma("sp", lnb_b.ap, IN.lnb.partition_broadcast(128), writes=[lnb_b], shared=True)
    S.dma("sp", bsT.ap, IN.b_spT, writes=[bsT], shared=True)
    mkw = A.mark()
    tril_sb = A.alloc("tril_sb", [128, 128], F32)
    wsp_sb = A.alloc("wsp_sb", [128, 8, 128], F32)
    S.dma("sp", tril_sb.ap, IN.tril, writes=[tril_sb], shared=True)
    S.dma("sp", wsp_sb.ap, IN.w_sp.rearrange("g t s -> t g s"), writes=[wsp_sb], shared=True)
    for g in range(8):
        S.op("dve", lambda e, g=g: e.tensor_tensor(out=wsp_sb.ap[:, g, :], in0=wsp_sb.ap[:, g, :], in1=tril_sb.ap,
                                                   op=ALU.mult), reads=[wsp_sb, tril_sb], writes=[wsp_sb])
    for g2 in range(2):
        ps = S.ps()
        for gg in range(4):
            g = g2 * 4 + gg
            S.op("pe", lambda e, ps=ps, g=g, gg=gg: e.transpose(out=ps.ap[:, gg * 128:(gg + 1) * 128],
                                                                in_=wsp_sb.ap[:, g, :], identity=ident_f.ap),
                 reads=[wsp_sb, ident_f], writes=[ps])
        S.op("dve", lambda e, ps=ps, g2=g2: e.tensor_copy(
            out=wmT.ap[:, g2 * 4:(g2 + 1) * 4, :], in_=ps.ap.rearrange("p (a b) -> p a b", a=4)),
            reads=[ps], writes=[wmT])
    A.release(mkw)
    lstat = [A.alloc(f"lstat{i}", [128, 8], F32) for i in range(2)]
    vtmp = [A.alloc(f"vtmp{i}", [128, D_SG], F32) for i in range(1)]
    vnb = [A.alloc(f"vnb{i}", [128, D_SG], BF16) for i in range(2)]
    junk2 = A.alloc("junk2", [128, D_SG], BF16)
    gub = [A.alloc(f"gub{i}", [128, 512], F32) for i in range(2)]
    ysb = [A.alloc(f"ysb{i}", [128, 512], BF16) for i in range(2)]
    ysT = [A.alloc(f"ysT{i}", [128, 8, 128], BF16) for i in range(2)]

    def tm_linear(wt, tt, ps):
        for kc in range(KC):
            S.op("pe", lambda e, kc=kc: e.matmul(ps.ap, lhsT=hTo.ap[:, kc, tt * 128:(tt + 1) * 128],
                                                  rhs=wt.ap[:, kc, :], start=(kc == 0), stop=(kc == KC - 1)),
                 reads=[hTo, wt], writes=[ps])

    load_w(wsl[0], IN.w_in, 0, D, C_V, 512)
    load_w(wsl[1], IN.w_in, 0, D, C_V + 512, 512)
    for cc in range(2):
        for tt in range(16):
            ps = S.ps()
            tm_linear(wsl[cc], tt, ps)
            S.op("act", lambda e, ps=ps, tt=tt, cc=cc: e.activation(
                out=gvz.ap[:, tt, cc * 512:(cc + 1) * 512], in_=ps.ap, func=AF.Gelu), reads=[ps], writes=[gvz])
    load_w(wsl[0], IN.w_in, 0, D, C_U, 512)
    load_w(wsl[1], IN.w_in, 0, D, C_U + 512, 512)
    for tt in range(16):
        ls = lstat[tt % 2]
        vt = vtmp[0]
        vn = vnb[tt % 2]
        gv = gvz.ap[:, tt, :]
        S.op("act", lambda e, gv=gv, ls=ls: e.activation(out=junk2.ap, in_=gv, func=AF.Identity,
                                                          accum_out=ls.ap[:, 0:1]), reads=[gvz], writes=[junk2, ls])
        S.op("act", lambda e, gv=gv, ls=ls: e.activation(out=junk2.ap, in_=gv, func=AF.Square,
                                                          accum_out=ls.ap[:, 1:2]), reads=[gvz], writes=[junk2, ls])
        S.op("dve", lambda e, ls=ls: e.tensor_scalar(out=ls.ap[:, 2:3], in0=ls.ap[:, 0:1], scalar1=1.0 / D_SG,
                                                     scalar2=None, op0=ALU.mult), reads=[ls], writes=[ls])
        S.op("dve", lambda e, ls=ls: e.tensor_tensor(out=ls.ap[:, 3:4], in0=ls.ap[:, 2:3], in1=ls.ap[:, 2:3],
                                                     op=ALU.mult), reads=[ls], writes=[ls])
        S.op("dve", lambda e, ls=ls: e.scalar_tensor_tensor(out=ls.ap[:, 4:5], in0=ls.ap[:, 1:2], scalar=1.0 / D_SG,
                                                            in1=ls.ap[:, 3:4], op0=ALU.mult, op1=ALU.subtract),
             reads=[ls], writes=[ls])
        S.op("act", lambda e, ls=ls: e.activation(out=ls.ap[:, 5:6], in_=ls.ap[:, 4:5], func=AF.Sqrt, bias=EPS),
             reads=[ls], writes=[ls])
        S.op("dve", lambda e, ls=ls: e.reciprocal(out=ls.ap[:, 6:7], in_=ls.ap[:, 5:6]), reads=[ls], writes=[ls])
        S.op("dve", lambda e, gv=gv, ls=ls, vt=vt: e.tensor_scalar(
            out=vt.ap, in0=gv, scalar1=ls.ap[:, 2:3], scalar2=ls.ap[:, 6:7], op0=ALU.subtract, op1=ALU.mult),
            reads=[gvz, ls], writes=[vt])
        S.op("pool", lambda e, vt=vt: e.tensor_tensor(out=vt.ap, in0=vt.ap, in1=lnw_b.ap, op=ALU.mult),
             reads=[vt, lnw_b], writes=[vt])
        S.op("dve", lambda e, vt=vt, vn=vn: e.tensor_tensor(out=vn.ap, in0=vt.ap, in1=lnb_b.ap, op=ALU.add),
             reads=[vt, lnb_b], writes=[vn])
        for g2 in range(2):
            ps = S.ps()
            for gg in range(4):
                g = g2 * 4 + gg
                S.op("pe", lambda e, ps=ps, g=g, gg=gg, vn=vn: e.matmul(
                    ps.ap[:, gg * 128:(gg + 1) * 128], lhsT=wmT.ap[:, g, :], rhs=vn.ap[:, g * 128:(g + 1) * 128],
                    start=True, stop=True), reads=[wmT, vn], writes=[ps])
            for gg in range(4):
                g = g2 * 4 + gg
                S.op("act", lambda e, ps=ps, g=g, gg=gg, tt=tt: e.activation(
                    out=gvz.ap[:, tt, g * 128:(g + 1) * 128], in_=ps.ap[:, gg * 128:(gg + 1) * 128],
                    func=AF.Identity, bias=bsT.ap[:, g:g + 1]), reads=[ps, bsT], writes=[gvz])
    for tt in range(16):
        yT = ysT[tt % 2]
        for cc in range(2):
            ps = S.ps()
            tm_linear(wsl[cc], tt, ps)
            gu = gub[cc]
            ys = ysb[cc]
            S.op("act", lambda e, ps=ps, gu=gu: e.activation(out=gu.ap, in_=ps.ap, func=AF.Gelu),
                 reads=[ps], writes=[gu])
            S.op("dve", lambda e, gu=gu, ys=ys, tt=tt, cc=cc: e.tensor_tensor(
                out=ys.ap, in0=gu.ap, in1=gvz.ap[:, tt, cc * 512:(cc + 1) * 512], op=ALU.mult),
                reads=[gu, gvz], writes=[ys])
            ps2 = S.ps()
            pb = ps2.ap.bitcast(BF16)
            for q in range(4):
                S.op("pe", lambda e, pb=pb, q=q, ys=ys: e.transpose(
                    out=pb[:, q * 128:(q + 1) * 128], in_=ys.ap[:, q * 128:(q + 1) * 128], identity=ident_b.ap),
                    reads=[ys, ident_b], writes=[ps2])
            S.op("dve", lambda e, pb=pb, cc=cc, yT=yT: e.tensor_copy(
                out=yT.ap[:, cc * 4:(cc + 1) * 4, :], in_=pb[:, 0:512].rearrange("p (a b) -> p a b", a=4)),
                reads=[ps2], writes=[yT])
        S.dma("sp", ysgT_d.ap[:, :, tt * 128:(tt + 1) * 128], yT.ap, reads=[yT], writes=[ysgT_d])
    A.release(mk1)
    if stop("pC1"):
        S.dma("sp", dbg_d.ap[0:1, 300:301], ones_f.ap[0:1, 0:1], reads=[ones_f, ysgT_d], final=True)
        return finish()

    mk2 = A.mark()
    wsl = [A.alloc(f"wslq{i}", [128, KC, 512], BF16) for i in range(3)]
    gst = [A.alloc(f"gst{i}", [128, 4, TOK], BF16) for i in range(2)]
    cols = [C_Q, C_Q + 512] + [C_GSG + i * 512 for i in range(4)] + [C_GATT + i * 512 for i in range(4)]
    for i in range(2):
        load_w(wsl[i], IN.w_in, 0, D, cols[i], 512)
    for i, c0 in enumerate(cols):
        if i + 2 < len(cols):
            load_w(wsl[(i + 2) % 3], IN.w_in, 0, D, cols[i + 2], 512)
        wt = wsl[i % 3]
        stg = gst[i % 2]
        for cl in range(4):
            for tg in range(4):
                ps = S.ps()
                for kc in range(KC):
                    S.op("pe", lambda e, ps=ps, wt=wt, cl=cl, tg=tg, kc=kc: e.matmul(
                        ps.ap, lhsT=wt.ap[:, kc, cl * 128:(cl + 1) * 128], rhs=hTo.ap[:, kc, tg * 512:(tg + 1) * 512],
                        start=(kc == 0), stop=(kc == KC - 1)), reads=[wt, hTo], writes=[ps])
                if i < 2:
                    h = i * 4 + cl
                    S.op("act", lambda e, ps=ps, h=h, tg=tg: e.activation(
                        out=QT.ap[:, h, tg * 512:(tg + 1) * 512], in_=ps.ap, func=AF.Identity, scale=float(128 ** -0.5)),
                        reads=[ps], writes=[QT])
                else:
                    S.op("act", lambda e, ps=ps, cl=cl, tg=tg, stg=stg: e.activation(
                        out=stg.ap[:, cl, tg * 512:(tg + 1) * 512], in_=ps.ap, func=AF.Sigmoid),
                        reads=[ps], writes=[stg])
        if i >= 2:
            dst = gsg_d if i < 6 else gatt_d
            c4 = (i - 2) % 4
            S.dma("sp", dst.ap[:, c4 * 4:(c4 + 1) * 4, :], stg.ap, reads=[stg], writes=[dst])
    A.release(mkC)
    if stop("pC"):
        S.dma("sp", dbg_d.ap[0:1, 300:301], ones_f.ap[0:1, 0:1], reads=[ones_f, gsg_d, gatt_d], final=True)
        S.dma("sp", hT_d.ap[:, 0:8, :], QT.ap, reads=[QT], writes=[hT_d], final=True)
        return finish()
    mkD = A.mark()
    S.ps_pool = [0, 1, 2, 3, 4, 5]
    blk_b = A.alloc("blk_b", [128, 3, 8, NBLK], F32)
    b31_b = A.alloc("b31_b", [128, 8], F32)
    khi = A.alloc("khi", [128, 8, NBLK], BF16)
    S.dma("sp", blk_b.ap.rearrange("p a s n -> p (a s n)"), IN.blkc.partition_broadcast(128), writes=[blk_b], shared=True)
    S.dma("sp", b31_b.ap, IN.relb[31:32, :].partition_broadcast(128), writes=[b31_b], shared=True)
    S.op("dve", lambda e: e.tensor_copy(out=khi.ap, in_=ksum.ap), reads=[ksum], writes=[khi])
    mkr = A.mark()
    raug = A.alloc("raug", [33, 8], F32)
    e_sb = A.alloc("e_sb", [33, RLEN], F32)
    r_sb = A.alloc("r_sb", [8, RLEN], F32)
    S.dma("sp", raug.ap[0:32, :], IN.relb, writes=[raug], shared=True)
    S.dma("sp", raug.ap[32:33, :], IN.negbig8, writes=[raug], shared=True)
    S.dma("sp", e_sb.ap, IN.ebkt, writes=[e_sb], shared=True)
    for n0 in range(0, RLEN, 512):
        n = min(512, RLEN - n0)
        ps = S.ps()
        S.op("pe", lambda e, ps=ps, n0=n0, n=n: e.matmul(ps.ap[0:8, 0:n], lhsT=raug.ap, rhs=e_sb.ap[:, n0:n0 + n],
                                                         start=True, stop=True), reads=[raug, e_sb], writes=[ps])
        S.op("dve", lambda e, ps=ps, n0=n0, n=n: e.tensor_copy(out=r_sb.ap[:, n0:n0 + n], in_=ps.ap[0:8, 0:n]),
             reads=[ps], writes=[r_sb])
    S.dma("sp", r_d.ap, r_sb.ap, reads=[r_sb], writes=[r_d])
    A.release(mkr)
    KT1 = A.alloc("KT1", [128, SEQ], BF16)
    Vh = [A.alloc(f"Vh{i}", [128, 64, 128], BF16) for i in range(2)]
    Bh1 = A.alloc("Bh1", [128, 2, 1536], F32)
    Sb2 = [A.alloc(f"Sb{i}", [128, SEQ], F32) for i in range(2)]
    Pb = [A.alloc(f"Pb{i}", [128, 1024], BF16) for i in range(2)]
    Pq = [[subtile(Pb[i], f"Pq{i}_{q}", Pb[i].ap[:, q * 256:(q + 1) * 256]) for q in range(4)] for i in range(2)]
    PTb = [A.alloc(f"PTb{i}", [128, 8, 128], BF16) for i in range(2)]
    yTs = [A.alloc(f"yTs{i}", [128, TOK], BF16) for i in range(2)]
    ob = [A.alloc(f"ob{i}", [128, 128], BF16) for i in range(2)]
    sm = [A.alloc(f"sm{i}", [128, 160], F32) for i in range(2)]
    rsb = [A.alloc(f"rsb{i}", [128, 32], F32) for i in range(2)]
    rsc = [[subtile(rsb[i], f"rsc{i}_{n}", rsb[i].ap[:, n:n + 1]) for n in range(32)] for i in range(2)]
    OPS = [S.psum[6], S.psum[7]]
    Brev1 = A.alloc("Brev1", [128, 1536], F32)
    antiI = A.alloc("antiI", [128, 128], F32)
    S.dma("sp", antiI.ap, IN.antiident, writes=[antiI], shared=True)

    def load_head(h):
        kt, vv, bb = KT1, Vh[h % 2], Bh1
        S.dma("sp", kt.ap, kT_d.ap[h], reads=[kT_d], writes=[kt])
        for hf in range(2):
            brv = Brev1
            src = bass.AP(r_d.ap.tensor, h * RLEN + 128 - 128 * hf, [[1, 128], [1, 1536]])
            S.dma("sp", brv.ap, src, reads=[r_d], writes=[brv])
            for wc in range(3):
                ps = S.ps()
                S.op("pe", lambda e, ps=ps, brv=brv, wc=wc: e.matmul(
                    ps.ap, lhsT=antiI.ap, rhs=brv.ap[:, wc * 512:(wc + 1) * 512], start=True, stop=True),
                    reads=[antiI, brv], writes=[ps])
                S.op("dve", lambda e, ps=ps, bb=bb, hf=hf, wc=wc: e.tensor_copy(
                    out=bb.ap[:, hf, wc * 512:(wc + 1) * 512], in_=ps.ap), reads=[ps], writes=[bb])
        for q4 in range(4):
            S.dma("sp", vv.ap[:, q4 * 16:(q4 + 1) * 16, :],
                  v_d.ap[q4 * 2048:(q4 + 1) * 2048, h * 128:(h + 1) * 128].rearrange("(n p) d -> p n d", p=128),
                  reads=[v_d], writes=[vv])

    pcs = [Tile(f"pcs{i}", None, "sb") for i in range(4)]
    wb = []
    npc = 0
    for eidx in range(NEXP):
        row = []
        for kind, (src, shp) in enumerate(((IN.w_eg, [D, DEXP]), (IN.w_eu, [D, DEXP]), (IN.w_ed, [DEXP, D]))):
            t = S.dram(f"wb_{eidx}_{kind}", shp, BF16)
            if kind < 2:
                sv = src[eidx].rearrange("(a b) n -> a (b n)", b=4)
                dv_ = t.ap.rearrange("(a b) n -> a (b n)", b=4)
            else:
                sv, dv_ = src[eidx], t.ap
            S.dma("pool", dv_, sv, writes=[t], semtile=pcs[npc % 4])
            npc += 1
            row.append(t)
        wb.append(row)

    iters = [(h, s, hf) for h in range(8) for s in range(8) for hf in range(2)]
    segc = [0]

    def stage1(idx):
        h, s, hf = iters[idx]
        if s == 0 and hf == 0:
            load_head(h)
        kt, bb = KT1, Bh1
        nblk = 4 * s + 4
        nch = 2 * s + 2
        t0 = s * 256 + hf * 128
        w = sm[idx % 2]
        Sb = Sb2[idx % 2]
        q_l = QT.ap[:, h, t0:t0 + 128]
        ps = S.ps()
        S.op("pe", lambda e, ps=ps: e.matmul(ps.ap[:, 0:NBLK], lhsT=q_l, rhs=khi.ap[:, h, :], start=True, stop=True),
             reads=[QT, khi], writes=[ps])
        S.op("dve", lambda e, ps=ps: e.tensor_tensor(out=w.ap[:, 0:32], in0=ps.ap[:, 0:NBLK], in1=blk_b.ap[:, 0, s, :],
                                                      op=ALU.add), reads=[ps, blk_b], writes=[w])
        S.op("dve", lambda e: e.max(out=w.ap[:, 32:40], in_=w.ap[:, 0:32]), reads=[w], writes=[w])
        S.op("dve", lambda e: e.tensor_scalar(out=w.ap[:, 40:72], in0=w.ap[:, 0:32], scalar1=w.ap[:, 34:35],
                                              scalar2=None, op0=ALU.is_lt), reads=[w], writes=[w])
        S.op("dve", lambda e: e.tensor_tensor(out=w.ap[:, 40:72], in0=w.ap[:, 40:72], in1=blk_b.ap[:, 1, s, :],
                                              op=ALU.mult), reads=[w, blk_b], writes=[w])
        S.op("dve", lambda e: e.scalar_tensor_tensor(
            out=w.ap[:, 40:72], in0=blk_b.ap[:, 2, s, :], scalar=b31_b.ap[:, h:h + 1], in1=w.ap[:, 40:72],
            op0=ALU.mult, op1=ALU.add), reads=[w, blk_b, b31_b], writes=[w])
        for c in range(nch):
            ps = S.ps()
            S.op("pe", lambda e, ps=ps, c=c: e.matmul(ps.ap, lhsT=q_l, rhs=kt.ap[:, c * 512:(c + 1) * 512],
                                                      start=True, stop=True), reads=[QT, kt], writes=[ps])
            wc = c - (2 * s - 1)
            if wc >= 0:
                S.op("dve", lambda e, ps=ps, c=c, wc=wc: e.tensor_tensor(
                    out=Sb.ap[:, c * 512:(c + 1) * 512], in0=ps.ap, in1=bb.ap[:, hf, wc * 512:(wc + 1) * 512],
                    op=ALU.add), reads=[ps, bb], writes=[Sb])
            elif c % 2 == 0:
                S.op("act", lambda e, ps=ps, c=c: e.activation(out=Sb.ap[:, c * 512:(c + 1) * 512], in_=ps.ap,
                                                                func=AF.Identity), reads=[ps], writes=[Sb])
            else:
                S.op("dve", lambda e, ps=ps, c=c: e.tensor_copy(out=Sb.ap[:, c * 512:(c + 1) * 512], in_=ps.ap),
                     reads=[ps], writes=[Sb])
        S.op("dve", lambda e: e.tensor_reduce(
            out=w.ap[:, 72:72 + nblk], in_=Sb.ap[:, 0:nblk * 256].rearrange("p (n k) -> p n k", k=256),
            axis=AX.X, op=ALU.max), reads=[Sb], writes=[w])
        S.op("dve", lambda e: e.tensor_tensor(out=w.ap[:, 72:72 + nblk], in0=w.ap[:, 72:72 + nblk],
                                              in1=w.ap[:, 40:40 + nblk], op=ALU.add), reads=[w], writes=[w])
        S.op("dve", lambda e: e.tensor_reduce(out=w.ap[:, 136:137], in_=w.ap[:, 72:72 + nblk],
                                              axis=AX.X, op=ALU.max, negate=True), reads=[w], writes=[w])
        S.op("dve", lambda e: e.tensor_scalar(out=w.ap[:, 72:72 + nblk], in0=w.ap[:, 40:40 + nblk],
                                              scalar1=w.ap[:, 136:137], scalar2=None, op0=ALU.add),
             reads=[w], writes=[w])

    def stage2(idx):
        h, s, hf = iters[idx]
        vv = Vh[h % 2]
        yT = yTs[h % 2]
        nblk = 4 * s + 4
        t0 = s * 256 + hf * 128
        w = sm[idx % 2]
        rs_ = rsb[idx % 2]
        Sb = Sb2[idx % 2]
        ops = OPS[idx % 2]
        nseg = s + 1
        for sg in range(nseg):
            pbuf = Pb[segc[0] % 2]
            pq = Pq[segc[0] % 2]
            ptb = PTb[segc[0] % 2]
            segc[0] += 1
            for b4 in range(4):
                n = sg * 4 + b4
                S.op("act", lambda e, pbuf=pbuf, b4=b4, n=n: e.activation(
                    out=pbuf.ap[:, b4 * 256:(b4 + 1) * 256], in_=Sb.ap[:, n * 256:(n + 1) * 256], func=AF.Exp,
                    bias=w.ap[:, 72 + n:73 + n], accum_out=rs_.ap[:, n:n + 1]),
                    reads=[Sb, w], writes=[pq[b4], rsc[idx % 2][n]])
            ps = S.ps()
            pb = ps.ap.bitcast(BF16)
            for k8 in range(8):
                S.op("pe", lambda e, pb=pb, k8=k8, pbuf=pbuf: e.transpose(
                    out=pb[:, k8 * 128:(k8 + 1) * 128], in_=pbuf.ap[:, k8 * 128:(k8 + 1) * 128],
                    identity=ident_b.ap), reads=[pq[k8 // 2], ident_b], writes=[ps])
            if sg % 2 == 0:
                S.op("dve", lambda e, pb=pb, ptb=ptb: e.tensor_copy(
                    out=ptb.ap, in_=pb.rearrange("p (a b) -> p a b", a=8)), reads=[ps], writes=[ptb])
            else:
                S.op("act", lambda e, pb=pb, ptb=ptb: e.activation(
                    out=ptb.ap, in_=pb.rearrange("p (a b) -> p a b", a=8), func=AF.Identity),
                    reads=[ps], writes=[ptb])
            for k8 in range(8):
                ktile = sg * 8 + k8
                S.op("pe", lambda e, ptb=ptb, k8=k8, ktile=ktile, sg=sg: e.matmul(
                    ops.ap[:, 0:128], lhsT=ptb.ap[:, k8, :], rhs=vv.ap[:, ktile, :],
                    start=(sg == 0 and k8 == 0), stop=(sg == nseg - 1 and k8 == 7)),
                    reads=[ptb, vv], writes=[ops])
        S.op("dve", lambda e: e.tensor_reduce(out=w.ap[:, 137:138], in_=rs_.ap[:, 0:nblk],
                                              axis=AX.X, op=ALU.add), reads=rsc[idx % 2][0:nblk], writes=[w])
        S.op("dve", lambda e: e.reciprocal(out=w.ap[:, 138:139], in_=w.ap[:, 137:138]), reads=[w], writes=[w])
        o_sb = ob[idx % 2]
        S.op("dve", lambda e: e.tensor_scalar(out=o_sb.ap, in0=ops.ap[:, 0:128], scalar1=w.ap[:, 138:139], scalar2=None,
                                              op0=ALU.mult), reads=[ops, w], writes=[o_sb])
        ps = S.ps()
        pb = ps.ap.bitcast(BF16)
        S.op("pe", lambda e, pb=pb: e.transpose(out=pb[:, 0:128], in_=o_sb.ap, identity=ident_b.ap),
             reads=[o_sb, ident_b], writes=[ps])
        S.op("act", lambda e, pb=pb: e.activation(out=yT.ap[:, t0:t0 + 128], in_=pb[:, 0:128], func=AF.Identity),
             reads=[ps], writes=[yT])
        if s == 7 and hf == 1:
            S.dma("sp", yattT_d.ap[:, h, :], yT.ap, reads=[yT], writes=[yattT_d])

    NIT = len(iters)
    stage1(0)
    for idx in range(NIT):
        if idx + 1 < NIT:
            stage1(idx + 1)
        stage2(idx)
    S.ps_pool = list(range(8))
    A.release(mkD)
    A.release(mkC - 0)
    if stop("pD"):
        S.dma("sp", dbg_d.ap[0:1, 300:301], ones_f.ap[0:1, 0:1], reads=[ones_f, yattT_d], final=True)
        return finish()
    A.release(0 + ksum.hi)
    mkE = A.mark()
    wos = [A.alloc(f"wos{i}", [128, 8, 512], BF16) for i in range(2)]
    woa = [A.alloc(f"woa{i}", [128, 8, 512], BF16) for i in range(2)]
    ysg_t = [A.alloc(f"ysg_t{i}", [128, 8, 512], BF16) for i in range(2)]
    yat_t = [A.alloc(f"yat_t{i}", [128, 8, 512], BF16) for i in range(2)]
    gs_t = [A.alloc(f"gs_t{i}", [128, 4, 512], BF16) for i in range(2)]
    ga_t = [A.alloc(f"ga_t{i}", [128, 4, 512], BF16) for i in range(2)]
    t1 = [A.alloc(f"t1_{i}", [128, 512], F32) for i in range(2)]
    t2 = [A.alloc(f"t2_{i}", [128, 512], F32) for i in range(2)]
    mst = [A.alloc(f"mst{i}", [128, 4, 512], BF16) for i in range(2)]
    load_w(wos[0], IN.w_osg, 0, D_SG, 0, 512)
    load_w(woa[0], IN.w_oatt, 0, D_ATT, 0, 512)
    k = 0
    for cg in range(4):
        if cg + 1 < 4:
            load_w(wos[(cg + 1) % 2], IN.w_osg, 0, D_SG, (cg + 1) * 512, 512)
            load_w(woa[(cg + 1) % 2], IN.w_oatt, 0, D_ATT, (cg + 1) * 512, 512)
        ws, wa = wos[cg % 2], woa[cg % 2]
        for tg in range(4):
            ys, ya, gs, ga, ms = ysg_t[k % 2], yat_t[k % 2], gs_t[k % 2], ga_t[k % 2], mst[k % 2]
            k += 1
            tsl = slice(tg * 512, (tg + 1) * 512)
            S.dma("sp", ys.ap, ysgT_d.ap[:, :, tsl], reads=[ysgT_d], writes=[ys])
            S.dma("sp", ya.ap, yattT_d.ap[:, :, tsl], reads=[yattT_d], writes=[ya])
            S.dma("sp", gs.ap, gsg_d.ap[:, cg * 4:(cg + 1) * 4, tsl], reads=[gsg_d], writes=[gs])
            S.dma("sp", ga.ap, gatt_d.ap[:, cg * 4:(cg + 1) * 4, tsl], reads=[gatt_d], writes=[ga])
            for cl in range(4):
                ps1 = S.ps()
                for kk in range(8):
                    S.op("pe", lambda e, ps1=ps1, kk=kk, cl=cl, ws=ws, ys=ys: e.matmul(
                        ps1.ap, lhsT=ws.ap[:, kk, cl * 128:(cl + 1) * 128], rhs=ys.ap[:, kk, :],
                        start=(kk == 0), stop=(kk == 7)), reads=[ws, ys], writes=[ps1])
                ps2 = S.ps()
                for kk in range(8):
                    S.op("pe", lambda e, ps2=ps2, kk=kk, cl=cl, wa=wa, ya=ya: e.matmul(
                        ps2.ap, lhsT=wa.ap[:, kk, cl * 128:(cl + 1) * 128], rhs=ya.ap[:, kk, :],
                        start=(kk == 0), stop=(kk == 7)), reads=[wa, ya], writes=[ps2])
                a1, a2 = t1[cl % 2], t2[cl % 2]
                S.op("dve", lambda e, ps1=ps1, a1=a1, gs=gs, cl=cl: e.tensor_tensor(
                    out=a1.ap, in0=ps1.ap, in1=gs.ap[:, cl, :], op=ALU.mult), reads=[ps1, gs], writes=[a1])
                S.op("dve", lambda e, ps2=ps2, a2=a2, ga=ga, cl=cl: e.tensor_tensor(
                    out=a2.ap, in0=ps2.ap, in1=ga.ap[:, cl, :], op=ALU.mult), reads=[ps2, ga], writes=[a2])
                S.op("pool", lambda e, a1=a1, a2=a2, ms=ms, cl=cl: e.tensor_tensor(
                    out=ms.ap[:, cl, :], in0=a1.ap, in1=a2.ap, op=ALU.add), reads=[a1, a2], writes=[ms])
            S.dma("sp", mrgT_d.ap[:, cg * 4:(cg + 1) * 4, tsl], ms.ap, reads=[ms], writes=[mrgT_d])
    A.release(mkE)
    if stop("pE1"):
        S.dma("sp", dbg_d.ap[0:1, 300:301], ones_f.ap[0:1, 0:1], reads=[ones_f, mrgT_d], final=True)
        return finish()

    def bcast_row(dst, col0, dt_tmp):
        for c in range(KC):
            dg = dt_tmp[c % 2]
            S.op("dve", lambda e, dg=dg, c=c: e.tensor_scalar(out=dg.ap, in0=ident_f.ap,
                                                               scalar1=modT.ap[:, col0 + c:col0 + c + 1], scalar2=None,
                                                               op0=ALU.mult), reads=[ident_f, modT], writes=[dg])
            ps = S.ps()
            S.op("pe", lambda e, ps=ps, dg=dg: e.matmul(ps.ap[:, 0:128], lhsT=ones_f.ap, rhs=dg.ap, start=True, stop=True),
                 reads=[ones_f, dg], writes=[ps])
            S.op("act", lambda e, ps=ps, c=c: e.activation(out=dst.ap[:, c * 128:(c + 1) * 128], in_=ps.ap[:, 0:128],
                                                            func=AF.Identity), reads=[ps], writes=[dst])

    mkE2 = A.mark()
    wo_sb = A.alloc("wo_sb", [128, KC, D], BF16)
    g1_b = A.alloc("g1_b", [128, D], F32)
    dtmp = [A.alloc(f"dtmp{i}", [128, 128], F32) for i in range(2)]
    for q4 in range(4):
        S.dma("pool", wo_sb.ap[:, :, q4 * 512:(q4 + 1) * 512], wview(IN.w_o, 0, D, q4 * 512, 512), writes=[wo_sb])
    bcast_row(g1_b, G1, dtmp)
    mrg_t = [A.alloc(f"mrg_t{i}", [128, KC, 512], BF16) for i in range(2)]
    xt2 = [A.alloc(f"xt2_{i}", [128, D], F32) for i in range(2)]
    x1t = [A.alloc(f"x1t{i}", [128, D], F32) for i in range(2)]
    xnb = A.alloc("xnbE", [128, 4, D], BF16)
    junk = A.alloc("junkE", [128, D], BF16)
    stat = [A.alloc(f"statE{i}", [128, 4], F32) for i in range(4)]
    h2g = A.alloc("h2g", [128, KC, 512], BF16)
    pp = A.alloc("ppE", [128, 512], F32)

    def norm_tile(xt, ss, xnb, tt, junk):
        S.op("act", lambda e: e.activation(out=junk.ap, in_=xt.ap, func=AF.Square, accum_out=ss.ap[:, 0:1]),
             reads=[xt], writes=[junk, ss])
        S.op("act", lambda e: e.activation(out=ss.ap[:, 1:2], in_=ss.ap[:, 0:1], func=AF.Sqrt, scale=1.0 / D, bias=EPS),
             reads=[ss], writes=[ss])
        S.op("dve", lambda e: e.reciprocal(out=ss.ap[:, 2:3], in_=ss.ap[:, 1:2]), reads=[ss], writes=[ss])
        S.op("act", lambda e: e.activation(out=xnb.ap[:, tt, :], in_=xt.ap, func=AF.Identity, scale=ss.ap[:, 2:3]),
             reads=[xt, ss], writes=[xnb])

    def transpose_group(xnb, gs, sh_col, hT):
        for kc in range(KC):
            ps = S.ps()
            pb = ps.ap.bitcast(BF16)
            for tt in range(4):
                S.op("pe", lambda e, pb=pb, tt=tt, kc=kc: e.transpose(
                    out=pb[:, tt * 128:(tt + 1) * 128], in_=xnb.ap[:, tt, kc * 128:(kc + 1) * 128],
                    identity=ident_b.ap), reads=[xnb, ident_b], writes=[ps])
            S.op("dve", lambda e, pb=pb, kc=kc: e.tensor_scalar(
                out=hT.ap[:, kc, :], in0=pb[:, 0:512], scalar1=gs.ap[:, kc:kc + 1],
                scalar2=modT.ap[:, sh_col + kc:sh_col + kc + 1], op0=ALU.mult, op1=ALU.add),
                reads=[ps, gs, modT], writes=[hT])

    for tg in range(4):
        mt = mrg_t[tg % 2]
        S.dma("sp", mt.ap, mrgT_d.ap[:, :, tg * 512:(tg + 1) * 512], reads=[mrgT_d], writes=[mt])
        for tt in range(4):
            r0 = tg * 512 + tt * 128
            xt = xt2[tt % 2]
            x1 = x1t[tt % 2]
            S.dma("sp", xt.ap, IN.xo[r0:r0 + 128, :], writes=[xt])
            for cc in range(4):
                ps = S.ps()
                for kc in range(KC):
                    S.op("pe", lambda e, ps=ps, kc=kc, tt=tt, cc=cc, mt=mt: e.matmul(
                        ps.ap, lhsT=mt.ap[:, kc, tt * 128:(tt + 1) * 128], rhs=wo_sb.ap[:, kc, cc * 512:(cc + 1) * 512],
                        start=(kc == 0), stop=(kc == KC - 1)), reads=[mt, wo_sb], writes=[ps])
                S.op("dve", lambda e, ps=ps, cc=cc: e.tensor_tensor(out=pp.ap, in0=ps.ap, in1=g1_b.ap[:, cc * 512:(cc + 1) * 512],
                                                                    op=ALU.mult), reads=[ps, g1_b], writes=[pp])
                S.op("pool", lambda e, cc=cc, xt=xt, x1=x1: e.tensor_tensor(
                    out=x1.ap[:, cc * 512:(cc + 1) * 512], in0=pp.ap, in1=xt.ap[:, cc * 512:(cc + 1) * 512], op=ALU.add),
                    reads=[pp, xt], writes=[x1])
            S.dma("sp", x1_d.ap[r0:r0 + 128, :], x1.ap, reads=[x1], writes=[x1_d])
            norm_tile(x1, stat[tt], xnb, tt, junk)
        transpose_group(xnb, g2s, SH2, h2g)
        S.dma("sp", h2T_d.ap[:, :, tg * 512:(tg + 1) * 512], h2g.ap, reads=[h2g], writes=[h2T_d])
    A.release(mkE2)
    if stop("pE2"):
        S.dma("sp", dbg_d.ap[0:1, 300:301], ones_f.ap[0:1, 0:1], reads=[ones_f, x1_d, h2T_d], final=True)
        return finish()

    g2_b = A.alloc("g2_b", [128, D], BF16)
    mkg = A.mark()
    g2_f = A.alloc("g2_f", [128, D], F32)
    dtmp = [A.alloc(f"dtmpF{i}", [128, 128], F32) for i in range(2)]
    bcast_row(g2_f, G2, dtmp)
    S.op("dve", lambda e: e.tensor_copy(out=g2_b.ap, in_=g2_f.ap), reads=[g2_f], writes=[g2_b])
    A.release(mkg)
    wr_f = A.alloc("wr_f", [128, KC, 20], F32)
    wr_b = A.alloc("wr_b", [128, KC, 20], BF16)
    S.dma("sp", wr_f.ap, IN.w_rt.rearrange("(k p) n -> p k n", p=128), writes=[wr_f], shared=True)
    S.op("dve", lambda e: e.tensor_copy(out=wr_b.ap, in_=wr_f.ap), reads=[wr_f], writes=[wr_b])
    h2h = A.alloc("h2h", [128, KC, 1024], BF16)
    accb = [A.alloc(f"accb{i}", [128, D], F32) for i in range(8)]
    acc = [[subtile(accb[t], f"acc{t}_{c}", accb[t].ap[:, c * 512:(c + 1) * 512]) for c in range(4)] for t in range(8)]
    ring = [A.alloc(f"ering{i}", [128, KC, 512], BF16) for i in range(4)]
    scr = A.alloc("scr", [128, D], F32)
    s1v = scr.ap.bitcast(BF16).rearrange("p (a b) -> p a b", a=8)[:, 0:8, 0:512] if False else None
    s1_full = scr.ap.bitcast(BF16)
    ATb = A.alloc("ATb", [128, 4, 1024], BF16)
    gts = A.alloc("gts", [128, 8, 16], F32)
    rw = A.alloc("rw", [128, 64], F32)
    pad8 = A.alloc("pad8", [128, 8], F32)
    fst = [A.alloc(f"fst{i}", [128, 4], F32) for i in range(2)]
    junkF = A.alloc("junkF", [128, 512], BF16)
    S.op("dve", lambda e: e.memset(pad8.ap, -BIG), writes=[pad8])

    def w2view(eidx, c0, ncols):
        return IN.w_ed[eidx][:, c0:c0 + ncols].rearrange("(k p) n -> p k n", p=128)

    for half in range(2):
        S.dma("sp", h2h.ap, h2T_d.ap[:, :, half * 1024:(half + 1) * 1024], reads=[h2T_d], writes=[h2h])
        for tt in range(8):
            r0 = half * 1024 + tt * 128
            S.dma("sp", accb[tt].ap, x1_d.ap[r0:r0 + 128, :], reads=[x1_d], writes=acc[tt])
        for tt in range(8):
            ps = S.ps()
            for kc in range(KC):
                S.op("pe", lambda e, ps=ps, kc=kc, tt=tt: e.matmul(
                    ps.ap[:, 0:20], lhsT=h2h.ap[:, kc, tt * 128:(tt + 1) * 128], rhs=wr_b.ap[:, kc, :],
                    start=(kc == 0), stop=(kc == KC - 1)), reads=[h2h, wr_b], writes=[ps])
            R_ = rw.ap
            dv = lambda fn, rd=(rw,), wr=(rw,): S.op("dve", fn, reads=list(rd), writes=list(wr))
            S.op("dve", lambda e, ps=ps: e.tensor_copy(out=R_[:, 0:20], in_=ps.ap[:, 0:20]), reads=[ps], writes=[rw])
            dv(lambda e: e.tensor_reduce(out=R_[:, 20:21], in_=R_[:, 0:4], axis=AX.X, op=ALU.max, negate=True))
            dv(lambda e: e.tensor_scalar(out=R_[:, 21:25], in0=R_[:, 0:4], scalar1=R_[:, 20:21], scalar2=0.0,
                                         op0=ALU.add, op1=ALU.is_ge))
            S.op("act", lambda e: e.activation(out=R_[:, 25:29], in_=R_[:, 0:4], func=AF.Exp, bias=R_[:, 20:21],
                                               accum_out=R_[:, 29:30]), reads=[rw], writes=[rw])
            dv(lambda e: e.reciprocal(out=R_[:, 30:31], in_=R_[:, 29:30]))
            dv(lambda e: e.tensor_scalar(out=R_[:, 31:35], in0=R_[:, 4:8], scalar1=R_[:, 21:22], scalar2=None, op0=ALU.mult))
            for g in range(1, 4):
                dv(lambda e, g=g: e.scalar_tensor_tensor(out=R_[:, 31:35], in0=R_[:, 4 + 4 * g:8 + 4 * g],
                                                         scalar=R_[:, 21 + g:22 + g], in1=R_[:, 31:35],
                                                         op0=ALU.mult, op1=ALU.add))
            S.op("dve", lambda e: e.tensor_copy(out=pad8.ap[:, 0:4], in_=R_[:, 31:35]), reads=[rw], writes=[pad8])
            S.op("dve", lambda e: e.max(out=R_[:, 35:43], in_=pad8.ap), reads=[pad8], writes=[rw])
            dv(lambda e: e.tensor_tensor(out=R_[:, 43:44], in0=R_[:, 36:37], in1=R_[:, 35:36], op=ALU.subtract))
            S.op("act", lambda e: e.activation(out=R_[:, 44:45], in_=R_[:, 43:44], func=AF.Exp), reads=[rw], writes=[rw])
            dv(lambda e: e.tensor_scalar(out=R_[:, 45:46], in0=R_[:, 44:45], scalar1=1.0, scalar2=None, op0=ALU.add))
            dv(lambda e: e.reciprocal(out=R_[:, 46:47], in_=R_[:, 45:46]))
            dv(lambda e: e.tensor_tensor(out=R_[:, 47:48], in0=R_[:, 44:45], in1=R_[:, 46:47], op=ALU.mult))
            dv(lambda e: e.tensor_scalar(out=R_[:, 48:52], in0=R_[:, 31:35], scalar1=R_[:, 35:36], scalar2=R_[:, 46:47],
                                         op0=ALU.is_ge, op1=ALU.mult))
            dv(lambda e: e.tensor_scalar(out=R_[:, 52:56], in0=R_[:, 31:35], scalar1=R_[:, 36:37], scalar2=R_[:, 47:48],
                                         op0=ALU.is_equal, op1=ALU.mult))
            dv(lambda e: e.tensor_tensor(out=R_[:, 56:60], in0=R_[:, 48:52], in1=R_[:, 52:56], op=ALU.add))
            dv(lambda e: e.tensor_scalar(out=R_[:, 56:60], in0=R_[:, 56:60], scalar1=R_[:, 30:31], scalar2=None, op0=ALU.mult))
            for g in range(4):
                S.op("dve", lambda e, g=g, tt=tt: e.tensor_scalar(out=gts.ap[:, tt, 4 * g:4 * g + 4], in0=R_[:, 56:60],
                                                                   scalar1=R_[:, 21 + g:22 + g], scalar2=None, op0=ALU.mult),
                     reads=[rw], writes=[gts])
        stage = 0

        def issue(st):
            eidx, kind = st // 3, st % 3
            sl = ring[st % 4]
            wt_ = wb[eidx][kind]
            if kind < 2:
                S.dma("sp", sl.ap, wt_.ap.rearrange("(k p) n -> p k n", p=128), reads=[wt_], writes=[sl])
            else:
                v = sl.ap.rearrange("p k n -> p (k n)").rearrange("p (k n) -> p k n", k=4)
                S.dma("sp", v, wt_.ap.rearrange("(k p) n -> p k n", p=128), reads=[wt_], writes=[sl])
                for k4 in range(4):
                    S.op("pool", lambda e, v=v, k4=k4: e.tensor_tensor(
                        out=v[:, k4, :], in0=v[:, k4, :], in1=g2_b.ap, op=ALU.mult),
                        reads=[sl, g2_b], writes=[sl])

        NST = NEXP * 3
        for st in range(min(3, NST)):
            issue(st)
        for eidx in range(NEXP):
            for kind in range(3):
                st = eidx * 3 + kind
                if st + 3 < NST:
                    issue(st + 3)
                sl = ring[st % 4]
                if kind == 0:
                    for tg in range(2):
                        for fc in range(4):
                            ps = S.ps()
                            for kc in range(KC):
                                S.op("pe", lambda e, ps=ps, kc=kc, fc=fc, tg=tg, sl=sl: e.matmul(
                                    ps.ap, lhsT=sl.ap[:, kc, fc * 128:(fc + 1) * 128], rhs=h2h.ap[:, kc, tg * 512:(tg + 1) * 512],
                                    start=(kc == 0), stop=(kc == KC - 1)), reads=[sl, h2h], writes=[ps])
                            o0 = (tg * 4 + fc) * 512
                            S.op("act", lambda e, ps=ps, o0=o0: e.activation(out=s1_full[:, o0:o0 + 512], in_=ps.ap,
                                                                              func=AF.Silu), reads=[ps], writes=[scr])
                elif kind == 1:
                    for tg in range(2):
                        for fc in range(4):
                            ps = S.ps()
                            for kc in range(KC):
                                S.op("pe", lambda e, ps=ps, kc=kc, fc=fc, tg=tg, sl=sl: e.matmul(
                                    ps.ap, lhsT=sl.ap[:, kc, fc * 128:(fc + 1) * 128], rhs=h2h.ap[:, kc, tg * 512:(tg + 1) * 512],
                                    start=(kc == 0), stop=(kc == KC - 1)), reads=[sl, h2h], writes=[ps])
                            o0 = (tg * 4 + fc) * 512
                            S.op("dve", lambda e, ps=ps, o0=o0, fc=fc, tg=tg: e.tensor_tensor(
                                out=ATb.ap[:, fc, tg * 512:(tg + 1) * 512], in0=ps.ap, in1=s1_full[:, o0:o0 + 512], op=ALU.mult),
                                reads=[ps, scr], writes=[ATb])
                else:
                    v = sl.ap.rearrange("p k n -> p (k n)").rearrange("p (k n) -> p k n", k=4)
                    for tt in range(8):
                        for cc in range(4):
                            ps = S.ps()
                            for fc in range(4):
                                S.op("pe", lambda e, ps=ps, fc=fc, tt=tt, cc=cc, v=v: e.matmul(
                                    ps.ap, lhsT=ATb.ap[:, fc, tt * 128:(tt + 1) * 128], rhs=v[:, fc, cc * 512:(cc + 1) * 512],
                                    start=(fc == 0), stop=(fc == 3)), reads=[ATb, sl], writes=[ps])
                            a = acc[tt][cc]
                            S.op("dve", lambda e, ps=ps, a=a, tt=tt, eidx=eidx: e.scalar_tensor_tensor(
                                out=a.ap, in0=ps.ap, scalar=gts.ap[:, tt, eidx:eidx + 1], in1=a.ap, op0=ALU.mult, op1=ALU.add),
                                reads=[ps, gts, a], writes=[a])
        S.dma("sp", scr.ap, IN.fnw.partition_broadcast(128), writes=[scr])
        for tt in range(8):
            ss = fst[tt % 2]
            r0 = half * 1024 + tt * 128
            for cc in range(4):
                S.op("act", lambda e, tt=tt, cc=cc, ss=ss: e.activation(
                    out=junkF.ap, in_=acc[tt][cc].ap, func=AF.Square, accum_out=ss.ap[:, cc:cc + 1]),
                    reads=[acc[tt][cc]], writes=[junkF, ss])
            S.op("dve", lambda e, ss=ss: e.tensor_reduce(out=ss.ap[:, 0:1], in_=ss.ap[:, 0:4], axis=AX.X, op=ALU.add),
                 reads=[ss], writes=[ss])
            S.op("act", lambda e, ss=ss: e.activation(out=ss.ap[:, 1:2], in_=ss.ap[:, 0:1], func=AF.Sqrt, scale=1.0 / D,
                                                      bias=EPS), reads=[ss], writes=[ss])
            S.op("dve", lambda e, ss=ss: e.reciprocal(out=ss.ap[:, 2:3], in_=ss.ap[:, 1:2]), reads=[ss], writes=[ss])
            for cc in range(4):
                a = acc[tt][cc]
                S.op("dve", lambda e, a=a, cc=cc, ss=ss: e.scalar_tensor_tensor(
                    out=a.ap, in0=a.ap, scalar=ss.ap[:, 2:3], in1=scr.ap[:, cc * 512:(cc + 1) * 512],
                    op0=ALU.mult, op1=ALU.mult), reads=[a, ss, scr], writes=[a])
            S.dma("sp", out_d[r0:r0 + 128, :], accb[tt].ap, reads=acc[tt], semtile=accb[tt], final=True)
    return finish()


def host_prepare(inp):
    f = lambda a: np.ascontiguousarray(np.asarray(a, dtype=np.float32))
    x = f(inp["x"])
    shared = {
        "w_ada": f(inp["w_ada"][0]),
        "b_adaT": f(np.asarray(inp["b_ada"][0]).reshape(96, 128).T),
        "n1T": f(np.asarray(inp["norm1_w"][0]).reshape(KC, 128).T),
        "n2T": f(np.asarray(inp["norm2_w"][0]).reshape(KC, 128).T),
        "fnw": f(np.asarray(inp["final_norm_w"]).reshape(1, D)),
        "w_in": f(inp["w_in"][0]),
        "lnw": f(np.asarray(inp["sg_ln_w"][0]).reshape(1, D_SG)),
        "lnb": f(np.asarray(inp["sg_ln_b"][0]).reshape(1, D_SG)),
        "w_sp": f(inp["w_spatial"][0]),
        "b_spT": f(np.asarray(inp["b_spatial"][0]).T),
        "relb": f(inp["rel_bias"]),
        "w_osg": f(inp["w_out_sg"][0]),
        "w_oatt": f(inp["w_out_att"][0]),
        "w_o": f(inp["w_o"][0]),
        "w_rt": f(np.concatenate([np.asarray(inp["w_router_group"][0]), np.asarray(inp["w_router_expert"][0])], axis=1)),
        "w_eg": f(inp["w_exp_gate"][0]),
        "w_eu": f(inp["w_exp_up"][0]),
        "w_ed": f(inp["w_exp_down"][0]),
    }
    maps = []
    for core in range(NCORE):
        b, j = core // 4, core % 4
        m = dict(shared)
        m["xs"] = x[b]
        xb = x[b].reshape(NBLK, 256, D)
        m["xo"] = np.ascontiguousarray(xb[j::4].reshape(TOK, D))
        m["cT"] = f(np.asarray(inp["c"][b]).reshape(KC, 128).T)
        m.update(host_consts(j))
        maps.append(m)
    return maps


def assemble(outs):
    res = np.zeros((BATCH, SEQ, D), dtype=np.float32)
    for core in range(NCORE):
        b, j = core // 4, core % 4
        res[b].reshape(NBLK, 256, D)[j::4] = outs[core].reshape(8, 256, D)
    return res


_NC_CACHE = {}


def kernel(**inputs):
    if "nc" not in _NC_CACHE:
        _NC_CACHE["nc"] = build_program()
    nc = _NC_CACHE["nc"]
    maps = host_prepare(inputs)
    maps = [{k: m[k] for k in nc._declared_inputs} for m in maps]
    res = run_bass_kernel_spmd(nc, maps, core_ids=list(range(NCORE)))
    return assemble([np.asarray(r["out"]) for r in res.results])
```
